# Optimizing a Trainium2 kernel written in Bass

```python
import math
import jax, jax.numpy as jnp
from jax import lax
import numpy as np

D_MODEL = 2048
BATCH = 4
SEQ = 2048
DEPTH = 2

HEAD_DIM = 128
ATTN_W = D_MODEL // 2
ATTN_HEADS = ATTN_W // HEAD_DIM
KV_HEADS = 2
KV_W = KV_HEADS * HEAD_DIM
IDX_HEADS = 8
IDX_DIM = 64
MAX_TOPK = 256
Q_BLOCK = 128
SSM_W = D_MODEL // 4
SSM_GROUP_CH = 16
SSM_GROUPS = SSM_W // SSM_GROUP_CH
SSM_STATE = 64
POOL_W = D_MODEL // 4
POOL_WINDOWS = (2, 4, 8, 16)
POOL_GROUPS = len(POOL_WINDOWS)
POOL_GROUP_CH = POOL_W // POOL_GROUPS
MIX_W = ATTN_W + SSM_W + POOL_W
IN_SIZES = (ATTN_W, KV_W, KV_W, IDX_HEADS * IDX_DIM, IDX_DIM, IDX_HEADS, SSM_W, POOL_W)
IN_W = sum(IN_SIZES)
ROPE_THETA = 500000.0
ROPE_FRAC = 4
LN_EPS = 1e-5
DEEPNORM_ALPHA = (2.0 * DEPTH) ** 0.25
DEEPNORM_BETA = (8.0 * DEPTH) ** -0.25
N_EXPERTS = 16
N_EXPERT_GROUPS = 4
EXPERTS_PER_GROUP = N_EXPERTS // N_EXPERT_GROUPS
TOP_K_EXPERTS = 2
D_FF_EXPERT = 1024

kernel_name = "hybrid_dsa_s5_pool_moe_deepnorm"


def layer_norm(x, g, b):
    xf = x.astype(jnp.float32)
    mu = jnp.mean(xf, axis=-1, keepdims=True)
    var = jnp.mean(jnp.square(xf - mu), axis=-1, keepdims=True)
    return ((xf - mu) * lax.rsqrt(var + LN_EPS) * g + b).astype(x.dtype)


def partial_rope(x, positions):
    d = x.shape[-1]
    rot = d // ROPE_FRAC
    half = rot // 2
    inv_freq = ROPE_THETA ** (-jnp.arange(half, dtype=jnp.float32) * 2.0 / rot)
    ang = positions.astype(jnp.float32)[..., None] * inv_freq
    cos = jnp.cos(ang)[:, :, None, :]
    sin = jnp.sin(ang)[:, :, None, :]
    xr = x[..., :rot].astype(jnp.float32)
    x1, x2 = xr[..., :half], xr[..., half:]
    rotated = jnp.concatenate([x1 * cos - x2 * sin, x2 * cos + x1 * sin], axis=-1)
    return jnp.concatenate([rotated.astype(x.dtype), x[..., rot:]], axis=-1)


def dsa_attention(q, k, v, q_idx, k_idx, w_idx):
    bsz, seq = q.shape[0], q.shape[1]
    top_k = min(MAX_TOPK, seq // 4)
    n_blocks = seq // Q_BLOCK
    group = ATTN_HEADS // KV_HEADS
    key_pos = jnp.arange(seq, dtype=jnp.int32)
    t_blocks = key_pos.reshape(n_blocks, Q_BLOCK)
    k_idx_f = k_idx.astype(jnp.float32)

    def to_blocks(a):
        return a.reshape(bsz, n_blocks, Q_BLOCK, *a.shape[2:]).swapaxes(0, 1)

    def one_block(args):
        qb, qib, wb, tb = args
        rel = jax.nn.relu(jnp.einsum("bqhd,bsd->bqhs", qib.astype(jnp.float32), k_idx_f)) * (IDX_DIM ** -0.5)
        iscore = jnp.einsum("bqhs,bqh->bqs", rel, wb.astype(jnp.float32)) * (IDX_HEADS ** -0.5)
        causal = key_pos[None, :] <= tb[:, None]
        iscore = jnp.where(causal[None], iscore, -jnp.inf)
        _, sel = lax.top_k(iscore, top_k)
        valid = sel <= tb[None, :, None]
        k_sel = jax.vmap(lambda kk, ii: kk[ii])(k, sel)
        v_sel = jax.vmap(lambda vv, ii: vv[ii])(v, sel)
        qg = qb.reshape(bsz, Q_BLOCK, KV_HEADS, group, HEAD_DIM)
        s = jnp.einsum("bqkgd,bqnkd->bqkgn", qg, k_sel).astype(jnp.float32) * (HEAD_DIM ** -0.5)
        s = jnp.where(valid[:, :, None, None, :], s, -jnp.inf)
        p = jax.nn.softmax(s, axis=-1).astype(v.dtype)
        o = jnp.einsum("bqkgn,bqnkd->bqkgd", p, v_sel)
        return o.reshape(bsz, Q_BLOCK, ATTN_W)

    out = lax.map(one_block, (to_blocks(q), to_blocks(q_idx), to_blocks(w_idx), t_blocks))
    return out.swapaxes(0, 1).reshape(bsz, seq, ATTN_W)


def s5_mixer(u, lam_re, lam_im, log_step, b_re, b_im, c_re, c_im, d_skip, w_glu, b_glu):
    bsz, seq, _ = u.shape
    f32 = jnp.float32
    uf = u.astype(f32).reshape(bsz, seq, SSM_GROUPS, SSM_GROUP_CH)
    lam = lax.complex(lam_re.astype(f32), lam_im.astype(f32))
    step = jnp.exp(log_step.astype(f32))[:, None]
    lam_bar = jnp.exp(lam * step)
    b = lax.complex(b_re.astype(f32), b_im.astype(f32))
    b_bar = ((lam_bar - 1.0) / lam)[..., None] * b
    bu = jnp.einsum("blgc,gpc->blgp", uf.astype(jnp.complex64), b_bar)
    a = jnp.broadcast_to(lam_bar, bu.shape)

    def combine(left, right):
        a_l, b_l = left
        a_r, b_r = right
        return a_r * a_l, a_r * b_l + b_r

    _, states = lax.associative_scan(combine, (a, bu), axis=1)
    cmat = lax.complex(c_re.astype(f32), c_im.astype(f32))
    y = jnp.real(jnp.einsum("blgp,gcp->blgc", states, cmat))
    y = y + d_skip.astype(f32).reshape(SSM_GROUPS, SSM_GROUP_CH) * uf
    y = jax.nn.gelu(y.reshape(bsz, seq, SSM_W))
    y = y * jax.nn.sigmoid(y @ w_glu.astype(f32) + b_glu.astype(f32))
    return y.astype(u.dtype)


def pool_mixer(u, w_pool, pool_scale):
    bsz, seq, _ = u.shape
    uf = u.astype(jnp.float32).reshape(bsz, seq, POOL_GROUPS, POOL_GROUP_CH)
    cs = jnp.concatenate([jnp.zeros_like(uf[:, :1]), jnp.cumsum(uf, axis=1)], axis=1)
    t = jnp.arange(seq, dtype=jnp.int32)
    outs = []
    for g, win in enumerate(POOL_WINDOWS):
        start = jnp.maximum(t + 1 - win, 0)
        cg = cs[:, :, g]
        window_sum = cg[:, 1:] - cg[:, start]
        count = jnp.minimum(t + 1, win).astype(jnp.float32)
        outs.append(window_sum / count[None, :, None] - uf[:, :, g])
    pooled = jnp.stack(outs, axis=2)
    y = jnp.einsum("blgc,gcd->blgd", pooled, w_pool.astype(jnp.float32)).reshape(bsz, seq, POOL_W)
    return (y * pool_scale.astype(jnp.float32)).astype(u.dtype)


def grouped_moe(u, w_router, e_gate, e_up, e_down):
    bsz, seq, d = u.shape
    xt = u.reshape(-1, d)
    n_tok = xt.shape[0]
    probs = jax.nn.softmax(xt.astype(jnp.float32) @ w_router.astype(jnp.float32), axis=-1)
    grp = probs.reshape(n_tok, N_EXPERT_GROUPS, EXPERTS_PER_GROUP)
    grp_score = jnp.sum(lax.top_k(grp, TOP_K_EXPERTS)[0], axis=-1)
    best = jnp.argmax(grp_score, axis=-1)
    in_grp = jnp.take_along_axis(grp, best[:, None, None], axis=1)[:, 0]
    top_p, top_i = lax.top_k(in_grp, TOP_K_EXPERTS)
    top_p = top_p / jnp.sum(top_p, axis=-1, keepdims=True)
    expert_ids = best[:, None] * EXPERTS_PER_GROUP + top_i
    gates = jnp.sum(jax.nn.one_hot(expert_ids, N_EXPERTS, dtype=jnp.float32) * top_p[..., None], axis=1)
    out = jnp.zeros((n_tok, d), jnp.float32)
    for e in range(N_EXPERTS):
        h = jax.nn.silu(xt @ e_gate[e]) * (xt @ e_up[e])
        out = out + gates[:, e:e + 1] * (h @ e_down[e]).astype(jnp.float32)
    return out.astype(u.dtype).reshape(bsz, seq, d)


def setup_inputs(seed: int = 0) -> dict:
    key = jax.random.key(seed)
    ks = jax.random.split(key, 32)
    f32 = jnp.float32
    nrm = lambda k, shape, s: jax.random.normal(k, shape, f32) * s
    x = jax.random.normal(ks[0], (BATCH, SEQ, D_MODEL), f32)
    c = jax.random.normal(ks[1], (BATCH, D_MODEL), f32)
    offset = jax.random.randint(ks[2], (BATCH, 1), 0, 4096, dtype=jnp.int32)
    positions = offset + jnp.arange(SEQ, dtype=jnp.int32)[None, :]
    w_ada = nrm(ks[3], (DEPTH, D_MODEL, 6 * D_MODEL), 0.1 * D_MODEL ** -0.5)
    b_ada = nrm(ks[4], (DEPTH, 6 * D_MODEL), 0.02)
    w_in = nrm(ks[5], (DEPTH, D_MODEL, IN_W), D_MODEL ** -0.5)
    w_out = nrm(ks[6], (DEPTH, MIX_W, D_MODEL), DEEPNORM_BETA * MIX_W ** -0.5)
    n_idx = jnp.arange(SSM_STATE, dtype=f32)
    ssm_lam_re = -0.5 + nrm(ks[7], (DEPTH, SSM_GROUPS, SSM_STATE), 0.01)
    ssm_lam_im = math.pi * n_idx[None, None, :] + nrm(ks[8], (DEPTH, SSM_GROUPS, SSM_STATE), 0.01)
    ssm_log_step = jax.random.uniform(ks[9], (DEPTH, SSM_GROUPS), f32, math.log(1e-3), math.log(1e-1))
    bs = (0.5 / SSM_GROUP_CH) ** 0.5
    cs = (0.5 / SSM_STATE) ** 0.5
    ssm_b_re = nrm(ks[10], (DEPTH, SSM_GROUPS, SSM_STATE, SSM_GROUP_CH), bs)
    ssm_b_im = nrm(ks[11], (DEPTH, SSM_GROUPS, SSM_STATE, SSM_GROUP_CH), bs)
    ssm_c_re = nrm(ks[12], (DEPTH, SSM_GROUPS, SSM_GROUP_CH, SSM_STATE), cs)
    ssm_c_im = nrm(ks[13], (DEPTH, SSM_GROUPS, SSM_GROUP_CH, SSM_STATE), cs)
    ssm_d = nrm(ks[14], (DEPTH, SSM_W), 1.0)
    ssm_w_glu = nrm(ks[15], (DEPTH, SSM_W, SSM_W), SSM_W ** -0.5)
    ssm_b_glu = nrm(ks[16], (DEPTH, SSM_W), 0.02)
    pool_w = nrm(ks[17], (DEPTH, POOL_GROUPS, POOL_GROUP_CH, POOL_GROUP_CH), POOL_GROUP_CH ** -0.5)
    pool_scale = 1.0 + nrm(ks[18], (DEPTH, POOL_W), 0.02)
    ln1_g = 1.0 + nrm(ks[19], (DEPTH, D_MODEL), 0.02)
    ln1_b = nrm(ks[20], (DEPTH, D_MODEL), 0.02)
    ln2_g = 1.0 + nrm(ks[21], (DEPTH, D_MODEL), 0.02)
    ln2_b = nrm(ks[22], (DEPTH, D_MODEL), 0.02)
    w_router = nrm(ks[23], (D_MODEL, N_EXPERTS), D_MODEL ** -0.5)
    e_gate = nrm(ks[24], (DEPTH, N_EXPERTS, D_MODEL, D_FF_EXPERT), D_MODEL ** -0.5)
    e_up = nrm(ks[25], (DEPTH, N_EXPERTS, D_MODEL, D_FF_EXPERT), D_MODEL ** -0.5)
    e_down = nrm(ks[26], (DEPTH, N_EXPERTS, D_FF_EXPERT, D_MODEL), DEEPNORM_BETA * D_FF_EXPERT ** -0.5)
    return {"x": x, "c": c, "positions": positions, "w_ada": w_ada, "b_ada": b_ada,
            "w_in": w_in, "w_out": w_out, "ssm_lam_re": ssm_lam_re, "ssm_lam_im": ssm_lam_im,
            "ssm_log_step": ssm_log_step, "ssm_b_re": ssm_b_re, "ssm_b_im": ssm_b_im,
            "ssm_c_re": ssm_c_re, "ssm_c_im": ssm_c_im, "ssm_d": ssm_d, "ssm_w_glu": ssm_w_glu,
            "ssm_b_glu": ssm_b_glu, "pool_w": pool_w, "pool_scale": pool_scale,
            "ln1_g": ln1_g, "ln1_b": ln1_b, "ln2_g": ln2_g, "ln2_b": ln2_b,
            "w_router": w_router, "e_gate": e_gate, "e_up": e_up, "e_down": e_down}


def reference(x, c, positions, w_ada, b_ada, w_in, w_out, ssm_lam_re, ssm_lam_im, ssm_log_step,
              ssm_b_re, ssm_b_im, ssm_c_re, ssm_c_im, ssm_d, ssm_w_glu, ssm_b_glu, pool_w, pool_scale,
              ln1_g, ln1_b, ln2_g, ln2_b, w_router, e_gate, e_up, e_down):
    bsz, seq, _ = x.shape
    cond = jax.nn.silu(c)
    offs = [0]
    for s in IN_SIZES:
        offs.append(offs[-1] + s)
    for l in range(DEPTH):
        ada = cond @ w_ada[l] + b_ada[l]
        sh1, sc1, g1, sh2, sc2, g2 = [a[:, None, :] for a in jnp.split(ada, 6, axis=-1)]
        u = x * (1.0 + sc1) + sh1
        proj = u @ w_in[l]
        q, k, v, iq, ik, iw, us, up = [proj[..., offs[i]:offs[i + 1]] for i in range(len(IN_SIZES))]
        q = partial_rope(q.reshape(bsz, seq, ATTN_HEADS, HEAD_DIM), positions)
        k = partial_rope(k.reshape(bsz, seq, KV_HEADS, HEAD_DIM), positions)
        v = v.reshape(bsz, seq, KV_HEADS, HEAD_DIM)
        iq = partial_rope(iq.reshape(bsz, seq, IDX_HEADS, IDX_DIM), positions)
        ik = partial_rope(ik[:, :, None, :], positions)[:, :, 0, :]
        y_attn = dsa_attention(q, k, v, iq, ik, iw)
        y_ssm = s5_mixer(us, ssm_lam_re[l], ssm_lam_im[l], ssm_log_step[l], ssm_b_re[l], ssm_b_im[l],
                         ssm_c_re[l], ssm_c_im[l], ssm_d[l], ssm_w_glu[l], ssm_b_glu[l])
        y_pool = pool_mixer(up, pool_w[l], pool_scale[l])
        mix = jnp.concatenate([y_attn, y_ssm, y_pool], axis=-1) @ w_out[l]
        x = layer_norm(DEEPNORM_ALPHA * x + (1.0 + g1) * mix, ln1_g[l], ln1_b[l])
        u2 = x * (1.0 + sc2) + sh2
        ffn = grouped_moe(u2, w_router, e_gate[l], e_up[l], e_down[l])
        x = layer_norm(DEEPNORM_ALPHA * x + (1.0 + g2) * ffn, ln2_g[l], ln2_b[l])
    return x
```

```python
import math
import contextlib
import numpy as np
import concourse.bass as bass
import concourse.mybir as mybir
from concourse.bass_utils import run_bass_kernel_spmd

F32 = mybir.dt.float32
BF16 = mybir.dt.bfloat16
I32 = mybir.dt.int32
AF = mybir.ActivationFunctionType
ALU = mybir.AluOpType
AX = mybir.AxisListType

ENGINES = ("sync", "scalar", "vector", "gpsimd", "tensor")
ALPHA = (2.0 * 2) ** 0.25
LN_EPS = 1e-5
PI = math.pi
NEG = -1e30

OQ, OK_, OV, OIQ, OIK, OIW, OUS, OUP = 0, 1024, 1280, 1536, 2048, 2112, 2120, 2632


class Prog:
    def __init__(self, nc):
        self.nc = nc
        self.ops = {e: [] for e in ENGINES}
        self.res = {}
        self.streams = {}
        self.pending = {e: {} for e in ENGINES}

    def _add(self, engine, fn, reads, writes, dma_key=None):
        skey = ("dma", dma_key) if dma_key is not None else ("eng", engine)
        st = self.streams.setdefault(skey, [])
        deps = dict(self.pending[engine])
        self.pending[engine] = {}

        def need(tok):
            if tok is None:
                return
            k, i = tok
            if k == skey and engine == "tensor" and dma_key is None:
                return
            if k[0] == "dma":
                i = len(self.streams[k]) - 1
                if k == skey:
                    i = len(st) - 1
            if i >= 0 and deps.get(k, -1) < i:
                deps[k] = i

        for r in reads:
            ent = self.res.get(r)
            if ent is not None:
                need(ent[0])
        for w in writes:
            ent = self.res.get(w)
            if ent is not None:
                need(ent[0])
                for t in ent[1]:
                    need(t)
        op = dict(fn=fn, deps=deps, skey=skey, idx=len(st), marked=False)
        st.append(op)
        self.ops[engine].append(op)
        tok = (skey, op["idx"])
        for w in writes:
            self.res[w] = [tok, []]
        for r in reads:
            if r in writes:
                continue
            ent = self.res.setdefault(r, [None, []])
            ent[1].append(tok)
        return tok

    def op(self, engine, fn, reads=(), writes=()):
        return self._add(engine, fn, reads, writes)

    def dma(self, engine, fn, reads=(), writes=(), key=None):
        return self._add(engine, fn, reads, writes, dma_key=key)

    def barrier(self):
        last = {k: len(st) - 1 for k, st in self.streams.items() if st}
        for e in ENGINES:
            for k, i in last.items():
                if self.pending[e].get(k, -1) < i:
                    self.pending[e][k] = i

    def emit(self, final_waits=()):
        nc = self.nc
        for e in ENGINES:
            waited = {}
            for op in self.ops[e]:
                nd = {}
                for k, i in op["deps"].items():
                    if waited.get(k, -1) >= i:
                        continue
                    waited[k] = i
                    nd[k] = i
                    self.streams[k][i]["marked"] = True
                op["deps"] = nd
        fin = {}
        for k, i in final_waits:
            if k[0] == "dma":
                i = len(self.streams[k]) - 1
            fin[k] = max(fin.get(k, -1), i)
        for k, st in self.streams.items():
            if st:
                fin[k] = len(st) - 1
        for k, i in fin.items():
            self.streams[k][i]["marked"] = True
        for k, st in self.streams.items():
            v = 0
            for op in st:
                if k[0] == "dma":
                    v += 16
                    op["inc"] = 16
                elif op["marked"]:
                    v += 1
                    op["inc"] = 1
                else:
                    op["inc"] = 0
                op["val"] = v
        with contextlib.ExitStack() as es:
            sems = {}
            for n, k in enumerate(self.streams):
                sems[k] = es.enter_context(nc.semaphore("s%d" % n))
            block = es.enter_context(nc.Block())

            def run(eng, ename):
                for op in self.ops[ename]:
                    for k, i in op["deps"].items():
                        eng.wait_ge(sems[k], self.streams[k][i]["val"])
                    ins = op["fn"](eng)
                    if op["inc"]:
                        ins.then_inc(sems[op["skey"]], op["inc"])
                if ename == "sync":
                    for k, i in fin.items():
                        eng.wait_ge(sems[k], self.streams[k][i]["val"])

            @block.sync
            def _(eng):
                run(eng, "sync")

            @block.scalar
            def _(eng):
                run(eng, "scalar")

            @block.vector
            def _(eng):
                run(eng, "vector")

            @block.gpsimd
            def _(eng):
                run(eng, "gpsimd")

            @block.tensor
            def _(eng):
                run(eng, "tensor")


SB_WORDS = 48400


def build_program(stop_after=None, dbg=()):
    nc = bass.Bass("TRN2", target_bir_lowering=False)

    def din(name, shape, dt=F32):
        return nc.dram_tensor(name, list(shape), dt, kind="ExternalInput").ap()

    xA = din("xA", [1024, 2048])
    xB = din("xB", [1024, 2048])
    c_in = din("c_in", [128, 16])
    pos_in2 = din("pos_in", [2, 128, 16], I32)
    cst = din("cst", [128, 3072])
    kbias2 = din("kbias", [2, 128, 1024])
    pcore2 = din("pcore", [2, 128, 80])
    w_ada_a = din("w_ada", [2, 2048, 12288])
    b_ada_a = din("b_ada", [2, 128, 12288])
    w_in_a = din("w_in", [2, 2048, 3144])
    w_out_a = din("w_out", [2, 2048, 2048])
    ssm_small_a = din("ssm_small", [2, 128, 48])
    ssm_bc_a = din("ssm_bc", [2, 128, 4 * 256])
    vecs_a = din("vecs", [2, 128, 12])
    w_glu_a = din("w_glu", [2, 512, 512])
    pool_w_a = din("pool_w", [2, 512, 128])
    lnp_a = din("lnp", [2, 128, 4 * 2048])
    w_router = din("w_router", [128, 256])
    e_gate_a = din("e_gate", [2, 16, 2048, 1024])
    e_up_a = din("e_up", [2, 16, 2048, 1024])
    e_down_a = din("e_down", [2, 16, 1024, 2048])
    xout_f = nc.dram_tensor("xout", [1024, 2048], F32, kind="ExternalOutput").ap()
    xmid = nc.dram_tensor("xmid_i", [1024, 2048], F32, kind="Internal").ap()
    S1 = nc.dram_tensor("s1_i", [1024, 2048], F32, kind="Internal").ap()
    S2 = nc.dram_tensor("s2_i", [1024, 2048], F32, kind="Internal").ap()
    dbg_out = {}

    es = contextlib.ExitStack()
    with es:
        SB = es.enter_context(nc.sbuf_tensor("SB", [128, SB_WORDS], F32))
        PS = [es.enter_context(nc.psum_tensor("ps%d" % i, [128, 512], F32)) for i in range(8)]
        PSK = ["ps%d" % i for i in range(8)]
        PSb = [t[:, :].bitcast(BF16) for t in PS]
        p = Prog(nc)
        top = [0]
        fin = []

        def A(n, dt=F32):
            w = n if dt != BF16 else (n + 1) // 2
            assert top[0] + w <= SB_WORDS, ("SBUF overflow", top[0], w)
            v = SB[:, top[0]:top[0] + w]
            top[0] += w
            return v if dt == F32 else v.bitcast(dt)

        def V(fn, r, w):
            return p.op("vector", fn, reads=r, writes=w)

        def S(fn, r, w):
            return p.op("scalar", fn, reads=r, writes=w)

        def mm(out, lhsT, rhs, start, stop, r, w):
            return p.op("tensor", lambda e: e.matmul(out, lhsT=lhsT, rhs=rhs, start=start, stop=stop), reads=r, writes=w)

        def tr(out, in_, ident, r, w):
            return p.op("tensor", lambda e: e.transpose(out=out, in_=in_, identity=ident), reads=r, writes=w)

        def dma(out, in_, r, w, key, eng="sync", slow=False):
            if slow:
                return p.dma(eng, lambda e: e.dma_start(out=out, in_=in_, allow_slow_non_contiguous=True), reads=r, writes=w, key=key)
            return p.dma(eng, lambda e: e.dma_start(out=out, in_=in_), reads=r, writes=w, key=key)

        def dma_w(out3, in3, w, key, nsplit=4):
            n = out3.shape[1]
            step = max(1, n // nsplit)
            tok = None
            for a in range(0, n, step):
                tok = dma(out3[:, a:a + step, :], in3[:, a:a + step, :], [], w, key, eng="gpsimd")
            return tok

        def dump(name, ap, r):
            if name in dbg_out:
                fin.append(dma(dbg_out[name], ap, r, [], "dbg_" + name))

        def act(out, in_, func, r, w, **kw):
            return S(lambda e: e.activation(out=out, in_=in_, func=func, **kw), r, w)

        def tt(out, in0, in1, op, r, w):
            return V(lambda e: e.tensor_tensor(out=out, in0=in0, in1=in1, op=op), r, w)

        def ts(out, in0, s1, s2, op0, op1, r, w):
            if op1 is None:
                return V(lambda e: e.tensor_scalar(out=out, in0=in0, scalar1=s1, scalar2=None, op0=op0), r, w)
            return V(lambda e: e.tensor_scalar(out=out, in0=in0, scalar1=s1, scalar2=s2, op0=op0, op1=op1), r, w)

        def stt(out, in0, scalar, in1, op0, op1, r, w):
            return V(lambda e: e.scalar_tensor_tensor(out=out, in0=in0, scalar=scalar, in1=in1, op0=op0, op1=op1), r, w)

        def cp(out, in_, r, w):
            return V(lambda e: e.tensor_copy(out=out, in_=in_), r, w)

        def G(fn, r, w):
            return p.op("gpsimd", fn, reads=r, writes=w)

        def gtt(out, in0, in1, op, r, w):
            return G(lambda e: e.tensor_tensor(out=out, in0=in0, in1=in1, op=op), r, w)

        def gstt(out, in0, scalar, in1, op0, op1, r, w):
            return G(lambda e: e.scalar_tensor_tensor(out=out, in0=in0, scalar=scalar, in1=in1, op0=op0, op1=op1), r, w)

        def vmax(out, in_, r, w):
            return V(lambda e: e.max(out=out, in_=in_), r, w)

        def vmr(out, rep_, vals, r, w):
            return V(lambda e: e.match_replace(out=out, in_to_replace=rep_, in_values=vals, imm_value=NEG), r, w)

        def red(out, in_, op, r, w):
            return V(lambda e: e.tensor_reduce(out=out, in_=in_, axis=AX.X, op=op), r, w)

        def recip(out, in_, r, w):
            return V(lambda e: e.reciprocal(out=out, in_=in_), r, w)

        def mset(out, val, w):
            return V(lambda e: e.memset(out, val), [], w)

        cst_sb = A(256 + 1024 + 32)
        ident_f = cst_sb[:, 0:128]
        iota_f = cst_sb[:, 128:256]
        ztri = cst_sb[:, 256:1280]
        invf = cst_sb[:, 1280:1304]
        dma(cst_sb[:, 0:256], cst[:, 0:256], [], ["cst"], "cst")
        dma(cst_sb[:, 256:1280], cst[:, 1024:2048], [], ["cst"], "cst")
        dma(cst_sb[:, 1280:1312], cst[:, 2048:2080], [], ["cst"], "cst")
        cstb = A(3 * 128, BF16)
        ident_b = cstb[:, 0:128]
        tri_b = cstb[:, 128:256]
        ones_b = cstb[:, 256:384]
        dma(ident_b, cst[:, 0:128], [], ["cstb"], "cstb", eng="gpsimd")
        dma(tri_b, cst[:, 256:384], [], ["cstb"], "cstb", eng="gpsimd")
        dma(ones_b, cst[:, 384:512], [], ["cstb"], "cstb", eng="gpsimd")
        pc_sb = A(80)
        pflag = pc_sb[:, 0:1]
        invcnt16 = pc_sb[:, 16:80].rearrange("p (g t) -> p g t", t=16)
        g1p = A(2048)
        g2p = A(2048)
        modp = A(64)
        modp3 = modp.rearrange("p (v k) -> p v k", k=16)
        gates = A(128)
        gates3 = gates.rearrange("p (t e) -> p t e", e=16)
        vec_sb = A(12)
        eps_t = A(1)
        V(lambda e: e.memset(eps_t, LN_EPS), [], ["eps"])
        perm_top = top[0]

        def run_pass(l, cfg, xpre, xown, xout, do_ada, prek, ownk, outk, noprefix=False):
            w_ada, b_ada, w_in, w_out = w_ada_a[l], b_ada_a[l], w_in_a[l], w_out_a[l]
            ssm_small, ssm_bc, vecs, w_glu, pool_w, lnp = ssm_small_a[l], ssm_bc_a[l], vecs_a[l], w_glu_a[l], pool_w_a[l], lnp_a[l]
            e_gate, e_up, e_down = e_gate_a[l], e_up_a[l], e_down_a[l]
            pos_in, kbias_in, pcore = pos_in2[cfg], kbias2[cfg], pcore2[cfg]
            dma(pc_sb, pcore[:, :], [], ["pc"], "pc")
            dma(vec_sb, vecs[:, :], [], ["vec"], "vec")
            if do_ada:
                phase_A(w_ada, b_ada)
            phase_rest(w_in, w_out, ssm_small, ssm_bc, w_glu, pool_w, lnp, e_gate, e_up, e_down, pos_in, kbias_in, xpre, xown, xout, prek, ownk, outk, noprefix)

        def phase_A(w_ada, b_ada):
            top[0] = perm_top
            c_sb = A(16)
            cond = A(16)
            condrep = A(16 * 128, BF16)
            condrep3 = condrep.rearrange("p (k j) -> p k j", j=128)
            ada = A(12288)
            tmpA = A(2048)
            tmpA3 = tmpA.rearrange("p (k j) -> p k j", j=128)
            wbA = [A(16 * 512, BF16) for _ in range(2)]
            dma(c_sb, c_in[:, :], [], ["c_sb"], "c_sb")
            act(cond, c_sb, AF.Silu, ["c_sb"], ["cond"])
            cp(condrep3, cond.unsqueeze(2).to_broadcast([128, 16, 128]), ["cond"], ["condrep"])
            dma(ada, b_ada[:, :], [], ["ada"], "ada")
            w_ada_r = w_ada.rearrange("(k p) n -> p k n", p=128)
            for nb in range(24):
                wv = wbA[nb % 2].rearrange("p (k n) -> p k n", n=512)
                dma_w(wv, w_ada_r[:, :, nb * 512:(nb + 1) * 512], ["wbA%d" % (nb % 2)], "wbA%d" % (nb % 2))
                bank = PS[nb % 2]
                for kc in range(16):
                    mm(bank[:, :], condrep3[:, kc, :], wv[:, kc, :], kc == 0, kc == 15, ["condrep", "wbA%d" % (nb % 2)], [PSK[nb % 2]])
                tt(ada[:, nb * 512:(nb + 1) * 512], bank[:, :], ada[:, nb * 512:(nb + 1) * 512], ALU.add, [PSK[nb % 2], "ada"], ["ada"])
            ts(g1p, ada[:, 2 * 2048:3 * 2048], 1.0, None, ALU.add, None, ["ada"], ["g1p"])
            ts(g2p, ada[:, 5 * 2048:6 * 2048], 1.0, None, ALU.add, None, ["ada"], ["g2p"])
            for slot, idx in enumerate((0, 1, 3, 4)):
                tt(tmpA3, ada[:, idx * 2048:(idx + 1) * 2048].rearrange("p (k j) -> p k j", j=128),
                   ident_f.unsqueeze(1).to_broadcast([128, 16, 128]), ALU.mult, ["ada", "cst"], ["tmpA"])
                V(lambda e, slot=slot: e.tensor_reduce(out=modp3[:, slot, :], in_=tmpA3, axis=AX.X, op=ALU.add), ["tmpA"], ["modp"])
            ts(modp3[:, 1, :], modp3[:, 1, :], 1.0, None, ALU.add, None, ["modp"], ["modp"])
            ts(modp3[:, 3, :], modp3[:, 3, :], 1.0, None, ALU.add, None, ["modp"], ["modp"])
            dump("ada", ada, ["ada"])
            dump("modp", modp, ["modp"])
            p.barrier()
            if stop_after == "A":
                p.emit(fin)
                return nc


        def phase_rest(w_in, w_out, ssm_small, ssm_bc, w_glu, pool_w, lnp, e_gate, e_up, e_down, pos_in, kbias_in, xpre, xown, xout, prek, ownk, outk, noprefix):
            nonlocal xts
            top[0] = perm_top
            mixT = A(16 * 1024, BF16)
            mixT3 = mixT.rearrange("p (k t) -> p k t", t=1024)
            uT3 = mixT3
            mix_top = top[0]
            usT = A(4 * 2048, BF16)
            usT3 = usT.rearrange("p (c t) -> p c t", t=2048)
            upT = A(4 * 1152, BF16)
            upT3 = upT.rearrange("p (g t) -> p g t", t=1152)
            l1_top = top[0]
            qT = A(8 * 1024, BF16)
            qT3 = qT.rearrange("p (h t) -> p h t", t=1024)
            kT = A(2 * 2048, BF16)
            kT3 = kT.rearrange("p (h t) -> p h t", t=2048)
            v_sb = A(16 * 256, BF16)
            v_sb4 = v_sb.rearrange("p (b h d) -> p b h d", h=2, d=128)
            ikT2 = A(2048, BF16)
            iqT = A(4 * 1024, BF16)
            iqT3 = iqT.rearrange("p (j t) -> p j t", t=1024)
            iw_sb = A(64)
            iw3 = iw_sb.rearrange("p (t h) -> p t h", h=8)
            cosT = A(16 * 24)
            sinT = A(16 * 24)
            cos3 = cosT.rearrange("p (t f) -> p t f", f=24)
            sin3 = sinT.rearrange("p (t f) -> p t f", f=24)
            att_top = top[0]
            wb = [A(16 * 512, BF16) for _ in range(2)]
            xts = [A(2048) for _ in range(2)]
            qt = A(512)
            kt = A(256)
            rtmp = A(4 * 64)
            pos_i = A(16, I32)
            pos_f = A(16)
            ang = A(16 * 24)
            ang3 = ang.rearrange("p (t f) -> p t f", f=24)
            sc_kf = A(16 * 24)
            sc_ki = A(16 * 24, I32)
            sc_t = A(16 * 24)

            def sin_of(out, in_, addc, kf, ki, t, rk, wk, kfk, kik, tk):
                ts(t, in_, addc, None, ALU.add, None, rk, [tk])
                ts(kf, t, 1.0 / (2 * PI), None, ALU.mult, None, [tk], [kfk])
                cp(ki, kf, [kfk], [kik])
                cp(kf, ki, [kik], [kfk])
                stt(t, kf, -2 * PI, t, ALU.mult, ALU.add, [kfk, tk], [tk])
                ts(kf, t, PI, -2 * PI, ALU.is_gt, ALU.mult, [tk], [kfk])
                tt(t, t, kf, ALU.add, [tk, kfk], [tk])
                ts(kf, t, -PI, 2 * PI, ALU.is_lt, ALU.mult, [tk], [kfk])
                tt(t, t, kf, ALU.add, [tk, kfk], [tk])
                act(out, t, AF.Sin, [tk], [wk])

            dma(pos_i, pos_in[:, :], [], ["pos_i"], "pos_i")
            cp(pos_f, pos_i, ["pos_i"], ["pos_f"])
            tt(ang3, pos_f.unsqueeze(2).to_broadcast([128, 16, 24]), invf.unsqueeze(1).to_broadcast([128, 16, 24]), ALU.mult, ["pos_f", "cst"], ["ang"])
            sin_of(sinT, ang, 0.0, sc_kf, sc_ki, sc_t, ["ang"], "sinT", "sc_kf", "sc_ki", "sc_t")
            sin_of(cosT, ang, PI / 2, sc_kf, sc_ki, sc_t, ["ang"], "cosT", "sc_kf", "sc_ki", "sc_t")
            dump("cosT", cosT, ["cosT"])

            w_in_r = w_in.rearrange("(k p) n -> p k n", p=128)
            wcnt = [0]

            def load_w(col0, ncols):
                i = wcnt[0] % 2
                wcnt[0] += 1
                wv = wb[i].rearrange("p (k n) -> p k n", n=512)
                dma_w(wv[:, :, 0:ncols], w_in_r[:, :, col0:col0 + ncols], ["wb%d" % i], "wb%d" % i)
                return wv, "wb%d" % i

            def rope(x3, nh, half, tile, foff, rk):
                cs = cos3[:, tile, foff:foff + half].unsqueeze(1).to_broadcast([128, nh, half])
                sn = sin3[:, tile, foff:foff + half].unsqueeze(1).to_broadcast([128, nh, half])
                x1 = x3[:, :, 0:half]
                x2 = x3[:, :, half:2 * half]
                t = [rtmp[:, j * 64:j * 64 + nh * half].rearrange("p (h f) -> p h f", f=half) for j in range(4)]
                tt(t[0], x1, cs, ALU.mult, [rk, "cosT"], ["rt0"])
                tt(t[1], x2, sn, ALU.mult, [rk, "sinT"], ["rt1"])
                tt(t[2], x2, cs, ALU.mult, [rk, "cosT"], ["rt2"])
                tt(t[3], x1, sn, ALU.mult, [rk, "sinT"], ["rt3"])
                tt(x1, t[0], t[1], ALU.subtract, ["rt0", "rt1"], [rk])
                tt(x2, t[2], t[3], ALU.add, ["rt2", "rt3"], [rk])

            def make_uT(xsrc, slot_sh, slot_sc, srck=()):
                for t in range(8):
                    xt = xts[t % 2]
                    xk = "xt%d" % (t % 2)
                    dma(xt, xsrc[t * 128:(t + 1) * 128, :], list(srck), [xk], xk)
                    for g in range(4):
                        b = 2 + (g % 2)
                        for j in range(4):
                            kc = g * 4 + j
                            tr(PS[b][:, j * 128:(j + 1) * 128], xt[:, kc * 128:(kc + 1) * 128], ident_f, [xk, "cst"], [PSK[b]])
                        for j in range(4):
                            kc = g * 4 + j
                            o = uT3[:, kc, t * 128:(t + 1) * 128]
                            i_ = PS[b][:, j * 128:(j + 1) * 128]
                            if j % 2 == 0:
                                act(o, i_, AF.Identity, [PSK[b], "modp"], ["uT"], scale=modp3[:, slot_sc, kc:kc + 1], bias=modp3[:, slot_sh, kc:kc + 1])
                            else:
                                ts(o, i_, modp3[:, slot_sc, kc:kc + 1], modp3[:, slot_sh, kc:kc + 1], ALU.mult, ALU.add, [PSK[b], "modp"], ["uT"])

            def proj_tok(col0, ncols, consume):
                wv, wk = load_w(col0, ncols)
                for t in range(8):
                    b = t % 2
                    for kc in range(16):
                        mm(PS[b][:, 0:ncols], uT3[:, kc, t * 128:(t + 1) * 128], wv[:, kc, 0:ncols], kc == 0, kc == 15, ["uT", wk], [PSK[b]])
                    consume(t, PS[b], PSK[b])

            def proj_T(col0, nchunks, consume, blocks):
                wv, wk = load_w(col0, nchunks * 128)
                n = 0
                for cc in range(nchunks):
                    for (t0, tn) in blocks:
                        b = n % 2
                        n += 1
                        for kc in range(16):
                            mm(PS[b][:, 0:tn], wv[:, kc, cc * 128:(cc + 1) * 128], uT3[:, kc, t0:t0 + tn], kc == 0, kc == 15, ["uT", wk], [PSK[b]])
                        consume(cc, t0, tn, PS[b], PSK[b])

            def kv_consumer(tile_base):
                def f(t, bank, bk):
                    gt = tile_base + t
                    act(kt, bank[:, 0:256], AF.Copy, [bk], ["kt"])
                    act(v_sb4[:, gt, :, :], bank[:, 256:512].rearrange("p (h d) -> p h d", d=128), AF.Copy, [bk], ["v_sb"])
                    rope(kt.rearrange("p (h d) -> p h d", d=128), 2, 16, gt, 0, "kt")
                    for h in range(2):
                        tr(PS[4][:, h * 128:(h + 1) * 128], kt[:, h * 128:(h + 1) * 128], ident_f, ["kt", "cst"], [PSK[4]])
                    cp(kT3[:, :, gt * 128:(gt + 1) * 128], PS[4][:, 0:256].rearrange("p (h t) -> p h t", t=128), [PSK[4]], ["kT"])
                return f

            def ik_consumer(tile_base, own):
                def f(t, bank, bk):
                    gt = tile_base + t
                    act(kt[:, 0:64], bank[:, 0:64], AF.Copy, [bk], ["kt"])
                    if own:
                        act(iw3[:, t, :], bank[:, 64:72], AF.Copy, [bk], ["iw"])
                    rope(kt[:, 0:64].rearrange("p (h d) -> p h d", d=64), 1, 8, gt, 16, "kt")
                    cp(kt[:, 64:128], kt[:, 0:64], ["kt"], ["kt"])
                    tr(PS[5][:, 0:128], kt[:, 0:128], ident_f, ["kt", "cst"], [PSK[5]])
                    cp(ikT2[:, gt * 128:(gt + 1) * 128], PS[5][:, 0:128], [PSK[5]], ["ikT2"])
                return f

            def us_consumer(tok_base, prefix):
                def f(cc, t0, tn, bank, bk):
                    o = usT3[:, cc, tok_base + t0:tok_base + t0 + tn]
                    if prefix:
                        ts(o, bank[:, 0:tn], pflag, None, ALU.mult, None, [bk, "pc"], ["usT"])
                    else:
                        act(o, bank[:, 0:tn], AF.Copy, [bk], ["usT"])
                return f

            def up_consumer(prefix):
                def f(cc, t0, tn, bank, bk):
                    if prefix:
                        ts(upT3[:, cc, 0:128], bank[:, 0:tn], pflag, None, ALU.mult, None, [bk, "pc"], ["upT"])
                    else:
                        act(upT3[:, cc, 128 + t0:128 + t0 + tn], bank[:, 0:tn], AF.Copy, [bk], ["upT"])
                return f

            def q_consumer(g):
                def f(t, bank, bk):
                    act(qt, bank[:, :], AF.Copy, [bk], ["qt"])
                    rope(qt.rearrange("p (h d) -> p h d", d=128), 4, 16, 8 + t, 0, "qt")
                    for h in range(4):
                        tr(PS[6][:, h * 128:(h + 1) * 128], qt[:, h * 128:(h + 1) * 128], ident_f, ["qt", "cst"], [PSK[6]])
                    cp(qT3[:, 4 * g:4 * g + 4, t * 128:(t + 1) * 128], PS[6][:, :].rearrange("p (h t) -> p h t", t=128), [PSK[6]], ["qT"])
                return f

            def iq_consumer(t, bank, bk):
                act(qt, bank[:, :], AF.Copy, [bk], ["qt"])
                rope(qt.rearrange("p (h d) -> p h d", d=64), 8, 8, 8 + t, 16, "qt")
                for j in range(4):
                    tr(PS[7][:, j * 128:(j + 1) * 128], qt[:, j * 128:(j + 1) * 128], ident_f, ["qt", "cst"], [PSK[7]])
                cp(iqT3[:, :, t * 128:(t + 1) * 128], PS[7][:, :].rearrange("p (j t) -> p j t", t=128), [PSK[7]], ["iqT"])

            if not noprefix:
                make_uT(xpre, 0, 1, srck=[prek])
                proj_tok(OK_, 512, kv_consumer(0))
                proj_tok(OIK, 72, ik_consumer(0, False))
                proj_T(OUS, 4, us_consumer(0, True), [(0, 512), (512, 512)])
                proj_T(OUP, 4, up_consumer(True), [(896, 128)])
            else:
                mset(upT3[:, :, 0:128], 0.0, ["upT"])
            make_uT(xown, 0, 1, srck=[ownk])
            dump("uT", None, None) if False else None
            proj_tok(OQ, 512, q_consumer(0))
            proj_tok(OQ + 512, 512, q_consumer(1))
            proj_tok(OK_, 512, kv_consumer(8))
            proj_tok(OIQ, 512, iq_consumer)
            proj_tok(OIK, 72, ik_consumer(8, True))
            proj_T(OUS, 4, us_consumer(1024, False), [(0, 512), (512, 512)])
            proj_T(OUP, 4, up_consumer(False), [(0, 512), (512, 512)])
            if "qT" in dbg_out:
                for nm, ap_, k_ in (("qT", qT, "qT"), ("kT", kT, "kT"), ("v_sb", v_sb, "v_sb"), ("ikT2", ikT2, "ikT2"),
                                    ("iqT", iqT, "iqT"), ("usT", usT, "usT"), ("upT", upT, "upT")):
                    fin.append(dma(dbg_out[nm], ap_, [k_], [], "dbg_" + nm, eng="gpsimd"))
                dump("iw", iw_sb, ["iw"])
            p.barrier()
            if stop_after == "BC":
                p.emit(fin)
                return nc

            top[0] = att_top
            accs = [A(2048), A(2048)]
            work = A(2048)
            rl = [A(512) for _ in range(2)]
            m8 = A(256)
            thr = A(1)
            m01 = A(2048, BF16)
            m01Ts = [A(16 * 128, BF16).rearrange("p (k q) -> p k q", q=128) for _ in range(2)]
            osb = A(512)
            dsb = A(512)
            mb_c = A(2)
            mset(mb_c[:, 0:1], 30000.0, ["mb_c"])
            mset(mb_c[:, 1:2], -30000.0, ["mb_c"])
            PTs = [A(512, BF16) for _ in range(2)]
            rden = A(512)
            kb_sb = A(1024)
            dma(kb_sb, kbias_in[:, :], [], ["kbias"], "kbias")
            SCALE = 128 ** -0.5
            k0 = 1024 if noprefix else 0
            kb0 = k0 // 128
            def geom(i):
                nk = 1024 + (i + 1) * 128 - k0
                return nk, nk // 128, (nk + 511) // 512

            cntr = [0]

            def indexer(i):
                nk, nkb, n5 = geom(i)
                acc, acck = accs[i % 2], "acc%d" % (i % 2)
                for h in range(8):
                    pr = (h % 2) * 64
                    for b5 in range(n5):
                        c0 = b5 * 512
                        w = min(512, nk - c0)
                        b = cntr[0] % 2
                        cntr[0] += 1
                        mm(PS[b][:, 0:w], iqT3[pr:pr + 64, h // 2, i * 128:(i + 1) * 128], ikT2[pr:pr + 64, k0 + c0:k0 + c0 + w], True, True, ["iqT", "ikT2"], [PSK[b]])
                        act(rl[b][:, 0:w], PS[b][:, 0:w], AF.Relu, [PSK[b]], ["rl%d" % b])
                        if h == 0:
                            if k0 + c0 < 1024:
                                in1 = kb_sb[:, c0:c0 + w]
                            else:
                                o0 = 896 - i * 128 + (k0 + c0 - 1024)
                                in1 = ztri[:, o0:o0 + w]
                            stt(acc[:, c0:c0 + w], rl[b][:, 0:w], iw3[:, i, h:h + 1], in1, ALU.mult, ALU.add, ["rl%d" % b, "iw", "kbias", "cst"], [acck])
                        else:
                            stt(acc[:, c0:c0 + w], rl[b][:, 0:w], iw3[:, i, h:h + 1], acc[:, c0:c0 + w], ALU.mult, ALU.add, ["rl%d" % b, "iw", acck], [acck])

            def topk(i):
                nk, nkb, n5 = geom(i)
                acc, acck = accs[i % 2], "acc%d" % (i % 2)
                if nk > 256:
                    cur, ck = acc, acck
                    for r in range(32):
                        vmax(m8[:, r * 8:(r + 1) * 8], cur[:, 0:nk], [ck], ["m8"])
                        if r < 31:
                            vmr(work[:, 0:nk], m8[:, r * 8:(r + 1) * 8], cur[:, 0:nk], [ck, "m8"], ["work"])
                            cur, ck = work, "work"
                    ts(thr, m8[:, 255:256], -1e29, None, ALU.max, None, ["m8"], ["thr"])
                    ts(m01[:, 0:nk], acc[:, 0:nk], thr[:, 0:1], None, ALU.is_ge, None, [acck, "thr"], ["m01"])
                else:
                    ts(m01[:, 0:nk], acc[:, 0:nk], -1e29, None, ALU.is_ge, None, [acck], ["m01"])

            def attn(i):
                nk, nkb, n5 = geom(i)
                m01T3, m01Tk = m01Ts[i % 2], "m01T%d" % (i % 2)
                for g0 in range(0, nkb, 8):
                    gn = min(8, nkb - g0)
                    b = 2 + (g0 // 8) % 2
                    for j in range(gn):
                        kb = g0 + j
                        tr(PSb[b][:, j * 128:(j + 1) * 128], m01[:, kb * 128:(kb + 1) * 128], ident_b, ["m01", "cstb"], [PSK[b]])
                    act(m01T3[:, g0:g0 + gn, :], PSb[b][:, 0:gn * 128].rearrange("p (k q) -> p k q", q=128), AF.Identity, [PSK[b], "mb_c"], [m01Tk],
                        scale=mb_c[:, 0:1], bias=mb_c[:, 1:2])
                for kvh in range(2):
                    for kb in range(nkb):
                        gkb = kb0 + kb
                        sb_ = 4 + kb % 2
                        pk = "PT%d" % (kb % 2)
                        PT = PTs[kb % 2]
                        PT3 = PT.rearrange("p (h q) -> p h q", q=128)
                        mm(PS[sb_][:, :], kT3[:, kvh, gkb * 128:(gkb + 1) * 128], qT3[:, 4 * kvh:4 * kvh + 4, i * 128:(i + 1) * 128], True, False, ["kT", "qT"], [PSK[sb_]])
                        for hh in range(4):
                            mm(PS[sb_][:, hh * 128:(hh + 1) * 128], ident_b, m01T3[:, kb, :], False, True, ["cstb", m01Tk], [PSK[sb_]])
                        act(PT, PS[sb_][:, :], AF.Exp, [PSK[sb_]], [pk], scale=SCALE)
                        mm(PS[6][:, :], v_sb4[:, gkb, kvh, :], PT, kb == 0, kb == nkb - 1, ["v_sb", pk], [PSK[6]])
                        mm(PS[7][:, :], ones_b, PT, kb == 0, kb == nkb - 1, ["cstb", pk], [PSK[7]])
                    act(osb, PS[6][:, :], AF.Copy, [PSK[6]], ["osb"])
                    act(dsb, PS[7][:, :], AF.Ln, [PSK[7]], ["dsb"])
                    act(dsb, dsb, AF.Exp, ["dsb"], ["dsb"], scale=-1.0)
                    gtt(mixT3[:, 4 * kvh:4 * kvh + 4, i * 128:(i + 1) * 128], osb.rearrange("p (h q) -> p h q", q=128),
                        dsb.rearrange("p (h q) -> p h q", q=128), ALU.mult, ["osb", "dsb"], ["mixT"])

            indexer(0)
            for i in range(8):
                if i + 1 < 8:
                    indexer(i + 1)
                topk(i)
                attn(i)
            p.barrier()
            if stop_after == "EF":
                if "mixT" in dbg_out:
                    fin.append(dma(dbg_out["mixT"], mixT, ["mixT"], [], "dbg_mixT", eng="gpsimd"))
                p.emit(fin)
                return nc

            top[0] = l1_top
            sm = A(48)
            dma(sm, ssm_small[:, :], [], ["sm"], "sm")
            lam_re, lam_im, lstep = sm[:, 0:16], sm[:, 16:32], sm[:, 32:48]
            PTre = A(2048)
            PTim = A(2048)
            PinvRe = A(2048)
            PinvIm = A(2048)
            W_B = [A(4 * 512, BF16) for _ in range(2)]
            Wc = [A(16 * 128, BF16) for _ in range(2)]
            wglu_sb = A(4 * 512, BF16)
            sv = A(16 * 24)
            ki16 = A(16, I32)
            ssm_tmp = top[0]
            bc_sb = A(1024)
            dma(bc_sb, ssm_bc[:, :], [], ["bc_sb"], "bc_sb")
            b_re3 = bc_sb[:, 0:256].rearrange("p (s c) -> p s c", c=16)
            b_im3 = bc_sb[:, 256:512].rearrange("p (s c) -> p s c", c=16)
            c_re3 = bc_sb[:, 512:768].rearrange("p (s c) -> p s c", c=16)
            c_im3 = bc_sb[:, 768:1024].rearrange("p (s c) -> p s c", c=16)
            svn = [0]

            def SV():
                v = sv[:, svn[0] * 16:(svn[0] + 1) * 16]
                svn[0] += 1
                return v
            dt_, ar, th = SV(), SV(), SV()
            act(dt_, lstep, AF.Exp, ["sm"], ["dt"])
            tt(ar, lam_re, dt_, ALU.mult, ["sm", "dt"], ["ar"])
            tt(th, lam_im, dt_, ALU.mult, ["sm", "dt"], ["th"])
            PIre = A(2048)
            PIim = A(2048)
            big1 = A(2048)
            big2 = A(2048)
            big3 = A(2048)
            bigi = A(2048, I32)
            B3 = lambda v: v.rearrange("p (s t) -> p s t", t=128)
            iota_b = iota_f.unsqueeze(1).to_broadcast([128, 16, 128])
            tt(B3(big1), th.unsqueeze(2).to_broadcast([128, 16, 128]), iota_b, ALU.mult, ["th", "cst"], ["big1"])
            sin_of(PIim, big1, 0.0, big2, bigi, big3, ["big1"], "sinS", "big2", "bigi", "big3")
            sin_of(PIre, big1, PI / 2, big2, bigi, big3, ["big1"], "cosS", "big2", "bigi", "big3")
            tt(B3(big1), ar.unsqueeze(2).to_broadcast([128, 16, 128]), iota_b, ALU.mult, ["ar", "cst"], ["big1"])
            act(big2, big1, AF.Exp, ["big1"], ["big2"])
            act(big3, big1, AF.Exp, ["big1"], ["big3"], scale=-1.0)
            tt(PTre, big2, PIre, ALU.mult, ["big2", "cosS"], ["PTre"])
            tt(PTim, big2, PIim, ALU.mult, ["big2", "sinS"], ["PTim"])
            tt(PIre, big3, PIre, ALU.mult, ["big3", "cosS"], ["cosS"])
            stt(PIim, big3, -1.0, PIim, ALU.mult, ALU.mult, ["big3", "sinS"], ["sinS"])
            for comp, (src, sk, dst, dk) in enumerate(((PIre, "cosS", PinvRe, "PinvRe"), (PIim, "sinS", PinvIm, "PinvIm"))):
                for g in range(4):
                    b = 2 * comp + (g % 2)
                    for j in range(4):
                        sb_i = g * 4 + j
                        tr(PS[b][:, j * 128:(j + 1) * 128], src[:, sb_i * 128:(sb_i + 1) * 128], ident_f, [sk, "cst"], [PSK[b]])
                    cp(dst[:, g * 512:(g + 1) * 512], PS[b][:, :], [PSK[b]], [dk])
            L_re, L_im = SV(), SV()
            a128, k1, t1_, mg = SV(), SV(), SV(), SV()
            ts(a128, th, 128.0, None, ALU.mult, None, ["th"], ["a128"])
            sin_of(L_im, a128, 0.0, k1, ki16, t1_, ["a128"], "Lsin", "k1", "ki16", "t1_")
            sin_of(L_re, a128, PI / 2, k1, ki16, t1_, ["a128"], "Lcos", "k1", "ki16", "t1_")
            act(mg, ar, AF.Exp, ["ar"], ["mg"], scale=128.0)
            tt(L_re, L_re, mg, ALU.mult, ["Lcos", "mg"], ["Lcos"])
            tt(L_im, L_im, mg, ALU.mult, ["Lsin", "mg"], ["Lsin"])
            PTre3, PTim3 = B3(PTre), B3(PTim)
            a_, b_, den_, m_re, m_im, u1, u2 = SV(), SV(), SV(), SV(), SV(), SV(), SV()
            ts(a_, PTre3[:, :, 1], -1.0, None, ALU.add, None, ["PTre"], ["a_"])
            cp(b_, PTim3[:, :, 1], ["PTim"], ["b_"])
            tt(u1, lam_re, lam_re, ALU.mult, ["sm"], ["u1"])
            tt(u2, lam_im, lam_im, ALU.mult, ["sm"], ["u2"])
            tt(den_, u1, u2, ALU.add, ["u1", "u2"], ["den_"])
            V(lambda e: e.reciprocal(out=den_, in_=den_), ["den_"], ["den_"])
            tt(u1, a_, lam_re, ALU.mult, ["a_", "sm"], ["u1"])
            tt(u2, b_, lam_im, ALU.mult, ["b_", "sm"], ["u2"])
            tt(m_re, u1, u2, ALU.add, ["u1", "u2"], ["m_re"])
            tt(m_re, m_re, den_, ALU.mult, ["m_re", "den_"], ["m_re"])
            tt(u1, b_, lam_re, ALU.mult, ["b_", "sm"], ["u1"])
            tt(u2, a_, lam_im, ALU.mult, ["a_", "sm"], ["u2"])
            tt(m_im, u1, u2, ALU.subtract, ["u1", "u2"], ["m_im"])
            tt(m_im, m_im, den_, ALU.mult, ["m_im", "den_"], ["m_im"])
            p.barrier()
            bigf = bigi.bitcast(F32)
            Bb_re, Bb_im, bt1, bt2 = bigf[:, 0:256], bigf[:, 256:512], bigf[:, 512:768], bigf[:, 768:1024]
            S3 = lambda v: v.rearrange("p (s c) -> p s c", c=16)
            mre_b = m_re.unsqueeze(2).to_broadcast([128, 16, 16])
            mim_b = m_im.unsqueeze(2).to_broadcast([128, 16, 16])
            tt(S3(bt1), b_re3, mre_b, ALU.mult, ["bc_sb", "m_re"], ["bt1"])
            tt(S3(bt2), b_im3, mim_b, ALU.mult, ["bc_sb", "m_im"], ["bt2"])
            tt(Bb_re, bt1, bt2, ALU.subtract, ["bt1", "bt2"], ["Bb_re"])
            tt(S3(bt1), b_im3, mre_b, ALU.mult, ["bc_sb", "m_re"], ["bt1"])
            tt(S3(bt2), b_re3, mim_b, ALU.mult, ["bc_sb", "m_im"], ["bt2"])
            tt(Bb_im, bt1, bt2, ALU.add, ["bt1", "bt2"], ["Bb_im"])
            for comp, (srcB, kB, srcC, cneg) in enumerate(((Bb_re, "Bb_re", c_re3, 1.0), (Bb_im, "Bb_im", c_im3, -1.0))):
                wide = big1 if comp == 0 else big2
                wk = "big1" if comp == 0 else "big2"
                V(lambda e, wide=wide: e.memset(wide, 0.0), [], [wk])
                w4 = wide.rearrange("p (a j c) -> p a j c", j=4, c=128)
                s4 = srcB.rearrange("p (a j c) -> p a j c", j=4, c=16)
                for j in range(4):
                    for gl in range(2):
                        cp(w4[gl * 64:(gl + 1) * 64, :, j, j * 32 + gl * 16:j * 32 + gl * 16 + 16], s4[gl * 64:(gl + 1) * 64, :, j, :], [kB], [wk])
                for g in range(4):
                    b = 4 + (g % 2)
                    for j in range(4):
                        sb_i = g * 4 + j
                        tr(PS[b][:, j * 128:(j + 1) * 128], wide[:, sb_i * 128:(sb_i + 1) * 128], ident_f, [wk, "cst"], [PSK[b]])
                    cp(W_B[comp][:, g * 512:(g + 1) * 512], PS[b][:, :], [PSK[b]], ["W_B%d" % comp])
                wide2 = big3 if comp == 0 else PIre
                wk2 = "big3" if comp == 0 else "cosS"
                V(lambda e, wide2=wide2: e.memset(wide2, 0.0), [], [wk2])
                w4 = wide2.rearrange("p (a j c) -> p a j c", j=4, c=128)
                s4 = srcC.rearrange("p (a j) c -> p a j c", j=4)
                for j in range(4):
                    for gl in range(2):
                        ts(w4[gl * 64:(gl + 1) * 64, :, j, j * 32 + gl * 16:j * 32 + gl * 16 + 16], s4[gl * 64:(gl + 1) * 64, :, j, :], cneg, None, ALU.mult, None, ["bc_sb"], [wk2])
                cp(Wc[comp], wide2, [wk2], ["Wc%d" % comp])
            p.barrier()
            top[0] = ssm_tmp
            wglu3 = wglu_sb.rearrange("p (k n) -> p k n", n=512)
            dma(wglu3, w_glu.rearrange("(k p) n -> p k n", p=128), [], ["wglu"], "wglu", eng="gpsimd")
            Xre = [A(2048, BF16) for _ in range(2)]
            Xim = [A(2048, BF16) for _ in range(2)]
            sTre = A(2048, BF16)
            sTim = A(2048, BF16)
            yg = A(4 * 1024, BF16)
            yg3 = yg.rearrange("p (c t) -> p c t", t=1024)
            tq = [A(512) for _ in range(4)]
            Are, Aim = A(512), A(512)
            car_re, car_im, al_re, al_im, cu1, cu2 = SV(), SV(), SV(), SV(), SV(), SV()
            yv = A(512)
            V(lambda e: e.memset(car_re, 0.0), [], ["car_re"])
            V(lambda e: e.memset(car_im, 0.0), [], ["car_im"])
            for j in range(8 if noprefix else 0, 16):
                own = j >= 8
                xr, xi = Xre[j % 2], Xim[j % 2]
                xrk, xik = "Xre%d" % (j % 2), "Xim%d" % (j % 2)
                for cc in range(4):
                    sl = slice(cc * 512, (cc + 1) * 512)
                    mm(PS[0][:, :], usT3[:, cc, j * 128:(j + 1) * 128], W_B[0][:, sl], True, True, ["usT", "W_B0"], [PSK[0]])
                    mm(PS[1][:, :], usT3[:, cc, j * 128:(j + 1) * 128], W_B[1][:, sl], True, True, ["usT", "W_B1"], [PSK[1]])
                    tt(tq[0], PS[0][:, :], PinvRe[:, sl], ALU.mult, [PSK[0], "PinvRe"], ["tq0"])
                    tt(tq[1], PS[1][:, :], PinvIm[:, sl], ALU.mult, [PSK[1], "PinvIm"], ["tq1"])
                    tt(tq[2], PS[1][:, :], PinvRe[:, sl], ALU.mult, [PSK[1], "PinvRe"], ["tq2"])
                    tt(tq[3], PS[0][:, :], PinvIm[:, sl], ALU.mult, [PSK[0], "PinvIm"], ["tq3"])
                    tt(xr[:, sl], tq[0], tq[1], ALU.subtract, ["tq0", "tq1"], [xrk])
                    tt(xi[:, sl], tq[2], tq[3], ALU.add, ["tq2", "tq3"], [xik])
                if not own:
                    for sb_i in range(16):
                        mm(PS[2][:, sb_i:sb_i + 1], xr[:, sb_i * 128:(sb_i + 1) * 128], ones_b[:, 0:1], True, True, [xrk, "cstb"], [PSK[2]])
                        mm(PS[3][:, sb_i:sb_i + 1], xi[:, sb_i * 128:(sb_i + 1) * 128], ones_b[:, 0:1], True, True, [xik, "cstb"], [PSK[3]])
                    tt(al_re, PS[2][:, 0:16], car_re, ALU.add, [PSK[2], "car_re"], ["al_re"])
                    tt(al_im, PS[3][:, 0:16], car_im, ALU.add, [PSK[3], "car_im"], ["al_im"])
                else:
                    tl = j - 8
                    for g in range(4):
                        for jj in range(4):
                            sb_i = g * 4 + jj
                            mm(PS[2][:, jj * 128:(jj + 1) * 128], xr[:, sb_i * 128:(sb_i + 1) * 128], tri_b, True, True, [xrk, "cstb"], [PSK[2]])
                            mm(PS[3][:, jj * 128:(jj + 1) * 128], xi[:, sb_i * 128:(sb_i + 1) * 128], tri_b, True, True, [xik, "cstb"], [PSK[3]])
                        gs = slice(g * 512, (g + 1) * 512)
                        A3 = lambda v: v.rearrange("p (s t) -> p s t", t=128)
                        tt(A3(Are), A3(PS[2][:, :]), car_re[:, g * 4:(g + 1) * 4].unsqueeze(2).to_broadcast([128, 4, 128]), ALU.add, [PSK[2], "car_re"], ["Are"])
                        tt(A3(Aim), A3(PS[3][:, :]), car_im[:, g * 4:(g + 1) * 4].unsqueeze(2).to_broadcast([128, 4, 128]), ALU.add, [PSK[3], "car_im"], ["Aim"])
                        cp(al_re[:, g * 4:(g + 1) * 4], A3(Are)[:, :, 127], ["Are"], ["al_re"])
                        cp(al_im[:, g * 4:(g + 1) * 4], A3(Aim)[:, :, 127], ["Aim"], ["al_im"])
                        tt(tq[0], PTre[:, gs], Are, ALU.mult, ["PTre", "Are"], ["tq0"])
                        tt(tq[1], PTim[:, gs], Aim, ALU.mult, ["PTim", "Aim"], ["tq1"])
                        tt(tq[2], PTre[:, gs], Aim, ALU.mult, ["PTre", "Aim"], ["tq2"])
                        tt(tq[3], PTim[:, gs], Are, ALU.mult, ["PTim", "Are"], ["tq3"])
                        tt(sTre[:, gs], tq[0], tq[1], ALU.subtract, ["tq0", "tq1"], ["sTre"])
                        tt(sTim[:, gs], tq[2], tq[3], ALU.add, ["tq2", "tq3"], ["sTim"])
                    for cc in range(4):
                        for jj in range(4):
                            sb_i = cc * 4 + jj
                            mm(PS[4][:, cc * 128:(cc + 1) * 128], Wc[0][:, sb_i * 128:(sb_i + 1) * 128], sTre[:, sb_i * 128:(sb_i + 1) * 128], jj == 0, False, ["Wc0", "sTre"], [PSK[4]])
                            mm(PS[4][:, cc * 128:(cc + 1) * 128], Wc[1][:, sb_i * 128:(sb_i + 1) * 128], sTim[:, sb_i * 128:(sb_i + 1) * 128], False, jj == 3, ["Wc1", "sTim"], [PSK[4]])
                    for cc in range(4):
                        stt(yv[:, cc * 128:(cc + 1) * 128], usT3[:, cc, j * 128:(j + 1) * 128], vec_sb[:, cc:cc + 1], PS[4][:, cc * 128:(cc + 1) * 128], ALU.mult, ALU.add, ["usT", "vec", PSK[4]], ["yv"])
                    act(yg3[:, :, tl * 128:(tl + 1) * 128], yv.rearrange("p (c t) -> p c t", t=128), AF.Gelu, ["yv"], ["yg"])
                tt(cu1, L_re, al_re, ALU.mult, ["Lcos", "al_re"], ["cu1"])
                tt(cu2, L_im, al_im, ALU.mult, ["Lsin", "al_im"], ["cu2"])
                tt(car_re, cu1, cu2, ALU.subtract, ["cu1", "cu2"], ["car_re"])
                tt(cu1, L_re, al_im, ALU.mult, ["Lcos", "al_im"], ["cu1"])
                tt(cu2, L_im, al_re, ALU.mult, ["Lsin", "al_re"], ["cu2"])
                tt(car_im, cu1, cu2, ALU.add, ["cu1", "cu2"], ["car_im"])
            sg = [A(512, BF16) for _ in range(2)]
            n = 0
            for co in range(4):
                for tb in range(2):
                    b = n % 2
                    n += 1
                    for cc in range(4):
                        mm(PS[b][:, :], wglu3[:, cc, co * 128:(co + 1) * 128], yg3[:, cc, tb * 512:(tb + 1) * 512], cc == 0, cc == 3, ["wglu", "yg"], [PSK[b]])
                    act(sg[b], PS[b][:, :], AF.Sigmoid, [PSK[b], "vec"], ["sg%d" % b], bias=vec_sb[:, 4 + co:5 + co])
                    tt(mixT3[:, 8 + co, tb * 512:(tb + 1) * 512], sg[b], yg3[:, co, tb * 512:(tb + 1) * 512], ALU.mult, ["sg%d" % b, "yg"], ["mixT"])
            p.barrier()

            top[0] = l1_top
            pw_sb = A(4 * 128, BF16)
            pw3 = pw_sb.rearrange("p (g d) -> p g d", d=128)
            dma(pw3, pool_w.rearrange("(g c) d -> c g d", c=128), [], ["pw"], "pw", eng="gpsimd")
            xf = A(1152)
            pa = A(1152)
            pb = A(1152)
            pl = A(1024, BF16)
            for g in range(4):
                win = 2 ** (g + 1)
                cp(xf, upT3[:, g, :], ["upT"], ["xf"])
                src, sk = xf, "xf"
                bufs = [(pa, "pa"), (pb, "pb")]
                sh = 1
                for s in range(g + 1):
                    dst, dk = bufs[s % 2]
                    tt(dst[:, 16:1152], src[:, 16:1152], src[:, 16 - sh:1152 - sh], ALU.add, [sk], [dk])
                    src, sk = dst, dk
                    sh *= 2
                dst, dk = bufs[(g + 1) % 2]
                stt(dst[:, 128:1152], src[:, 128:1152], 1.0 / win, xf[:, 128:1152], ALU.mult, ALU.subtract, [sk, "xf"], [dk])
                tt(dst[:, 128:144], src[:, 128:144], invcnt16[:, g, :], ALU.mult, [sk, "pc"], [dk])
                tt(dst[:, 128:144], dst[:, 128:144], xf[:, 128:144], ALU.subtract, [dk, "xf"], [dk])
                cp(pl, dst[:, 128:1152], [dk], ["pl"])
                for tb in range(2):
                    b = tb
                    mm(PS[b][:, :], pw3[:, g, :], pl[:, tb * 512:(tb + 1) * 512], True, True, ["pw", "pl"], [PSK[b]])
                    ts(mixT3[:, 12 + g, tb * 512:(tb + 1) * 512], PS[b][:, :], vec_sb[:, 8 + g:9 + g], None, ALU.mult, None, [PSK[b], "vec"], ["mixT"])
            p.barrier()
            if "mixT" in dbg_out:
                fin.append(dma(dbg_out["mixT"], mixT, ["mixT"], [], "dbg_mixT", eng="gpsimd"))
                p.barrier()
            if stop_after == "H":
                p.emit(fin)
                return nc

            top[0] = mix_top
            wbI = [A(16 * 512, BF16) for _ in range(2)]
            rbuf = A(8 * 2048)
            r3 = rbuf.rearrange("p (t c) -> p t c", c=2048)
            xch = [A(512) for _ in range(2)]
            tmpI = A(512)
            lng = A(2048)
            lnb = A(2048)
            u2Tf = A(16 * 128)
            u2Tf3 = u2Tf.rearrange("p (k t) -> p k t", t=128)
            wr_sb = A(256)
            wr_hi = A(256, BF16)
            wr_lo = A(256, BF16)
            wrh3 = wr_hi.rearrange("p (k e) -> p k e", e=16)
            wrl3 = wr_lo.rearrange("p (k e) -> p k e", e=16)
            st_ = A(32)
            dma(lng, lnp[:, 0:2048], [], ["lng"], "lng")
            dma(lnb, lnp[:, 2048:4096], [], ["lnb"], "lnb")
            dma(wr_sb, w_router[:, :], [], ["wr"], "wr")
            cp(wr_hi, wr_sb, ["wr"], ["wr_hi"])
            tt(wr_lo, wr_sb, wr_hi, ALU.subtract, ["wr", "wr_hi"], ["wr_lo"])
            w_out_r = w_out.rearrange("(k p) n -> p k n", p=128)
            n = 0
            for cg in range(4):
                wv = wbI[cg % 2].rearrange("p (k n) -> p k n", n=512)
                wk = "wbI%d" % (cg % 2)
                dma_w(wv, w_out_r[:, :, cg * 512:(cg + 1) * 512], [wk], wk)
                for t in range(8):
                    b = n % 2
                    xc = xch[n % 2]
                    xck = "xch%d" % (n % 2)
                    n += 1
                    dma(xc, xown[t * 128:(t + 1) * 128, cg * 512:(cg + 1) * 512], [ownk], [xck], xck)
                    for kc in range(16):
                        mm(PS[b][:, :], mixT3[:, kc, t * 128:(t + 1) * 128], wv[:, kc, :], kc == 0, kc == 15, ["mixT", wk], [PSK[b]])
                    tt(tmpI, PS[b][:, :], g1p[:, cg * 512:(cg + 1) * 512], ALU.mult, [PSK[b], "g1p"], ["tmpI"])
                    stt(r3[:, t, cg * 512:(cg + 1) * 512], xc, ALU_ALPHA, tmpI, ALU.mult, ALU.add, [xck, "tmpI"], ["r%d" % t])
            p.barrier()

            def layer_norm(x_ap, xk, g_ap, b_ap, gk, bk, sidx, junk, junkk):
                s_sum = st_[:, sidx * 4 + 0:sidx * 4 + 1]
                s_mean = st_[:, sidx * 4 + 1:sidx * 4 + 2]
                s_ss = st_[:, sidx * 4 + 2:sidx * 4 + 3]
                s_rstd = st_[:, sidx * 4 + 3:sidx * 4 + 4]
                sk_ = "st%d" % sidx
                V(lambda e: e.tensor_reduce(out=s_sum, in_=x_ap, axis=AX.X, op=ALU.add), [xk], [sk_])
                ts(s_mean, s_sum, 1.0 / 2048, None, ALU.mult, None, [sk_], [sk_])
                ts(x_ap, x_ap, s_mean, None, ALU.subtract, None, [xk, sk_], [xk])
                tt(junk, x_ap, x_ap, ALU.mult, [xk], [junkk])
                red(s_ss, junk, ALU.add, [junkk], [sk_])
                act(s_rstd, s_ss, AF.Sqrt, [sk_, "eps"], [sk_], scale=1.0 / 2048, bias=eps_t[:, 0:1])
                V(lambda e: e.reciprocal(out=s_rstd, in_=s_rstd), [sk_], [sk_])
                stt(x_ap, x_ap, s_rstd, g_ap, ALU.mult, ALU.mult, [xk, sk_, gk], [xk])
                tt(x_ap, x_ap, b_ap, ALU.add, [xk, bk], [xk])

            u2T3 = mixT3
            rt_ = A(16 * 8)
            for t in range(8):
                xk = "r%d" % t
                x1 = r3[:, t, :]
                layer_norm(x1, xk, lng, lnb, "lng", "lnb", t % 2, u2Tf, "u2Tf")
                dma(xmid[t * 128:(t + 1) * 128, :], x1, [xk], ["xmid"], "xmid_w")
            p.barrier()
            xts = [u2Tf, lng]
            make_uT(xmid, 2, 3, srck=["xmid"])
            for t in range(8):
                for kc in range(16):
                    hi_ = u2T3[:, kc, t * 128:(t + 1) * 128]
                    mm(PS[4][:, 0:16], hi_, wrh3[:, kc, :], kc == 0, False, ["uT", "wr_hi"], [PSK[4]])
                    mm(PS[4][:, 0:16], hi_, wrl3[:, kc, :], False, kc == 15, ["uT", "wr_lo"], [PSK[4]])
                lg = rt_[:, 0:16]
                mx = rt_[:, 16:17]
                ex = rt_[:, 32:48]
                pr6 = rt_[:, 48:72]
                gsc = rt_[:, 72:76]
                gmx = rt_[:, 76:77]
                goh = rt_[:, 80:84]
                eg = rt_[:, 84:100]
                m1 = rt_[:, 100:101]
                m2 = rt_[:, 101:102]
                eg2 = rt_[:, 104:120]
                msk = rt_[:, 120:128] if False else None
                cp(lg, PS[4][:, 0:16], [PSK[4]], ["rt"])
                if stop_after == "I3":
                    cp(gates3[:, t, :], lg, ["rt"], ["gates"])
                    continue
                V(lambda e: e.tensor_reduce(out=mx, in_=lg, axis=AX.X, op=ALU.max), ["rt"], ["rt"])
                ts(lg, lg, mx, None, ALU.subtract, None, ["rt"], ["rt"])
                act(ex, lg, AF.Exp, ["rt"], ["rt"])
                ex3 = ex.rearrange("p (g j) -> p g j", j=4)
                pr63 = pr6.rearrange("p (g k) -> p g k", k=6)
                kk = 0
                for a in range(4):
                    for bq in range(a + 1, 4):
                        tt(pr63[:, :, kk], ex3[:, :, a], ex3[:, :, bq], ALU.add, ["rt"], ["rt"])
                        kk += 1
                V(lambda e: e.tensor_reduce(out=gsc, in_=pr63, axis=AX.X, op=ALU.max), ["rt"], ["rt"])
                V(lambda e: e.tensor_reduce(out=gmx, in_=gsc, axis=AX.X, op=ALU.max), ["rt"], ["rt"])
                ts(goh, gsc, gmx, None, ALU.is_ge, None, ["rt"], ["rt"])
                tt(eg.rearrange("p (g j) -> p g j", j=4), ex3, goh.unsqueeze(2).to_broadcast([128, 4, 4]), ALU.mult, ["rt"], ["rt"])
                V(lambda e: e.tensor_reduce(out=m1, in_=eg, axis=AX.X, op=ALU.max), ["rt"], ["rt"])
                ts(eg2, eg, m1, None, ALU.is_lt, None, ["rt"], ["rt"])
                tt(eg2, eg2, eg, ALU.mult, ["rt"], ["rt"])
                V(lambda e: e.tensor_reduce(out=m2, in_=eg2, axis=AX.X, op=ALU.max), ["rt"], ["rt"])
                ts(eg2, eg, m2, None, ALU.is_ge, None, ["rt"], ["rt"])
                tt(eg2, eg2, eg, ALU.mult, ["rt"], ["rt"])
                tt(m1, m1, m2, ALU.add, ["rt"], ["rt"])
                V(lambda e: e.reciprocal(out=m1, in_=m1), ["rt"], ["rt"])
                ts(gates3[:, t, :], eg2, m1, None, ALU.mult, None, ["rt"], ["gates"])
            dump("gates", gates, ["gates"])
            p.barrier()
            if stop_after in ("I", "I1", "I2", "I3"):
                fin.append(("dma", "xmid_w") and p.res["xmid"][0])
                p.emit(fin)
                return nc

            top[0] = mix_top
            accm = A(8 * 2048)
            acc3 = accm.rearrange("p (t c) -> p t c", c=2048)
            hT = A(8 * 1024, BF16)
            hT3 = hT.rearrange("p (f t) -> p f t", t=1024)
            NB = 3
            wg = [A(16 * 256, BF16) for _ in range(2)]
            wu = [A(16 * 256, BF16) for _ in range(2)]
            wd = [A(8 * 512, BF16) for _ in range(2)]
            sgm = [A(512, BF16) for _ in range(2)]
            moe_top = top[0]
            mset(accm, 0.0, ["acc_%d" % t for t in range(8)])
            ng = 0
            nd_ = 0
            nps = 0
            for ex_i in range(16):
                eg_r = e_gate[ex_i].rearrange("(k p) f -> p k f", p=128)
                eu_r = e_up[ex_i].rearrange("(k p) f -> p k f", p=128)
                ed_r = e_down[ex_i].rearrange("(f p) c -> p f c", p=128)
                for fcp in range(4):
                    i = ng % 2
                    ng += 1
                    wgv = wg[i].rearrange("p (k f) -> p k f", f=256)
                    wuv = wu[i].rearrange("p (k f) -> p k f", f=256)
                    dma_w(wgv, eg_r[:, :, fcp * 256:(fcp + 1) * 256], ["wg%d" % i], "wg%d" % i)
                    dma_w(wuv, eu_r[:, :, fcp * 256:(fcp + 1) * 256], ["wu%d" % i], "wu%d" % i)
                    for sub in range(2):
                        fc = fcp * 2 + sub
                        fsl = slice(sub * 128, (sub + 1) * 128)
                        for th_ in range(2):
                            tsl = slice(th_ * 512, (th_ + 1) * 512)
                            bg, bu = 0 + (nps % 2), 2 + (nps % 2)
                            s_ = sgm[nps % 2]
                            sk_ = "sgm%d" % (nps % 2)
                            nps += 1
                            for kc in range(16):
                                mm(PS[bg][:, :], wgv[:, kc, fsl], u2T3[:, kc, tsl], kc == 0, kc == 15, ["wg%d" % i, "uT"], [PSK[bg]])
                            for kc in range(16):
                                mm(PS[bu][:, :], wuv[:, kc, fsl], u2T3[:, kc, tsl], kc == 0, kc == 15, ["wu%d" % i, "uT"], [PSK[bu]])
                            act(s_, PS[bg][:, :], AF.Silu, [PSK[bg]], [sk_])
                            tt(hT3[:, fc, tsl], s_, PS[bu][:, :], ALU.mult, [sk_, PSK[bu]], ["hT"])
                for cg in range(4):
                    i = nd_ % 2
                    wdv = wd[i].rearrange("p (f c) -> p f c", c=512)
                    dma_w(wdv, ed_r[:, :, cg * 512:(cg + 1) * 512], ["wd%d" % i], "wd%d" % i, nsplit=2)
                    for t in range(8):
                        b = 4 + (nd_ * 8 + t) % 4
                        for fc in range(8):
                            mm(PS[b][:, :], hT3[:, fc, t * 128:(t + 1) * 128], wdv[:, fc, :], fc == 0, fc == 7, ["hT", "wd%d" % i], [PSK[b]])
                        stt(acc3[:, t, cg * 512:(cg + 1) * 512], PS[b][:, :], gates3[:, t, ex_i:ex_i + 1], acc3[:, t, cg * 512:(cg + 1) * 512], ALU.mult, ALU.add, [PSK[b], "gates", "acc_%d" % t], ["acc_%d" % t])
                    nd_ += 1
            p.barrier()
            top[0] = mix_top + 8 * 2048
            lng2 = A(2048)
            lnb2 = A(2048)
            xm = [A(2048) for _ in range(2)]
            junk = A(2048)
            st_ = A(32)
            dma(lng2, lnp[:, 4096:6144], [], ["lng2"], "lng2")
            dma(lnb2, lnp[:, 6144:8192], [], ["lnb2"], "lnb2")
            for t in range(8):
                xk = "acc_%d" % t
                a_t = acc3[:, t, :]
                xmk = "xm%d" % (t % 2)
                dma(xm[t % 2], xmid[t * 128:(t + 1) * 128, :], ["xmid"], [xmk], xmk)
                tt(a_t, a_t, g2p, ALU.mult, [xk, "g2p"], [xk])
                stt(a_t, xm[t % 2], ALU_ALPHA, a_t, ALU.mult, ALU.add, [xmk, xk], [xk])
                layer_norm(a_t, xk, lng2, lnb2, "lng2", "lnb2", t % 2, junk, "junk")
                fin.append(dma(xout[t * 128:(t + 1) * 128, :], a_t, [xk], [outk], "xout"))
            p.barrier()

        xts = None
        run_pass(0, 0, xA, xA, S1, True, "xA", "xA", "S1", noprefix=True)
        run_pass(0, 1, xA, xB, S2, False, "xA", "xB", "S2")
        run_pass(1, 1, S1, S2, xout_f, True, "S1", "S2", "xoutf")
        p.emit(fin)
    return nc


ALU_ALPHA = float(ALPHA)


def _consts():
    c = np.zeros((128, 3072), np.float32)
    c[:, 0:128] = np.eye(128, dtype=np.float32)
    c[:, 128:256] = np.arange(128, dtype=np.float32)[None, :]
    c[:, 256:384] = np.triu(np.ones((128, 128), np.float32))
    c[:, 384:512] = 1.0
    zt = np.zeros((128, 1024), np.float32)
    q = np.arange(128)[:, None]
    s = np.arange(128)[None, :]
    zt[:, 896:1024] = np.where(s <= q, 0.0, NEG)
    c[:, 1024:2048] = zt
    rot, half = 32, 16
    c[:, 2048:2064] = (500000.0 ** (-np.arange(half, dtype=np.float32) * 2.0 / rot))[None, :]
    rot, half = 16, 8
    c[:, 2064:2072] = (500000.0 ** (-np.arange(half, dtype=np.float32) * 2.0 / rot))[None, :]
    return c


def _layer_inputs(inp, l):
    f = np.float32
    rep = lambda v: np.ascontiguousarray(np.broadcast_to(np.asarray(v, f)[None, :], (128, v.shape[0])))
    d = {}
    d["b_ada"] = rep(inp["b_ada"][l])

    def st_layout(a):
        return np.ascontiguousarray(a.reshape(16, 2, 64).transpose(1, 2, 0).reshape(128, 16))
    lam_re = st_layout(inp["ssm_lam_re"][l])
    lam_im = st_layout(inp["ssm_lam_im"][l])
    lstep = st_layout(np.broadcast_to(inp["ssm_log_step"][l][:, None], (32, 64)))
    d["ssm_small"] = np.concatenate([lam_re, lam_im, lstep], axis=1).astype(f)

    def b_layout(a):
        return a.reshape(16, 2, 64, 16).transpose(1, 2, 0, 3).reshape(128, 256)

    def c_layout(a):
        return a.reshape(16, 2, 16, 64).transpose(1, 3, 0, 2).reshape(128, 256)
    d["ssm_bc"] = np.ascontiguousarray(np.concatenate(
        [b_layout(inp["ssm_b_re"][l]), b_layout(inp["ssm_b_im"][l]), c_layout(inp["ssm_c_re"][l]), c_layout(inp["ssm_c_im"][l])], axis=1)).astype(f)
    pk = lambda v: np.asarray(v, f).reshape(4, 128).T
    d["vecs"] = np.ascontiguousarray(np.concatenate([pk(inp["ssm_d"][l]), pk(inp["ssm_b_glu"][l]), pk(inp["pool_scale"][l])], axis=1))
    d["pool_w"] = np.ascontiguousarray(inp["pool_w"][l].reshape(512, 128))
    d["lnp"] = np.ascontiguousarray(np.concatenate([rep(inp["ln1_g"][l]), rep(inp["ln1_b"][l]), rep(inp["ln2_g"][l]), rep(inp["ln2_b"][l])], axis=1))
    return d


def _shared_inputs(inp):
    f = np.float32
    per = [_layer_inputs(inp, l) for l in range(2)]
    d = {k: np.ascontiguousarray(np.stack([per[0][k], per[1][k]], axis=0)) for k in per[0]}
    for k, src in (("w_ada", "w_ada"), ("w_in", "w_in"), ("w_out", "w_out"), ("w_glu", "ssm_w_glu"),
                   ("e_gate", "e_gate"), ("e_up", "e_up"), ("e_down", "e_down")):
        d[k] = np.ascontiguousarray(np.asarray(inp[src], f))
    d["w_router"] = np.ascontiguousarray(np.asarray(inp["w_router"], f).reshape(16, 128, 16).transpose(1, 0, 2).reshape(128, 256))
    d["cst"] = _consts()
    return d


def _cfg(pos_b, h):
    f = np.float32
    own = pos_b[h * 1024:(h + 1) * 1024].reshape(8, 128).T
    pre = pos_b[0:1024].reshape(8, 128).T if h == 1 else np.zeros((128, 8), np.int32)
    pos_in = np.concatenate([pre, own], axis=1).astype(np.int32)
    kbias = np.full((128, 1024), 0.0 if h == 1 else NEG, f)
    pc = np.zeros((128, 80), f)
    pc[:, 0] = float(h)
    for g, win in enumerate((2, 4, 8, 16)):
        t = np.arange(16) + h * 1024
        pc[:, 16 + g * 16:32 + g * 16] = (1.0 / np.minimum(t + 1, win))[None, :]
    return pos_in, kbias, pc


def _core_inputs(inp, core):
    b, h = core // 2, core % 2
    f = np.float32
    d = {}
    xb = np.asarray(inp["x"][b], f)
    d["xA"] = np.ascontiguousarray(xb[0:1024])
    d["xB"] = np.ascontiguousarray(xb[h * 1024:(h + 1) * 1024])
    d["c_in"] = np.ascontiguousarray(np.asarray(inp["c"][b], f).reshape(16, 128).T)
    pos = np.asarray(inp["positions"][b], np.int32)
    c0 = _cfg(pos, 0)
    c1 = _cfg(pos, h)
    d["pos_in"] = np.ascontiguousarray(np.stack([c0[0], c1[0]], 0))
    d["kbias"] = np.ascontiguousarray(np.stack([c0[1], c1[1]], 0))
    d["pcore"] = np.ascontiguousarray(np.stack([c0[2], c1[2]], 0))
    return d


_NC_CACHE = {}


def kernel(**inputs):
    inp = {k: np.asarray(v) for k, v in inputs.items()}
    if "prog" not in _NC_CACHE:
        _NC_CACHE["prog"] = build_program()
    nc = _NC_CACHE["prog"]
    shared = _shared_inputs(inp)
    in_maps = []
    for core in range(8):
        d = dict(shared)
        d.update(_core_inputs(inp, core))
        in_maps.append(d)
    res = run_bass_kernel_spmd(nc, in_maps, core_ids=list(range(8)))
    out = np.empty((4, 2048, 2048), np.float32)
    for core in range(8):
        b, h = core // 2, core % 2
        out[b, h * 1024:(h + 1) * 1024] = res.results[core]["xout"]
    return out
```

```python
import math
import contextlib
import numpy as np
import concourse.bass as bass
import concourse.mybir as mybir
from concourse.bass_utils import run_bass_kernel_spmd

F32 = mybir.dt.float32
BF16 = mybir.dt.bfloat16
I32 = mybir.dt.int32
AF = mybir.ActivationFunctionType
ALU = mybir.AluOpType
AX = mybir.AxisListType

ENGINES = ("sync", "scalar", "vector", "gpsimd", "tensor")
ALPHA = (2.0 * 2) ** 0.25
LN_EPS = 1e-5
PI = math.pi
NEG = -1e30

OQ, OK_, OV, OIQ, OIK, OIW, OUS, OUP = 0, 1024, 1280, 1536, 2048, 2112, 2120, 2632


class Prog:
    def __init__(self, nc):
        self.nc = nc
        self.ops = {e: [] for e in ENGINES}
        self.res = {}
        self.streams = {}
        self.pending = {e: {} for e in ENGINES}

    def _add(self, engine, fn, reads, writes, dma_key=None):
        skey = ("dma", dma_key) if dma_key is not None else ("eng", engine)
        st = self.streams.setdefault(skey, [])
        deps = dict(self.pending[engine])
        self.pending[engine] = {}

        def need(tok):
            if tok is None:
                return
            k, i = tok
            if k == skey and engine == "tensor" and dma_key is None:
                return
            if k[0] == "dma":
                i = len(self.streams[k]) - 1
                if k == skey:
                    i = len(st) - 1
            if i >= 0 and deps.get(k, -1) < i:
                deps[k] = i

        for r in reads:
            ent = self.res.get(r)
            if ent is not None:
                need(ent[0])
        for w in writes:
            ent = self.res.get(w)
            if ent is not None:
                need(ent[0])
                for t in ent[1]:
                    need(t)
        op = dict(fn=fn, deps=deps, skey=skey, idx=len(st), marked=False)
        st.append(op)
        self.ops[engine].append(op)
        tok = (skey, op["idx"])
        for w in writes:
            self.res[w] = [tok, []]
        for r in reads:
            if r in writes:
                continue
            ent = self.res.setdefault(r, [None, []])
            ent[1].append(tok)
        return tok

    def op(self, engine, fn, reads=(), writes=()):
        return self._add(engine, fn, reads, writes)

    def dma(self, engine, fn, reads=(), writes=(), key=None):
        return self._add(engine, fn, reads, writes, dma_key=key)

    def barrier(self):
        last = {k: len(st) - 1 for k, st in self.streams.items() if st}
        for e in ENGINES:
            for k, i in last.items():
                if self.pending[e].get(k, -1) < i:
                    self.pending[e][k] = i

    def emit(self, final_waits=()):
        nc = self.nc
        for e in ENGINES:
            waited = {}
            for op in self.ops[e]:
                nd = {}
                for k, i in op["deps"].items():
                    if waited.get(k, -1) >= i:
                        continue
                    waited[k] = i
                    nd[k] = i
                    self.streams[k][i]["marked"] = True
                op["deps"] = nd
        fin = {}
        for k, i in final_waits:
            if k[0] == "dma":
                i = len(self.streams[k]) - 1
            fin[k] = max(fin.get(k, -1), i)
        for k, st in self.streams.items():
            if st:
                fin[k] = len(st) - 1
        for k, i in fin.items():
            self.streams[k][i]["marked"] = True
        for k, st in self.streams.items():
            v = 0
            for op in st:
                if k[0] == "dma":
                    v += 16
                    op["inc"] = 16
                elif op["marked"]:
                    v += 1
                    op["inc"] = 1
                else:
                    op["inc"] = 0
                op["val"] = v
        with contextlib.ExitStack() as es:
            sems = {}
            for n, k in enumerate(self.streams):
                sems[k] = es.enter_context(nc.semaphore("s%d" % n))
            block = es.enter_context(nc.Block())

            def run(eng, ename):
                for op in self.ops[ename]:
                    for k, i in op["deps"].items():
                        eng.wait_ge(sems[k], self.streams[k][i]["val"])
                    ins = op["fn"](eng)
                    if op["inc"]:
                        ins.then_inc(sems[op["skey"]], op["inc"])
                if ename == "sync":
                    for k, i in fin.items():
                        eng.wait_ge(sems[k], self.streams[k][i]["val"])

            @block.sync
            def _(eng):
                run(eng, "sync")

            @block.scalar
            def _(eng):
                run(eng, "scalar")

            @block.vector
            def _(eng):
                run(eng, "vector")

            @block.gpsimd
            def _(eng):
                run(eng, "gpsimd")

            @block.tensor
            def _(eng):
                run(eng, "tensor")


SB_WORDS = 48400


def build_program(stop_after=None, dbg=()):
    nc = bass.Bass("TRN2", target_bir_lowering=False)

    def din(name, shape, dt=F32):
        return nc.dram_tensor(name, list(shape), dt, kind="ExternalInput").ap()

    xA = din("xA", [1024, 2048])
    xB = din("xB", [1024, 2048])
    c_in = din("c_in", [128, 16])
    pos_in2 = din("pos_in", [2, 128, 16], I32)
    cst = din("cst", [128, 3072])
    kbias2 = din("kbias", [2, 128, 1024])
    pcore2 = din("pcore", [2, 128, 80])
    w_ada_a = din("w_ada", [2, 2048, 12288])
    b_ada_a = din("b_ada", [2, 128, 12288])
    w_in_a = din("w_in", [2, 2048, 3144])
    w_out_a = din("w_out", [2, 2048, 2048])
    ssm_small_a = din("ssm_small", [2, 128, 48])
    ssm_bc_a = din("ssm_bc", [2, 128, 4 * 256])
    vecs_a = din("vecs", [2, 128, 12])
    w_glu_a = din("w_glu", [2, 512, 512])
    pool_w_a = din("pool_w", [2, 512, 128])
    lnp_a = din("lnp", [2, 128, 4 * 2048])
    w_router = din("w_router", [128, 256])
    e_gate_a = din("e_gate", [2, 16, 2048, 1024])
    e_up_a = din("e_up", [2, 16, 2048, 1024])
    e_down_a = din("e_down", [2, 16, 1024, 2048])
    xout_f = nc.dram_tensor("xout", [1024, 2048], F32, kind="ExternalOutput").ap()
    xmid = nc.dram_tensor("xmid_i", [1024, 2048], F32, kind="Internal").ap()
    S1 = nc.dram_tensor("s1_i", [1024, 2048], F32, kind="Internal").ap()
    S2 = nc.dram_tensor("s2_i", [1024, 2048], F32, kind="Internal").ap()
    dbg_out = {}

    es = contextlib.ExitStack()
    with es:
        SB = es.enter_context(nc.sbuf_tensor("SB", [128, SB_WORDS], F32))
        PS = [es.enter_context(nc.psum_tensor("ps%d" % i, [128, 512], F32)) for i in range(8)]
        PSK = ["ps%d" % i for i in range(8)]
        PSb = [t[:, :].bitcast(BF16) for t in PS]
        p = Prog(nc)
        top = [0]
        fin = []

        def A(n, dt=F32):
            w = n if dt != BF16 else (n + 1) // 2
            assert top[0] + w <= SB_WORDS, ("SBUF overflow", top[0], w)
            v = SB[:, top[0]:top[0] + w]
            top[0] += w
            return v if dt == F32 else v.bitcast(dt)

        def V(fn, r, w):
            return p.op("vector", fn, reads=r, writes=w)

        def S(fn, r, w):
            return p.op("scalar", fn, reads=r, writes=w)

        def mm(out, lhsT, rhs, start, stop, r, w):
            return p.op("tensor", lambda e: e.matmul(out, lhsT=lhsT, rhs=rhs, start=start, stop=stop), reads=r, writes=w)

        def tr(out, in_, ident, r, w):
            return p.op("tensor", lambda e: e.transpose(out=out, in_=in_, identity=ident), reads=r, writes=w)

        def dma(out, in_, r, w, key, eng="sync", slow=False):
            if slow:
                return p.dma(eng, lambda e: e.dma_start(out=out, in_=in_, allow_slow_non_contiguous=True), reads=r, writes=w, key=key)
            return p.dma(eng, lambda e: e.dma_start(out=out, in_=in_), reads=r, writes=w, key=key)

        def dma_w(out3, in3, w, key, nsplit=4):
            n = out3.shape[1]
            step = max(1, n // nsplit)
            tok = None
            for a in range(0, n, step):
                tok = dma(out3[:, a:a + step, :], in3[:, a:a + step, :], [], w, key, eng="gpsimd")
            return tok

        def dump(name, ap, r):
            if name in dbg_out:
                fin.append(dma(dbg_out[name], ap, r, [], "dbg_" + name))

        def act(out, in_, func, r, w, **kw):
            return S(lambda e: e.activation(out=out, in_=in_, func=func, **kw), r, w)

        def tt(out, in0, in1, op, r, w):
            return V(lambda e: e.tensor_tensor(out=out, in0=in0, in1=in1, op=op), r, w)

        def ts(out, in0, s1, s2, op0, op1, r, w):
            if op1 is None:
                return V(lambda e: e.tensor_scalar(out=out, in0=in0, scalar1=s1, scalar2=None, op0=op0), r, w)
            return V(lambda e: e.tensor_scalar(out=out, in0=in0, scalar1=s1, scalar2=s2, op0=op0, op1=op1), r, w)

        def stt(out, in0, scalar, in1, op0, op1, r, w):
            return V(lambda e: e.scalar_tensor_tensor(out=out, in0=in0, scalar=scalar, in1=in1, op0=op0, op1=op1), r, w)

        def cp(out, in_, r, w):
            return V(lambda e: e.tensor_copy(out=out, in_=in_), r, w)

        def G(fn, r, w):
            return p.op("gpsimd", fn, reads=r, writes=w)

        def gtt(out, in0, in1, op, r, w):
            return G(lambda e: e.tensor_tensor(out=out, in0=in0, in1=in1, op=op), r, w)

        def gstt(out, in0, scalar, in1, op0, op1, r, w):
            return G(lambda e: e.scalar_tensor_tensor(out=out, in0=in0, scalar=scalar, in1=in1, op0=op0, op1=op1), r, w)

        def vmax(out, in_, r, w):
            return V(lambda e: e.max(out=out, in_=in_), r, w)

        def vmr(out, rep_, vals, r, w):
            return V(lambda e: e.match_replace(out=out, in_to_replace=rep_, in_values=vals, imm_value=NEG), r, w)

        def red(out, in_, op, r, w):
            return V(lambda e: e.tensor_reduce(out=out, in_=in_, axis=AX.X, op=op), r, w)

        def recip(out, in_, r, w):
            return V(lambda e: e.reciprocal(out=out, in_=in_), r, w)

        def mset(out, val, w):
            return V(lambda e: e.memset(out, val), [], w)

        cst_sb = A(256 + 1024 + 32)
        ident_f = cst_sb[:, 0:128]
        iota_f = cst_sb[:, 128:256]
        ztri = cst_sb[:, 256:1280]
        invf = cst_sb[:, 1280:1304]
        dma(cst_sb[:, 0:256], cst[:, 0:256], [], ["cst"], "cst")
        dma(cst_sb[:, 256:1280], cst[:, 1024:2048], [], ["cst"], "cst")
        dma(cst_sb[:, 1280:1312], cst[:, 2048:2080], [], ["cst"], "cst")
        cstb = A(3 * 128, BF16)
        ident_b = cstb[:, 0:128]
        tri_b = cstb[:, 128:256]
        ones_b = cstb[:, 256:384]
        dma(ident_b, cst[:, 0:128], [], ["cstb"], "cstb", eng="gpsimd")
        dma(tri_b, cst[:, 256:384], [], ["cstb"], "cstb", eng="gpsimd")
        dma(ones_b, cst[:, 384:512], [], ["cstb"], "cstb", eng="gpsimd")
        pc_sb = A(80)
        pflag = pc_sb[:, 0:1]
        invcnt16 = pc_sb[:, 16:80].rearrange("p (g t) -> p g t", t=16)
        g1p = A(2048)
        g2p = A(2048)
        modp = A(64)
        modp3 = modp.rearrange("p (v k) -> p v k", k=16)
        gates = A(128)
        gates3 = gates.rearrange("p (t e) -> p t e", e=16)
        vec_sb = A(12)
        eps_t = A(1)
        V(lambda e: e.memset(eps_t, LN_EPS), [], ["eps"])
        perm_top = top[0]

        def run_pass(l, cfg, xpre, xown, xout, do_ada, prek, ownk, outk, noprefix=False):
            w_ada, b_ada, w_in, w_out = w_ada_a[l], b_ada_a[l], w_in_a[l], w_out_a[l]
            ssm_small, ssm_bc, vecs, w_glu, pool_w, lnp = ssm_small_a[l], ssm_bc_a[l], vecs_a[l], w_glu_a[l], pool_w_a[l], lnp_a[l]
            e_gate, e_up, e_down = e_gate_a[l], e_up_a[l], e_down_a[l]
            pos_in, kbias_in, pcore = pos_in2[cfg], kbias2[cfg], pcore2[cfg]
            dma(pc_sb, pcore[:, :], [], ["pc"], "pc")
            dma(vec_sb, vecs[:, :], [], ["vec"], "vec")
            if do_ada:
                phase_A(w_ada, b_ada)
            phase_rest(w_in, w_out, ssm_small, ssm_bc, w_glu, pool_w, lnp, e_gate, e_up, e_down, pos_in, kbias_in, xpre, xown, xout, prek, ownk, outk, noprefix)

        def phase_A(w_ada, b_ada):
            top[0] = perm_top
            c_sb = A(16)
            cond = A(16)
            condrep = A(16 * 128, BF16)
            condrep3 = condrep.rearrange("p (k j) -> p k j", j=128)
            condf = A(16 * 128)
            condf3 = condf.rearrange("p (k j) -> p k j", j=128)
            ada = A(12288)
            wbA = [A(16 * 512, BF16) for _ in range(2)]
            wbF = [A(16 * 512) for _ in range(2)]
            tmpA = wbF[0][:, 0:2048]
            tmpA3 = tmpA.rearrange("p (k j) -> p k j", j=128)
            dma(c_sb, c_in[:, :], [], ["c_sb"], "c_sb")
            act(cond, c_sb, AF.Silu, ["c_sb"], ["cond"])
            cp(condrep3, cond.unsqueeze(2).to_broadcast([128, 16, 128]), ["cond"], ["condrep"])
            cp(condf3, cond.unsqueeze(2).to_broadcast([128, 16, 128]), ["cond"], ["condf"])
            dma(ada, b_ada[:, :], [], ["ada"], "ada")
            w_ada_r = w_ada.rearrange("(k p) n -> p k n", p=128)
            for nb in range(24):
                j = (nb // 2) % 2
                src = w_ada_r[:, :, nb * 512:(nb + 1) * 512]
                if nb % 2 == 0:
                    wv = wbA[j].rearrange("p (k n) -> p k n", n=512)
                    wk = "wbA%d" % j
                    dma_w(wv, src, [wk], wk)
                    bank, bk, lhs, lk = PS[j], PSK[j], condrep3, "condrep"
                else:
                    wv = wbF[j].rearrange("p (k n) -> p k n", n=512)
                    wk = "wbF%d" % j
                    for q, eng in enumerate(("sync", "scalar")):
                        for a in range(q * 8, q * 8 + 8, 4):
                            dma(wv[:, a:a + 4, :], src[:, a:a + 4, :], [], [wk], wk + eng, eng=eng)
                    bank, bk, lhs, lk = PS[2 + j], PSK[2 + j], condf3, "condf"
                for kc in range(16):
                    mm(bank[:, :], lhs[:, kc, :], wv[:, kc, :], kc == 0, kc == 15, [lk, wk], [bk])
                tt(ada[:, nb * 512:(nb + 1) * 512], bank[:, :], ada[:, nb * 512:(nb + 1) * 512], ALU.add, [bk, "ada"], ["ada"])
            p.barrier()
            ts(g1p, ada[:, 2 * 2048:3 * 2048], 1.0, None, ALU.add, None, ["ada"], ["g1p"])
            ts(g2p, ada[:, 5 * 2048:6 * 2048], 1.0, None, ALU.add, None, ["ada"], ["g2p"])
            for slot, idx in enumerate((0, 1, 3, 4)):
                tt(tmpA3, ada[:, idx * 2048:(idx + 1) * 2048].rearrange("p (k j) -> p k j", j=128),
                   ident_f.unsqueeze(1).to_broadcast([128, 16, 128]), ALU.mult, ["ada", "cst"], ["tmpA"])
                V(lambda e, slot=slot: e.tensor_reduce(out=modp3[:, slot, :], in_=tmpA3, axis=AX.X, op=ALU.add), ["tmpA"], ["modp"])
            ts(modp3[:, 1, :], modp3[:, 1, :], 1.0, None, ALU.add, None, ["modp"], ["modp"])
            ts(modp3[:, 3, :], modp3[:, 3, :], 1.0, None, ALU.add, None, ["modp"], ["modp"])
            dump("ada", ada, ["ada"])
            dump("modp", modp, ["modp"])
            p.barrier()
            if stop_after == "A":
                p.emit(fin)
                return nc


        def phase_rest(w_in, w_out, ssm_small, ssm_bc, w_glu, pool_w, lnp, e_gate, e_up, e_down, pos_in, kbias_in, xpre, xown, xout, prek, ownk, outk, noprefix):
            nonlocal xts
            top[0] = perm_top
            mixT = A(16 * 1024, BF16)
            mixT3 = mixT.rearrange("p (k t) -> p k t", t=1024)
            uT3 = mixT3
            mix_top = top[0]
            usT = A(4 * 2048, BF16)
            usT3 = usT.rearrange("p (c t) -> p c t", t=2048)
            upT = A(4 * 1152, BF16)
            upT3 = upT.rearrange("p (g t) -> p g t", t=1152)
            l1_top = top[0]
            qT = A(8 * 1024, BF16)
            qT3 = qT.rearrange("p (h t) -> p h t", t=1024)
            kT = A(2 * 2048, BF16)
            kT3 = kT.rearrange("p (h t) -> p h t", t=2048)
            v_sb = A(16 * 256, BF16)
            v_sb4 = v_sb.rearrange("p (b h d) -> p b h d", h=2, d=128)
            ikT2 = A(2048, BF16)
            iqT = A(4 * 1024, BF16)
            iqT3 = iqT.rearrange("p (j t) -> p j t", t=1024)
            iw_sb = A(64)
            iw3 = iw_sb.rearrange("p (t h) -> p t h", h=8)
            cosT = A(16 * 24)
            sinT = A(16 * 24)
            cos3 = cosT.rearrange("p (t f) -> p t f", f=24)
            sin3 = sinT.rearrange("p (t f) -> p t f", f=24)
            att_top = top[0]
            wb = [A(16 * 512, BF16) for _ in range(2)]
            xts = [A(2048) for _ in range(2)]
            qt = A(512)
            kt = A(256)
            rtmp = A(4 * 64)
            pos_i = A(16, I32)
            pos_f = A(16)
            ang = A(16 * 24)
            ang3 = ang.rearrange("p (t f) -> p t f", f=24)
            sc_kf = A(16 * 24)
            sc_ki = A(16 * 24, I32)
            sc_t = A(16 * 24)

            def sin_of(out, in_, addc, kf, ki, t, rk, wk, kfk, kik, tk):
                ts(t, in_, addc, None, ALU.add, None, rk, [tk])
                ts(kf, t, 1.0 / (2 * PI), None, ALU.mult, None, [tk], [kfk])
                cp(ki, kf, [kfk], [kik])
                cp(kf, ki, [kik], [kfk])
                stt(t, kf, -2 * PI, t, ALU.mult, ALU.add, [kfk, tk], [tk])
                ts(kf, t, PI, -2 * PI, ALU.is_gt, ALU.mult, [tk], [kfk])
                tt(t, t, kf, ALU.add, [tk, kfk], [tk])
                ts(kf, t, -PI, 2 * PI, ALU.is_lt, ALU.mult, [tk], [kfk])
                tt(t, t, kf, ALU.add, [tk, kfk], [tk])
                act(out, t, AF.Sin, [tk], [wk])

            dma(pos_i, pos_in[:, :], [], ["pos_i"], "pos_i")
            cp(pos_f, pos_i, ["pos_i"], ["pos_f"])
            tt(ang3, pos_f.unsqueeze(2).to_broadcast([128, 16, 24]), invf.unsqueeze(1).to_broadcast([128, 16, 24]), ALU.mult, ["pos_f", "cst"], ["ang"])
            sin_of(sinT, ang, 0.0, sc_kf, sc_ki, sc_t, ["ang"], "sinT", "sc_kf", "sc_ki", "sc_t")
            sin_of(cosT, ang, PI / 2, sc_kf, sc_ki, sc_t, ["ang"], "cosT", "sc_kf", "sc_ki", "sc_t")
            dump("cosT", cosT, ["cosT"])

            w_in_r = w_in.rearrange("(k p) n -> p k n", p=128)
            wcnt = [0]

            def load_w(col0, ncols):
                i = wcnt[0] % 2
                wcnt[0] += 1
                wv = wb[i].rearrange("p (k n) -> p k n", n=512)
                dma_w(wv[:, :, 0:ncols], w_in_r[:, :, col0:col0 + ncols], ["wb%d" % i], "wb%d" % i)
                return wv, "wb%d" % i

            def rope(x3, nh, half, tile, foff, rk):
                cs = cos3[:, tile, foff:foff + half].unsqueeze(1).to_broadcast([128, nh, half])
                sn = sin3[:, tile, foff:foff + half].unsqueeze(1).to_broadcast([128, nh, half])
                x1 = x3[:, :, 0:half]
                x2 = x3[:, :, half:2 * half]
                t = [rtmp[:, j * 64:j * 64 + nh * half].rearrange("p (h f) -> p h f", f=half) for j in range(4)]
                tt(t[0], x1, cs, ALU.mult, [rk, "cosT"], ["rt0"])
                tt(t[1], x2, sn, ALU.mult, [rk, "sinT"], ["rt1"])
                tt(t[2], x2, cs, ALU.mult, [rk, "cosT"], ["rt2"])
                tt(t[3], x1, sn, ALU.mult, [rk, "sinT"], ["rt3"])
                tt(x1, t[0], t[1], ALU.subtract, ["rt0", "rt1"], [rk])
                tt(x2, t[2], t[3], ALU.add, ["rt2", "rt3"], [rk])

            def make_uT(xsrc, slot_sh, slot_sc, srck=()):
                for t in range(8):
                    xt = xts[t % 2]
                    xk = "xt%d" % (t % 2)
                    dma(xt, xsrc[t * 128:(t + 1) * 128, :], list(srck), [xk], xk)
                    for g in range(4):
                        b = 2 + (g % 2)
                        for j in range(4):
                            kc = g * 4 + j
                            tr(PS[b][:, j * 128:(j + 1) * 128], xt[:, kc * 128:(kc + 1) * 128], ident_f, [xk, "cst"], [PSK[b]])
                        for j in range(4):
                            kc = g * 4 + j
                            o = uT3[:, kc, t * 128:(t + 1) * 128]
                            i_ = PS[b][:, j * 128:(j + 1) * 128]
                            if j % 2 == 0:
                                act(o, i_, AF.Identity, [PSK[b], "modp"], ["uT"], scale=modp3[:, slot_sc, kc:kc + 1], bias=modp3[:, slot_sh, kc:kc + 1])
                            else:
                                ts(o, i_, modp3[:, slot_sc, kc:kc + 1], modp3[:, slot_sh, kc:kc + 1], ALU.mult, ALU.add, [PSK[b], "modp"], ["uT"])

            def proj_tok(col0, ncols, consume):
                wv, wk = load_w(col0, ncols)
                for t in range(8):
                    b = t % 2
                    for kc in range(16):
                        mm(PS[b][:, 0:ncols], uT3[:, kc, t * 128:(t + 1) * 128], wv[:, kc, 0:ncols], kc == 0, kc == 15, ["uT", wk], [PSK[b]])
                    consume(t, PS[b], PSK[b])

            def proj_T(col0, nchunks, consume, blocks):
                wv, wk = load_w(col0, nchunks * 128)
                n = 0
                for cc in range(nchunks):
                    for (t0, tn) in blocks:
                        b = n % 2
                        n += 1
                        for kc in range(16):
                            mm(PS[b][:, 0:tn], wv[:, kc, cc * 128:(cc + 1) * 128], uT3[:, kc, t0:t0 + tn], kc == 0, kc == 15, ["uT", wk], [PSK[b]])
                        consume(cc, t0, tn, PS[b], PSK[b])

            def kv_consumer(tile_base):
                def f(t, bank, bk):
                    gt = tile_base + t
                    act(kt, bank[:, 0:256], AF.Copy, [bk], ["kt"])
                    act(v_sb4[:, gt, :, :], bank[:, 256:512].rearrange("p (h d) -> p h d", d=128), AF.Copy, [bk], ["v_sb"])
                    rope(kt.rearrange("p (h d) -> p h d", d=128), 2, 16, gt, 0, "kt")
                    for h in range(2):
                        tr(PS[4][:, h * 128:(h + 1) * 128], kt[:, h * 128:(h + 1) * 128], ident_f, ["kt", "cst"], [PSK[4]])
                    cp(kT3[:, :, gt * 128:(gt + 1) * 128], PS[4][:, 0:256].rearrange("p (h t) -> p h t", t=128), [PSK[4]], ["kT"])
                return f

            def ik_consumer(tile_base, own):
                def f(t, bank, bk):
                    gt = tile_base + t
                    act(kt[:, 0:64], bank[:, 0:64], AF.Copy, [bk], ["kt"])
                    if own:
                        act(iw3[:, t, :], bank[:, 64:72], AF.Copy, [bk], ["iw"])
                    rope(kt[:, 0:64].rearrange("p (h d) -> p h d", d=64), 1, 8, gt, 16, "kt")
                    cp(kt[:, 64:128], kt[:, 0:64], ["kt"], ["kt"])
                    tr(PS[5][:, 0:128], kt[:, 0:128], ident_f, ["kt", "cst"], [PSK[5]])
                    cp(ikT2[:, gt * 128:(gt + 1) * 128], PS[5][:, 0:128], [PSK[5]], ["ikT2"])
                return f

            def us_consumer(tok_base, prefix):
                def f(cc, t0, tn, bank, bk):
                    o = usT3[:, cc, tok_base + t0:tok_base + t0 + tn]
                    if prefix:
                        ts(o, bank[:, 0:tn], pflag, None, ALU.mult, None, [bk, "pc"], ["usT"])
                    else:
                        act(o, bank[:, 0:tn], AF.Copy, [bk], ["usT"])
                return f

            def up_consumer(prefix):
                def f(cc, t0, tn, bank, bk):
                    if prefix:
                        ts(upT3[:, cc, 0:128], bank[:, 0:tn], pflag, None, ALU.mult, None, [bk, "pc"], ["upT"])
                    else:
                        act(upT3[:, cc, 128 + t0:128 + t0 + tn], bank[:, 0:tn], AF.Copy, [bk], ["upT"])
                return f

            def q_consumer(g):
                def f(t, bank, bk):
                    act(qt, bank[:, :], AF.Copy, [bk], ["qt"])
                    rope(qt.rearrange("p (h d) -> p h d", d=128), 4, 16, 8 + t, 0, "qt")
                    for h in range(4):
                        tr(PS[6][:, h * 128:(h + 1) * 128], qt[:, h * 128:(h + 1) * 128], ident_f, ["qt", "cst"], [PSK[6]])
                    cp(qT3[:, 4 * g:4 * g + 4, t * 128:(t + 1) * 128], PS[6][:, :].rearrange("p (h t) -> p h t", t=128), [PSK[6]], ["qT"])
                return f

            def iq_consumer(t, bank, bk):
                act(qt, bank[:, :], AF.Copy, [bk], ["qt"])
                rope(qt.rearrange("p (h d) -> p h d", d=64), 8, 8, 8 + t, 16, "qt")
                for j in range(4):
                    tr(PS[7][:, j * 128:(j + 1) * 128], qt[:, j * 128:(j + 1) * 128], ident_f, ["qt", "cst"], [PSK[7]])
                cp(iqT3[:, :, t * 128:(t + 1) * 128], PS[7][:, :].rearrange("p (j t) -> p j t", t=128), [PSK[7]], ["iqT"])

            if not noprefix:
                make_uT(xpre, 0, 1, srck=[prek])
                proj_tok(OK_, 512, kv_consumer(0))
                proj_tok(OIK, 72, ik_consumer(0, False))
                proj_T(OUS, 4, us_consumer(0, True), [(0, 512), (512, 512)])
                proj_T(OUP, 4, up_consumer(True), [(896, 128)])
            else:
                mset(upT3[:, :, 0:128], 0.0, ["upT"])
            make_uT(xown, 0, 1, srck=[ownk])
            dump("uT", None, None) if False else None
            proj_tok(OQ, 512, q_consumer(0))
            proj_tok(OQ + 512, 512, q_consumer(1))
            proj_tok(OK_, 512, kv_consumer(8))
            proj_tok(OIQ, 512, iq_consumer)
            proj_tok(OIK, 72, ik_consumer(8, True))
            proj_T(OUS, 4, us_consumer(1024, False), [(0, 512), (512, 512)])
            proj_T(OUP, 4, up_consumer(False), [(0, 512), (512, 512)])
            if "qT" in dbg_out:
                for nm, ap_, k_ in (("qT", qT, "qT"), ("kT", kT, "kT"), ("v_sb", v_sb, "v_sb"), ("ikT2", ikT2, "ikT2"),
                                    ("iqT", iqT, "iqT"), ("usT", usT, "usT"), ("upT", upT, "upT")):
                    fin.append(dma(dbg_out[nm], ap_, [k_], [], "dbg_" + nm, eng="gpsimd"))
                dump("iw", iw_sb, ["iw"])
            p.barrier()
            if stop_after == "BC":
                p.emit(fin)
                return nc

            top[0] = att_top
            accs = [A(2048), A(2048)]
            work = A(2048)
            rl = [A(512) for _ in range(2)]
            m8 = A(256)
            thr = A(1)
            m01 = A(2048, BF16)
            m01Ts = [A(16 * 128, BF16).rearrange("p (k q) -> p k q", q=128) for _ in range(2)]
            osb = A(512)
            dsb = A(512)
            mb_c = A(2)
            mset(mb_c[:, 0:1], 30000.0, ["mb_c"])
            mset(mb_c[:, 1:2], -30000.0, ["mb_c"])
            PTs = [A(512, BF16) for _ in range(2)]
            rden = A(512)
            kb_sb = A(1024)
            dma(kb_sb, kbias_in[:, :], [], ["kbias"], "kbias")
            SCALE = 128 ** -0.5
            k0 = 1024 if noprefix else 0
            kb0 = k0 // 128
            def geom(i):
                nk = 1024 + (i + 1) * 128 - k0
                return nk, nk // 128, (nk + 511) // 512

            cntr = [0]

            def indexer(i):
                nk, nkb, n5 = geom(i)
                acc, acck = accs[i % 2], "acc%d" % (i % 2)
                for h in range(8):
                    pr = (h % 2) * 64
                    for b5 in range(n5):
                        c0 = b5 * 512
                        w = min(512, nk - c0)
                        b = cntr[0] % 2
                        cntr[0] += 1
                        mm(PS[b][:, 0:w], iqT3[pr:pr + 64, h // 2, i * 128:(i + 1) * 128], ikT2[pr:pr + 64, k0 + c0:k0 + c0 + w], True, True, ["iqT", "ikT2"], [PSK[b]])
                        act(rl[b][:, 0:w], PS[b][:, 0:w], AF.Relu, [PSK[b]], ["rl%d" % b])
                        if h == 0:
                            if k0 + c0 < 1024:
                                in1 = kb_sb[:, c0:c0 + w]
                            else:
                                o0 = 896 - i * 128 + (k0 + c0 - 1024)
                                in1 = ztri[:, o0:o0 + w]
                            stt(acc[:, c0:c0 + w], rl[b][:, 0:w], iw3[:, i, h:h + 1], in1, ALU.mult, ALU.add, ["rl%d" % b, "iw", "kbias", "cst"], [acck])
                        else:
                            stt(acc[:, c0:c0 + w], rl[b][:, 0:w], iw3[:, i, h:h + 1], acc[:, c0:c0 + w], ALU.mult, ALU.add, ["rl%d" % b, "iw", acck], [acck])

            def topk(i):
                nk, nkb, n5 = geom(i)
                acc, acck = accs[i % 2], "acc%d" % (i % 2)
                if nk > 256:
                    cur, ck = acc, acck
                    for r in range(32):
                        vmax(m8[:, r * 8:(r + 1) * 8], cur[:, 0:nk], [ck], ["m8"])
                        if r < 31:
                            vmr(work[:, 0:nk], m8[:, r * 8:(r + 1) * 8], cur[:, 0:nk], [ck, "m8"], ["work"])
                            cur, ck = work, "work"
                    ts(thr, m8[:, 255:256], -1e29, None, ALU.max, None, ["m8"], ["thr"])
                    ts(m01[:, 0:nk], acc[:, 0:nk], thr[:, 0:1], None, ALU.is_ge, None, [acck, "thr"], ["m01"])
                else:
                    ts(m01[:, 0:nk], acc[:, 0:nk], -1e29, None, ALU.is_ge, None, [acck], ["m01"])

            def attn(i):
                nk, nkb, n5 = geom(i)
                m01T3, m01Tk = m01Ts[i % 2], "m01T%d" % (i % 2)
                for g0 in range(0, nkb, 8):
                    gn = min(8, nkb - g0)
                    b = 2 + (g0 // 8) % 2
                    for j in range(gn):
                        kb = g0 + j
                        tr(PSb[b][:, j * 128:(j + 1) * 128], m01[:, kb * 128:(kb + 1) * 128], ident_b, ["m01", "cstb"], [PSK[b]])
                    act(m01T3[:, g0:g0 + gn, :], PSb[b][:, 0:gn * 128].rearrange("p (k q) -> p k q", q=128), AF.Identity, [PSK[b], "mb_c"], [m01Tk],
                        scale=mb_c[:, 0:1], bias=mb_c[:, 1:2])
                for kvh in range(2):
                    for kb in range(nkb):
                        gkb = kb0 + kb
                        sb_ = 4 + kb % 2
                        pk = "PT%d" % (kb % 2)
                        PT = PTs[kb % 2]
                        PT3 = PT.rearrange("p (h q) -> p h q", q=128)
                        mm(PS[sb_][:, :], kT3[:, kvh, gkb * 128:(gkb + 1) * 128], qT3[:, 4 * kvh:4 * kvh + 4, i * 128:(i + 1) * 128], True, False, ["kT", "qT"], [PSK[sb_]])
                        for hh in range(4):
                            mm(PS[sb_][:, hh * 128:(hh + 1) * 128], ident_b, m01T3[:, kb, :], False, True, ["cstb", m01Tk], [PSK[sb_]])
                        act(PT, PS[sb_][:, :], AF.Exp, [PSK[sb_]], [pk], scale=SCALE)
                        mm(PS[6][:, :], v_sb4[:, gkb, kvh, :], PT, kb == 0, kb == nkb - 1, ["v_sb", pk], [PSK[6]])
                        mm(PS[7][:, :], ones_b, PT, kb == 0, kb == nkb - 1, ["cstb", pk], [PSK[7]])
                    act(osb, PS[6][:, :], AF.Copy, [PSK[6]], ["osb"])
                    act(dsb, PS[7][:, :], AF.Ln, [PSK[7]], ["dsb"])
                    act(dsb, dsb, AF.Exp, ["dsb"], ["dsb"], scale=-1.0)
                    gtt(mixT3[:, 4 * kvh:4 * kvh + 4, i * 128:(i + 1) * 128], osb.rearrange("p (h q) -> p h q", q=128),
                        dsb.rearrange("p (h q) -> p h q", q=128), ALU.mult, ["osb", "dsb"], ["mixT"])

            indexer(0)
            for i in range(8):
                if i + 1 < 8:
                    indexer(i + 1)
                topk(i)
                attn(i)
            p.barrier()
            if stop_after == "EF":
                if "mixT" in dbg_out:
                    fin.append(dma(dbg_out["mixT"], mixT, ["mixT"], [], "dbg_mixT", eng="gpsimd"))
                p.emit(fin)
                return nc

            top[0] = l1_top
            sm = A(48)
            dma(sm, ssm_small[:, :], [], ["sm"], "sm")
            lam_re, lam_im, lstep = sm[:, 0:16], sm[:, 16:32], sm[:, 32:48]
            PTre = A(2048)
            PTim = A(2048)
            PinvRe = A(2048)
            PinvIm = A(2048)
            W_B = [A(4 * 512, BF16) for _ in range(2)]
            Wc = [A(16 * 128, BF16) for _ in range(2)]
            wglu_sb = A(4 * 512, BF16)
            sv = A(16 * 24)
            ki16 = A(16, I32)
            ssm_tmp = top[0]
            bc_sb = A(1024)
            dma(bc_sb, ssm_bc[:, :], [], ["bc_sb"], "bc_sb")
            b_re3 = bc_sb[:, 0:256].rearrange("p (s c) -> p s c", c=16)
            b_im3 = bc_sb[:, 256:512].rearrange("p (s c) -> p s c", c=16)
            c_re3 = bc_sb[:, 512:768].rearrange("p (s c) -> p s c", c=16)
            c_im3 = bc_sb[:, 768:1024].rearrange("p (s c) -> p s c", c=16)
            svn = [0]

            def SV():
                v = sv[:, svn[0] * 16:(svn[0] + 1) * 16]
                svn[0] += 1
                return v
            dt_, ar, th = SV(), SV(), SV()
            act(dt_, lstep, AF.Exp, ["sm"], ["dt"])
            tt(ar, lam_re, dt_, ALU.mult, ["sm", "dt"], ["ar"])
            tt(th, lam_im, dt_, ALU.mult, ["sm", "dt"], ["th"])
            PIre = A(2048)
            PIim = A(2048)
            big1 = A(2048)
            big2 = A(2048)
            big3 = A(2048)
            bigi = A(2048, I32)
            B3 = lambda v: v.rearrange("p (s t) -> p s t", t=128)
            iota_b = iota_f.unsqueeze(1).to_broadcast([128, 16, 128])
            tt(B3(big1), th.unsqueeze(2).to_broadcast([128, 16, 128]), iota_b, ALU.mult, ["th", "cst"], ["big1"])
            sin_of(PIim, big1, 0.0, big2, bigi, big3, ["big1"], "sinS", "big2", "bigi", "big3")
            sin_of(PIre, big1, PI / 2, big2, bigi, big3, ["big1"], "cosS", "big2", "bigi", "big3")
            tt(B3(big1), ar.unsqueeze(2).to_broadcast([128, 16, 128]), iota_b, ALU.mult, ["ar", "cst"], ["big1"])
            act(big2, big1, AF.Exp, ["big1"], ["big2"])
            act(big3, big1, AF.Exp, ["big1"], ["big3"], scale=-1.0)
            tt(PTre, big2, PIre, ALU.mult, ["big2", "cosS"], ["PTre"])
            tt(PTim, big2, PIim, ALU.mult, ["big2", "sinS"], ["PTim"])
            tt(PIre, big3, PIre, ALU.mult, ["big3", "cosS"], ["cosS"])
            stt(PIim, big3, -1.0, PIim, ALU.mult, ALU.mult, ["big3", "sinS"], ["sinS"])
            for comp, (src, sk, dst, dk) in enumerate(((PIre, "cosS", PinvRe, "PinvRe"), (PIim, "sinS", PinvIm, "PinvIm"))):
                for g in range(4):
                    b = 2 * comp + (g % 2)
                    for j in range(4):
                        sb_i = g * 4 + j
                        tr(PS[b][:, j * 128:(j + 1) * 128], src[:, sb_i * 128:(sb_i + 1) * 128], ident_f, [sk, "cst"], [PSK[b]])
                    cp(dst[:, g * 512:(g + 1) * 512], PS[b][:, :], [PSK[b]], [dk])
            L_re, L_im = SV(), SV()
            a128, k1, t1_, mg = SV(), SV(), SV(), SV()
            ts(a128, th, 128.0, None, ALU.mult, None, ["th"], ["a128"])
            sin_of(L_im, a128, 0.0, k1, ki16, t1_, ["a128"], "Lsin", "k1", "ki16", "t1_")
            sin_of(L_re, a128, PI / 2, k1, ki16, t1_, ["a128"], "Lcos", "k1", "ki16", "t1_")
            act(mg, ar, AF.Exp, ["ar"], ["mg"], scale=128.0)
            tt(L_re, L_re, mg, ALU.mult, ["Lcos", "mg"], ["Lcos"])
            tt(L_im, L_im, mg, ALU.mult, ["Lsin", "mg"], ["Lsin"])
            PTre3, PTim3 = B3(PTre), B3(PTim)
            a_, b_, den_, m_re, m_im, u1, u2 = SV(), SV(), SV(), SV(), SV(), SV(), SV()
            ts(a_, PTre3[:, :, 1], -1.0, None, ALU.add, None, ["PTre"], ["a_"])
            cp(b_, PTim3[:, :, 1], ["PTim"], ["b_"])
            tt(u1, lam_re, lam_re, ALU.mult, ["sm"], ["u1"])
            tt(u2, lam_im, lam_im, ALU.mult, ["sm"], ["u2"])
            tt(den_, u1, u2, ALU.add, ["u1", "u2"], ["den_"])
            V(lambda e: e.reciprocal(out=den_, in_=den_), ["den_"], ["den_"])
            tt(u1, a_, lam_re, ALU.mult, ["a_", "sm"], ["u1"])
            tt(u2, b_, lam_im, ALU.mult, ["b_", "sm"], ["u2"])
            tt(m_re, u1, u2, ALU.add, ["u1", "u2"], ["m_re"])
            tt(m_re, m_re, den_, ALU.mult, ["m_re", "den_"], ["m_re"])
            tt(u1, b_, lam_re, ALU.mult, ["b_", "sm"], ["u1"])
            tt(u2, a_, lam_im, ALU.mult, ["a_", "sm"], ["u2"])
            tt(m_im, u1, u2, ALU.subtract, ["u1", "u2"], ["m_im"])
            tt(m_im, m_im, den_, ALU.mult, ["m_im", "den_"], ["m_im"])
            p.barrier()
            bigf = bigi.bitcast(F32)
            Bb_re, Bb_im, bt1, bt2 = bigf[:, 0:256], bigf[:, 256:512], bigf[:, 512:768], bigf[:, 768:1024]
            S3 = lambda v: v.rearrange("p (s c) -> p s c", c=16)
            mre_b = m_re.unsqueeze(2).to_broadcast([128, 16, 16])
            mim_b = m_im.unsqueeze(2).to_broadcast([128, 16, 16])
            tt(S3(bt1), b_re3, mre_b, ALU.mult, ["bc_sb", "m_re"], ["bt1"])
            tt(S3(bt2), b_im3, mim_b, ALU.mult, ["bc_sb", "m_im"], ["bt2"])
            tt(Bb_re, bt1, bt2, ALU.subtract, ["bt1", "bt2"], ["Bb_re"])
            tt(S3(bt1), b_im3, mre_b, ALU.mult, ["bc_sb", "m_re"], ["bt1"])
            tt(S3(bt2), b_re3, mim_b, ALU.mult, ["bc_sb", "m_im"], ["bt2"])
            tt(Bb_im, bt1, bt2, ALU.add, ["bt1", "bt2"], ["Bb_im"])
            for comp, (srcB, kB, srcC, cneg) in enumerate(((Bb_re, "Bb_re", c_re3, 1.0), (Bb_im, "Bb_im", c_im3, -1.0))):
                wide = big1 if comp == 0 else big2
                wk = "big1" if comp == 0 else "big2"
                V(lambda e, wide=wide: e.memset(wide, 0.0), [], [wk])
                w4 = wide.rearrange("p (a j c) -> p a j c", j=4, c=128)
                s4 = srcB.rearrange("p (a j c) -> p a j c", j=4, c=16)
                for j in range(4):
                    for gl in range(2):
                        cp(w4[gl * 64:(gl + 1) * 64, :, j, j * 32 + gl * 16:j * 32 + gl * 16 + 16], s4[gl * 64:(gl + 1) * 64, :, j, :], [kB], [wk])
                for g in range(4):
                    b = 4 + (g % 2)
                    for j in range(4):
                        sb_i = g * 4 + j
                        tr(PS[b][:, j * 128:(j + 1) * 128], wide[:, sb_i * 128:(sb_i + 1) * 128], ident_f, [wk, "cst"], [PSK[b]])
                    cp(W_B[comp][:, g * 512:(g + 1) * 512], PS[b][:, :], [PSK[b]], ["W_B%d" % comp])
                wide2 = big3 if comp == 0 else PIre
                wk2 = "big3" if comp == 0 else "cosS"
                V(lambda e, wide2=wide2: e.memset(wide2, 0.0), [], [wk2])
                w4 = wide2.rearrange("p (a j c) -> p a j c", j=4, c=128)
                s4 = srcC.rearrange("p (a j) c -> p a j c", j=4)
                for j in range(4):
                    for gl in range(2):
                        ts(w4[gl * 64:(gl + 1) * 64, :, j, j * 32 + gl * 16:j * 32 + gl * 16 + 16], s4[gl * 64:(gl + 1) * 64, :, j, :], cneg, None, ALU.mult, None, ["bc_sb"], [wk2])
                cp(Wc[comp], wide2, [wk2], ["Wc%d" % comp])
            p.barrier()
            top[0] = ssm_tmp
            wglu3 = wglu_sb.rearrange("p (k n) -> p k n", n=512)
            dma(wglu3, w_glu.rearrange("(k p) n -> p k n", p=128), [], ["wglu"], "wglu", eng="gpsimd")
            Xre = [A(2048, BF16) for _ in range(2)]
            Xim = [A(2048, BF16) for _ in range(2)]
            sTre = A(2048, BF16)
            sTim = A(2048, BF16)
            yg = A(4 * 1024, BF16)
            yg3 = yg.rearrange("p (c t) -> p c t", t=1024)
            tq = [A(512) for _ in range(4)]
            Are, Aim = A(512), A(512)
            car_re, car_im, al_re, al_im, cu1, cu2 = SV(), SV(), SV(), SV(), SV(), SV()
            yv = A(512)
            V(lambda e: e.memset(car_re, 0.0), [], ["car_re"])
            V(lambda e: e.memset(car_im, 0.0), [], ["car_im"])
            for j in range(8 if noprefix else 0, 16):
                own = j >= 8
                xr, xi = Xre[j % 2], Xim[j % 2]
                xrk, xik = "Xre%d" % (j % 2), "Xim%d" % (j % 2)
                for cc in range(4):
                    sl = slice(cc * 512, (cc + 1) * 512)
                    mm(PS[0][:, :], usT3[:, cc, j * 128:(j + 1) * 128], W_B[0][:, sl], True, True, ["usT", "W_B0"], [PSK[0]])
                    mm(PS[1][:, :], usT3[:, cc, j * 128:(j + 1) * 128], W_B[1][:, sl], True, True, ["usT", "W_B1"], [PSK[1]])
                    tt(tq[0], PS[0][:, :], PinvRe[:, sl], ALU.mult, [PSK[0], "PinvRe"], ["tq0"])
                    tt(tq[1], PS[1][:, :], PinvIm[:, sl], ALU.mult, [PSK[1], "PinvIm"], ["tq1"])
                    tt(tq[2], PS[1][:, :], PinvRe[:, sl], ALU.mult, [PSK[1], "PinvRe"], ["tq2"])
                    tt(tq[3], PS[0][:, :], PinvIm[:, sl], ALU.mult, [PSK[0], "PinvIm"], ["tq3"])
                    tt(xr[:, sl], tq[0], tq[1], ALU.subtract, ["tq0", "tq1"], [xrk])
                    tt(xi[:, sl], tq[2], tq[3], ALU.add, ["tq2", "tq3"], [xik])
                if not own:
                    for sb_i in range(16):
                        mm(PS[2][:, sb_i:sb_i + 1], xr[:, sb_i * 128:(sb_i + 1) * 128], ones_b[:, 0:1], True, True, [xrk, "cstb"], [PSK[2]])
                        mm(PS[3][:, sb_i:sb_i + 1], xi[:, sb_i * 128:(sb_i + 1) * 128], ones_b[:, 0:1], True, True, [xik, "cstb"], [PSK[3]])
                    tt(al_re, PS[2][:, 0:16], car_re, ALU.add, [PSK[2], "car_re"], ["al_re"])
                    tt(al_im, PS[3][:, 0:16], car_im, ALU.add, [PSK[3], "car_im"], ["al_im"])
                else:
                    tl = j - 8
                    for g in range(4):
                        for jj in range(4):
                            sb_i = g * 4 + jj
                            mm(PS[2][:, jj * 128:(jj + 1) * 128], xr[:, sb_i * 128:(sb_i + 1) * 128], tri_b, True, True, [xrk, "cstb"], [PSK[2]])
                            mm(PS[3][:, jj * 128:(jj + 1) * 128], xi[:, sb_i * 128:(sb_i + 1) * 128], tri_b, True, True, [xik, "cstb"], [PSK[3]])
                        gs = slice(g * 512, (g + 1) * 512)
                        A3 = lambda v: v.rearrange("p (s t) -> p s t", t=128)
                        tt(A3(Are), A3(PS[2][:, :]), car_re[:, g * 4:(g + 1) * 4].unsqueeze(2).to_broadcast([128, 4, 128]), ALU.add, [PSK[2], "car_re"], ["Are"])
                        tt(A3(Aim), A3(PS[3][:, :]), car_im[:, g * 4:(g + 1) * 4].unsqueeze(2).to_broadcast([128, 4, 128]), ALU.add, [PSK[3], "car_im"], ["Aim"])
                        cp(al_re[:, g * 4:(g + 1) * 4], A3(Are)[:, :, 127], ["Are"], ["al_re"])
                        cp(al_im[:, g * 4:(g + 1) * 4], A3(Aim)[:, :, 127], ["Aim"], ["al_im"])
                        tt(tq[0], PTre[:, gs], Are, ALU.mult, ["PTre", "Are"], ["tq0"])
                        tt(tq[1], PTim[:, gs], Aim, ALU.mult, ["PTim", "Aim"], ["tq1"])
                        tt(tq[2], PTre[:, gs], Aim, ALU.mult, ["PTre", "Aim"], ["tq2"])
                        tt(tq[3], PTim[:, gs], Are, ALU.mult, ["PTim", "Are"], ["tq3"])
                        tt(sTre[:, gs], tq[0], tq[1], ALU.subtract, ["tq0", "tq1"], ["sTre"])
                        tt(sTim[:, gs], tq[2], tq[3], ALU.add, ["tq2", "tq3"], ["sTim"])
                    for cc in range(4):
                        for jj in range(4):
                            sb_i = cc * 4 + jj
                            mm(PS[4][:, cc * 128:(cc + 1) * 128], Wc[0][:, sb_i * 128:(sb_i + 1) * 128], sTre[:, sb_i * 128:(sb_i + 1) * 128], jj == 0, False, ["Wc0", "sTre"], [PSK[4]])
                            mm(PS[4][:, cc * 128:(cc + 1) * 128], Wc[1][:, sb_i * 128:(sb_i + 1) * 128], sTim[:, sb_i * 128:(sb_i + 1) * 128], False, jj == 3, ["Wc1", "sTim"], [PSK[4]])
                    for cc in range(4):
                        stt(yv[:, cc * 128:(cc + 1) * 128], usT3[:, cc, j * 128:(j + 1) * 128], vec_sb[:, cc:cc + 1], PS[4][:, cc * 128:(cc + 1) * 128], ALU.mult, ALU.add, ["usT", "vec", PSK[4]], ["yv"])
                    act(yg3[:, :, tl * 128:(tl + 1) * 128], yv.rearrange("p (c t) -> p c t", t=128), AF.Gelu, ["yv"], ["yg"])
                tt(cu1, L_re, al_re, ALU.mult, ["Lcos", "al_re"], ["cu1"])
                tt(cu2, L_im, al_im, ALU.mult, ["Lsin", "al_im"], ["cu2"])
                tt(car_re, cu1, cu2, ALU.subtract, ["cu1", "cu2"], ["car_re"])
                tt(cu1, L_re, al_im, ALU.mult, ["Lcos", "al_im"], ["cu1"])
                tt(cu2, L_im, al_re, ALU.mult, ["Lsin", "al_re"], ["cu2"])
                tt(car_im, cu1, cu2, ALU.add, ["cu1", "cu2"], ["car_im"])
            sg = [A(512, BF16) for _ in range(2)]
            n = 0
            for co in range(4):
                for tb in range(2):
                    b = n % 2
                    n += 1
                    for cc in range(4):
                        mm(PS[b][:, :], wglu3[:, cc, co * 128:(co + 1) * 128], yg3[:, cc, tb * 512:(tb + 1) * 512], cc == 0, cc == 3, ["wglu", "yg"], [PSK[b]])
                    act(sg[b], PS[b][:, :], AF.Sigmoid, [PSK[b], "vec"], ["sg%d" % b], bias=vec_sb[:, 4 + co:5 + co])
                    tt(mixT3[:, 8 + co, tb * 512:(tb + 1) * 512], sg[b], yg3[:, co, tb * 512:(tb + 1) * 512], ALU.mult, ["sg%d" % b, "yg"], ["mixT"])
            p.barrier()

            top[0] = l1_top
            pw_sb = A(4 * 128, BF16)
            pw3 = pw_sb.rearrange("p (g d) -> p g d", d=128)
            dma(pw3, pool_w.rearrange("(g c) d -> c g d", c=128), [], ["pw"], "pw", eng="gpsimd")
            xf = A(1152)
            pa = A(1152)
            pb = A(1152)
            pl = A(1024, BF16)
            for g in range(4):
                win = 2 ** (g + 1)
                cp(xf, upT3[:, g, :], ["upT"], ["xf"])
                src, sk = xf, "xf"
                bufs = [(pa, "pa"), (pb, "pb")]
                sh = 1
                for s in range(g + 1):
                    dst, dk = bufs[s % 2]
                    tt(dst[:, 16:1152], src[:, 16:1152], src[:, 16 - sh:1152 - sh], ALU.add, [sk], [dk])
                    src, sk = dst, dk
                    sh *= 2
                dst, dk = bufs[(g + 1) % 2]
                stt(dst[:, 128:1152], src[:, 128:1152], 1.0 / win, xf[:, 128:1152], ALU.mult, ALU.subtract, [sk, "xf"], [dk])
                tt(dst[:, 128:144], src[:, 128:144], invcnt16[:, g, :], ALU.mult, [sk, "pc"], [dk])
                tt(dst[:, 128:144], dst[:, 128:144], xf[:, 128:144], ALU.subtract, [dk, "xf"], [dk])
                cp(pl, dst[:, 128:1152], [dk], ["pl"])
                for tb in range(2):
                    b = tb
                    mm(PS[b][:, :], pw3[:, g, :], pl[:, tb * 512:(tb + 1) * 512], True, True, ["pw", "pl"], [PSK[b]])
                    ts(mixT3[:, 12 + g, tb * 512:(tb + 1) * 512], PS[b][:, :], vec_sb[:, 8 + g:9 + g], None, ALU.mult, None, [PSK[b], "vec"], ["mixT"])
            p.barrier()
            if "mixT" in dbg_out:
                fin.append(dma(dbg_out["mixT"], mixT, ["mixT"], [], "dbg_mixT", eng="gpsimd"))
                p.barrier()
            if stop_after == "H":
                p.emit(fin)
                return nc

            top[0] = mix_top
            wbI = [A(16 * 512, BF16) for _ in range(2)]
            rbuf = A(8 * 2048)
            r3 = rbuf.rearrange("p (t c) -> p t c", c=2048)
            xch = [A(512) for _ in range(2)]
            tmpI = A(512)
            lng = A(2048)
            lnb = A(2048)
            u2Tf = A(16 * 128)
            u2Tf3 = u2Tf.rearrange("p (k t) -> p k t", t=128)
            wr_sb = A(256)
            wr_hi = A(256, BF16)
            wr_lo = A(256, BF16)
            wrh3 = wr_hi.rearrange("p (k e) -> p k e", e=16)
            wrl3 = wr_lo.rearrange("p (k e) -> p k e", e=16)
            st_ = A(32)
            dma(lng, lnp[:, 0:2048], [], ["lng"], "lng")
            dma(lnb, lnp[:, 2048:4096], [], ["lnb"], "lnb")
            dma(wr_sb, w_router[:, :], [], ["wr"], "wr")
            cp(wr_hi, wr_sb, ["wr"], ["wr_hi"])
            tt(wr_lo, wr_sb, wr_hi, ALU.subtract, ["wr", "wr_hi"], ["wr_lo"])
            w_out_r = w_out.rearrange("(k p) n -> p k n", p=128)
            n = 0
            for cg in range(4):
                wv = wbI[cg % 2].rearrange("p (k n) -> p k n", n=512)
                wk = "wbI%d" % (cg % 2)
                dma_w(wv, w_out_r[:, :, cg * 512:(cg + 1) * 512], [wk], wk)
                for t in range(8):
                    b = n % 2
                    xc = xch[n % 2]
                    xck = "xch%d" % (n % 2)
                    n += 1
                    dma(xc, xown[t * 128:(t + 1) * 128, cg * 512:(cg + 1) * 512], [ownk], [xck], xck)
                    for kc in range(16):
                        mm(PS[b][:, :], mixT3[:, kc, t * 128:(t + 1) * 128], wv[:, kc, :], kc == 0, kc == 15, ["mixT", wk], [PSK[b]])
                    tt(tmpI, PS[b][:, :], g1p[:, cg * 512:(cg + 1) * 512], ALU.mult, [PSK[b], "g1p"], ["tmpI"])
                    stt(r3[:, t, cg * 512:(cg + 1) * 512], xc, ALU_ALPHA, tmpI, ALU.mult, ALU.add, [xck, "tmpI"], ["r%d" % t])
            p.barrier()

            def layer_norm(x_ap, xk, g_ap, b_ap, gk, bk, sidx, junk, junkk):
                s_sum = st_[:, sidx * 4 + 0:sidx * 4 + 1]
                s_mean = st_[:, sidx * 4 + 1:sidx * 4 + 2]
                s_ss = st_[:, sidx * 4 + 2:sidx * 4 + 3]
                s_rstd = st_[:, sidx * 4 + 3:sidx * 4 + 4]
                sk_ = "st%d" % sidx
                V(lambda e: e.tensor_reduce(out=s_sum, in_=x_ap, axis=AX.X, op=ALU.add), [xk], [sk_])
                ts(s_mean, s_sum, 1.0 / 2048, None, ALU.mult, None, [sk_], [sk_])
                ts(x_ap, x_ap, s_mean, None, ALU.subtract, None, [xk, sk_], [xk])
                tt(junk, x_ap, x_ap, ALU.mult, [xk], [junkk])
                red(s_ss, junk, ALU.add, [junkk], [sk_])
                act(s_rstd, s_ss, AF.Sqrt, [sk_, "eps"], [sk_], scale=1.0 / 2048, bias=eps_t[:, 0:1])
                V(lambda e: e.reciprocal(out=s_rstd, in_=s_rstd), [sk_], [sk_])
                stt(x_ap, x_ap, s_rstd, g_ap, ALU.mult, ALU.mult, [xk, sk_, gk], [xk])
                tt(x_ap, x_ap, b_ap, ALU.add, [xk, bk], [xk])

            u2T3 = mixT3
            rt_ = A(16 * 8)
            for t in range(8):
                xk = "r%d" % t
                x1 = r3[:, t, :]
                layer_norm(x1, xk, lng, lnb, "lng", "lnb", t % 2, u2Tf, "u2Tf")
                dma(xmid[t * 128:(t + 1) * 128, :], x1, [xk], ["xmid"], "xmid_w")
            p.barrier()
            xts = [u2Tf, lng]
            make_uT(xmid, 2, 3, srck=["xmid"])
            for t in range(8):
                for kc in range(16):
                    hi_ = u2T3[:, kc, t * 128:(t + 1) * 128]
                    mm(PS[4][:, 0:16], hi_, wrh3[:, kc, :], kc == 0, False, ["uT", "wr_hi"], [PSK[4]])
                    mm(PS[4][:, 0:16], hi_, wrl3[:, kc, :], False, kc == 15, ["uT", "wr_lo"], [PSK[4]])
                lg = rt_[:, 0:16]
                mx = rt_[:, 16:17]
                ex = rt_[:, 32:48]
                pr6 = rt_[:, 48:72]
                gsc = rt_[:, 72:76]
                gmx = rt_[:, 76:77]
                goh = rt_[:, 80:84]
                eg = rt_[:, 84:100]
                m1 = rt_[:, 100:101]
                m2 = rt_[:, 101:102]
                eg2 = rt_[:, 104:120]
                msk = rt_[:, 120:128] if False else None
                cp(lg, PS[4][:, 0:16], [PSK[4]], ["rt"])
                if stop_after == "I3":
                    cp(gates3[:, t, :], lg, ["rt"], ["gates"])
                    continue
                V(lambda e: e.tensor_reduce(out=mx, in_=lg, axis=AX.X, op=ALU.max), ["rt"], ["rt"])
                ts(lg, lg, mx, None, ALU.subtract, None, ["rt"], ["rt"])
                act(ex, lg, AF.Exp, ["rt"], ["rt"])
                ex3 = ex.rearrange("p (g j) -> p g j", j=4)
                pr63 = pr6.rearrange("p (g k) -> p g k", k=6)
                kk = 0
                for a in range(4):
                    for bq in range(a + 1, 4):
                        tt(pr63[:, :, kk], ex3[:, :, a], ex3[:, :, bq], ALU.add, ["rt"], ["rt"])
                        kk += 1
                V(lambda e: e.tensor_reduce(out=gsc, in_=pr63, axis=AX.X, op=ALU.max), ["rt"], ["rt"])
                V(lambda e: e.tensor_reduce(out=gmx, in_=gsc, axis=AX.X, op=ALU.max), ["rt"], ["rt"])
                ts(goh, gsc, gmx, None, ALU.is_ge, None, ["rt"], ["rt"])
                tt(eg.rearrange("p (g j) -> p g j", j=4), ex3, goh.unsqueeze(2).to_broadcast([128, 4, 4]), ALU.mult, ["rt"], ["rt"])
                V(lambda e: e.tensor_reduce(out=m1, in_=eg, axis=AX.X, op=ALU.max), ["rt"], ["rt"])
                ts(eg2, eg, m1, None, ALU.is_lt, None, ["rt"], ["rt"])
                tt(eg2, eg2, eg, ALU.mult, ["rt"], ["rt"])
                V(lambda e: e.tensor_reduce(out=m2, in_=eg2, axis=AX.X, op=ALU.max), ["rt"], ["rt"])
                ts(eg2, eg, m2, None, ALU.is_ge, None, ["rt"], ["rt"])
                tt(eg2, eg2, eg, ALU.mult, ["rt"], ["rt"])
                tt(m1, m1, m2, ALU.add, ["rt"], ["rt"])
                V(lambda e: e.reciprocal(out=m1, in_=m1), ["rt"], ["rt"])
                ts(gates3[:, t, :], eg2, m1, None, ALU.mult, None, ["rt"], ["gates"])
            dump("gates", gates, ["gates"])
            p.barrier()
            if stop_after in ("I", "I1", "I2", "I3"):
                fin.append(("dma", "xmid_w") and p.res["xmid"][0])
                p.emit(fin)
                return nc

            top[0] = mix_top
            accm = A(8 * 2048)
            acc3 = accm.rearrange("p (t c) -> p t c", c=2048)
            hT = A(8 * 1024, BF16)
            hT3 = hT.rearrange("p (f t) -> p f t", t=1024)
            NB = 3
            wg = [A(16 * 256, BF16) for _ in range(2)]
            wu = [A(16 * 256, BF16) for _ in range(2)]
            wd = [A(8 * 512, BF16) for _ in range(2)]
            sgm = [A(512, BF16) for _ in range(2)]
            moe_top = top[0]
            mset(accm, 0.0, ["acc_%d" % t for t in range(8)])
            ng = 0
            nd_ = 0
            nps = 0
            for ex_i in range(16):
                eg_r = e_gate[ex_i].rearrange("(k p) f -> p k f", p=128)
                eu_r = e_up[ex_i].rearrange("(k p) f -> p k f", p=128)
                ed_r = e_down[ex_i].rearrange("(f p) c -> p f c", p=128)
                for fcp in range(4):
                    i = ng % 2
                    ng += 1
                    wgv = wg[i].rearrange("p (k f) -> p k f", f=256)
                    wuv = wu[i].rearrange("p (k f) -> p k f", f=256)
                    dma_w(wgv, eg_r[:, :, fcp * 256:(fcp + 1) * 256], ["wg%d" % i], "wg%d" % i)
                    dma_w(wuv, eu_r[:, :, fcp * 256:(fcp + 1) * 256], ["wu%d" % i], "wu%d" % i)
                    for sub in range(2):
                        fc = fcp * 2 + sub
                        fsl = slice(sub * 128, (sub + 1) * 128)
                        for th_ in range(2):
                            tsl = slice(th_ * 512, (th_ + 1) * 512)
                            bg, bu = 0 + (nps % 2), 2 + (nps % 2)
                            s_ = sgm[nps % 2]
                            sk_ = "sgm%d" % (nps % 2)
                            nps += 1
                            for kc in range(16):
                                mm(PS[bg][:, :], wgv[:, kc, fsl], u2T3[:, kc, tsl], kc == 0, kc == 15, ["wg%d" % i, "uT"], [PSK[bg]])
                            for kc in range(16):
                                mm(PS[bu][:, :], wuv[:, kc, fsl], u2T3[:, kc, tsl], kc == 0, kc == 15, ["wu%d" % i, "uT"], [PSK[bu]])
                            act(s_, PS[bg][:, :], AF.Silu, [PSK[bg]], [sk_])
                            tt(hT3[:, fc, tsl], s_, PS[bu][:, :], ALU.mult, [sk_, PSK[bu]], ["hT"])
                for cg in range(4):
                    i = nd_ % 2
                    wdv = wd[i].rearrange("p (f c) -> p f c", c=512)
                    dma_w(wdv, ed_r[:, :, cg * 512:(cg + 1) * 512], ["wd%d" % i], "wd%d" % i, nsplit=2)
                    for t in range(8):
                        b = 4 + (nd_ * 8 + t) % 4
                        for fc in range(8):
                            mm(PS[b][:, :], hT3[:, fc, t * 128:(t + 1) * 128], wdv[:, fc, :], fc == 0, fc == 7, ["hT", "wd%d" % i], [PSK[b]])
                        stt(acc3[:, t, cg * 512:(cg + 1) * 512], PS[b][:, :], gates3[:, t, ex_i:ex_i + 1], acc3[:, t, cg * 512:(cg + 1) * 512], ALU.mult, ALU.add, [PSK[b], "gates", "acc_%d" % t], ["acc_%d" % t])
                    nd_ += 1
            p.barrier()
            top[0] = mix_top + 8 * 2048
            lng2 = A(2048)
            lnb2 = A(2048)
            xm = [A(2048) for _ in range(2)]
            junk = A(2048)
            st_ = A(32)
            dma(lng2, lnp[:, 4096:6144], [], ["lng2"], "lng2")
            dma(lnb2, lnp[:, 6144:8192], [], ["lnb2"], "lnb2")
            for t in range(8):
                xk = "acc_%d" % t
                a_t = acc3[:, t, :]
                xmk = "xm%d" % (t % 2)
                dma(xm[t % 2], xmid[t * 128:(t + 1) * 128, :], ["xmid"], [xmk], xmk)
                tt(a_t, a_t, g2p, ALU.mult, [xk, "g2p"], [xk])
                stt(a_t, xm[t % 2], ALU_ALPHA, a_t, ALU.mult, ALU.add, [xmk, xk], [xk])
                layer_norm(a_t, xk, lng2, lnb2, "lng2", "lnb2", t % 2, junk, "junk")
                fin.append(dma(xout[t * 128:(t + 1) * 128, :], a_t, [xk], [outk], "xout"))
            p.barrier()

        xts = None
        run_pass(0, 0, xA, xA, S1, True, "xA", "xA", "S1", noprefix=True)
        run_pass(0, 1, xA, xB, S2, False, "xA", "xB", "S2")
        run_pass(1, 1, S1, S2, xout_f, True, "S1", "S2", "xoutf")
        p.emit(fin)
    return nc


ALU_ALPHA = float(ALPHA)


def _consts():
    c = np.zeros((128, 3072), np.float32)
    c[:, 0:128] = np.eye(128, dtype=np.float32)
    c[:, 128:256] = np.arange(128, dtype=np.float32)[None, :]
    c[:, 256:384] = np.triu(np.ones((128, 128), np.float32))
    c[:, 384:512] = 1.0
    zt = np.zeros((128, 1024), np.float32)
    q = np.arange(128)[:, None]
    s = np.arange(128)[None, :]
    zt[:, 896:1024] = np.where(s <= q, 0.0, NEG)
    c[:, 1024:2048] = zt
    rot, half = 32, 16
    c[:, 2048:2064] = (500000.0 ** (-np.arange(half, dtype=np.float32) * 2.0 / rot))[None, :]
    rot, half = 16, 8
    c[:, 2064:2072] = (500000.0 ** (-np.arange(half, dtype=np.float32) * 2.0 / rot))[None, :]
    return c


def _layer_inputs(inp, l):
    f = np.float32
    rep = lambda v: np.ascontiguousarray(np.broadcast_to(np.asarray(v, f)[None, :], (128, v.shape[0])))
    d = {}
    d["b_ada"] = rep(inp["b_ada"][l])

    def st_layout(a):
        return np.ascontiguousarray(a.reshape(16, 2, 64).transpose(1, 2, 0).reshape(128, 16))
    lam_re = st_layout(inp["ssm_lam_re"][l])
    lam_im = st_layout(inp["ssm_lam_im"][l])
    lstep = st_layout(np.broadcast_to(inp["ssm_log_step"][l][:, None], (32, 64)))
    d["ssm_small"] = np.concatenate([lam_re, lam_im, lstep], axis=1).astype(f)

    def b_layout(a):
        return a.reshape(16, 2, 64, 16).transpose(1, 2, 0, 3).reshape(128, 256)

    def c_layout(a):
        return a.reshape(16, 2, 16, 64).transpose(1, 3, 0, 2).reshape(128, 256)
    d["ssm_bc"] = np.ascontiguousarray(np.concatenate(
        [b_layout(inp["ssm_b_re"][l]), b_layout(inp["ssm_b_im"][l]), c_layout(inp["ssm_c_re"][l]), c_layout(inp["ssm_c_im"][l])], axis=1)).astype(f)
    pk = lambda v: np.asarray(v, f).reshape(4, 128).T
    d["vecs"] = np.ascontiguousarray(np.concatenate([pk(inp["ssm_d"][l]), pk(inp["ssm_b_glu"][l]), pk(inp["pool_scale"][l])], axis=1))
    d["pool_w"] = np.ascontiguousarray(inp["pool_w"][l].reshape(512, 128))
    d["lnp"] = np.ascontiguousarray(np.concatenate([rep(inp["ln1_g"][l]), rep(inp["ln1_b"][l]), rep(inp["ln2_g"][l]), rep(inp["ln2_b"][l])], axis=1))
    return d


def _shared_inputs(inp):
    f = np.float32
    per = [_layer_inputs(inp, l) for l in range(2)]
    d = {k: np.ascontiguousarray(np.stack([per[0][k], per[1][k]], axis=0)) for k in per[0]}
    for k, src in (("w_ada", "w_ada"), ("w_in", "w_in"), ("w_out", "w_out"), ("w_glu", "ssm_w_glu"),
                   ("e_gate", "e_gate"), ("e_up", "e_up"), ("e_down", "e_down")):
        d[k] = np.ascontiguousarray(np.asarray(inp[src], f))
    d["w_router"] = np.ascontiguousarray(np.asarray(inp["w_router"], f).reshape(16, 128, 16).transpose(1, 0, 2).reshape(128, 256))
    d["cst"] = _consts()
    return d


def _cfg(pos_b, h):
    f = np.float32
    own = pos_b[h * 1024:(h + 1) * 1024].reshape(8, 128).T
    pre = pos_b[0:1024].reshape(8, 128).T if h == 1 else np.zeros((128, 8), np.int32)
    pos_in = np.concatenate([pre, own], axis=1).astype(np.int32)
    kbias = np.full((128, 1024), 0.0 if h == 1 else NEG, f)
    pc = np.zeros((128, 80), f)
    pc[:, 0] = float(h)
    for g, win in enumerate((2, 4, 8, 16)):
        t = np.arange(16) + h * 1024
        pc[:, 16 + g * 16:32 + g * 16] = (1.0 / np.minimum(t + 1, win))[None, :]
    return pos_in, kbias, pc


def _core_inputs(inp, core):
    b, h = core // 2, core % 2
    f = np.float32
    d = {}
    xb = np.asarray(inp["x"][b], f)
    d["xA"] = np.ascontiguousarray(xb[0:1024])
    d["xB"] = np.ascontiguousarray(xb[h * 1024:(h + 1) * 1024])
    d["c_in"] = np.ascontiguousarray(np.asarray(inp["c"][b], f).reshape(16, 128).T)
    pos = np.asarray(inp["positions"][b], np.int32)
    c0 = _cfg(pos, 0)
    c1 = _cfg(pos, h)
    d["pos_in"] = np.ascontiguousarray(np.stack([c0[0], c1[0]], 0))
    d["kbias"] = np.ascontiguousarray(np.stack([c0[1], c1[1]], 0))
    d["pcore"] = np.ascontiguousarray(np.stack([c0[2], c1[2]], 0))
    return d


_NC_CACHE = {}


def kernel(**inputs):
    inp = {k: np.asarray(v) for k, v in inputs.items()}
    if "prog" not in _NC_CACHE:
        _NC_CACHE["prog"] = build_program()
    nc = _NC_CACHE["prog"]
    shared = _shared_inputs(inp)
    in_maps = []
    for core in range(8):
        d = dict(shared)
        d.update(_core_inputs(inp, core))
        in_maps.append(d)
    res = run_bass_kernel_spmd(nc, in_maps, core_ids=list(range(8)))
    out = np.empty((4, 2048, 2048), np.float32)
    for core in range(8):
        b, h = core // 2, core % 2
        out[b, h * 1024:(h + 1) * 1024] = res.results[core]["xout"]
    return out
```

```python
import math
import contextlib
import numpy as np
import concourse.bass as bass
import concourse.mybir as mybir
from concourse.bass_utils import run_bass_kernel_spmd

F32 = mybir.dt.float32
BF16 = mybir.dt.bfloat16
I32 = mybir.dt.int32
AF = mybir.ActivationFunctionType
ALU = mybir.AluOpType
AX = mybir.AxisListType

ENGINES = ("sync", "scalar", "vector", "gpsimd", "tensor")
ALPHA = (2.0 * 2) ** 0.25
LN_EPS = 1e-5
PI = math.pi
NEG = -1e30

OQ, OK_, OV, OIQ, OIK, OIW, OUS, OUP = 0, 1024, 1280, 1536, 2048, 2112, 2120, 2632


class Prog:
    def __init__(self, nc):
        self.nc = nc
        self.ops = {e: [] for e in ENGINES}
        self.res = {}
        self.streams = {}
        self.pending = {e: {} for e in ENGINES}

    def _add(self, engine, fn, reads, writes, dma_key=None):
        skey = ("dma", dma_key) if dma_key is not None else ("eng", engine)
        st = self.streams.setdefault(skey, [])
        deps = dict(self.pending[engine])
        self.pending[engine] = {}

        def need(tok):
            if tok is None:
                return
            k, i = tok
            if k == skey and engine == "tensor" and dma_key is None:
                return
            if k[0] == "dma":
                i = len(self.streams[k]) - 1
                if k == skey:
                    i = len(st) - 1
            if i >= 0 and deps.get(k, -1) < i:
                deps[k] = i

        for r in reads:
            ent = self.res.get(r)
            if ent is not None:
                need(ent[0])
        for w in writes:
            ent = self.res.get(w)
            if ent is not None:
                need(ent[0])
                for t in ent[1]:
                    need(t)
        op = dict(fn=fn, deps=deps, skey=skey, idx=len(st), marked=False)
        st.append(op)
        self.ops[engine].append(op)
        tok = (skey, op["idx"])
        for w in writes:
            self.res[w] = [tok, []]
        for r in reads:
            if r in writes:
                continue
            ent = self.res.setdefault(r, [None, []])
            ent[1].append(tok)
        return tok

    def op(self, engine, fn, reads=(), writes=()):
        return self._add(engine, fn, reads, writes)

    def dma(self, engine, fn, reads=(), writes=(), key=None):
        return self._add(engine, fn, reads, writes, dma_key=key)

    def barrier(self):
        last = {k: len(st) - 1 for k, st in self.streams.items() if st}
        for e in ENGINES:
            for k, i in last.items():
                if self.pending[e].get(k, -1) < i:
                    self.pending[e][k] = i

    def emit(self, final_waits=()):
        nc = self.nc
        for e in ENGINES:
            waited = {}
            for op in self.ops[e]:
                nd = {}
                for k, i in op["deps"].items():
                    if waited.get(k, -1) >= i:
                        continue
                    waited[k] = i
                    nd[k] = i
                    self.streams[k][i]["marked"] = True
                op["deps"] = nd
        fin = {}
        for k, i in final_waits:
            if k[0] == "dma":
                i = len(self.streams[k]) - 1
            fin[k] = max(fin.get(k, -1), i)
        for k, st in self.streams.items():
            if st:
                fin[k] = len(st) - 1
        for k, i in fin.items():
            self.streams[k][i]["marked"] = True
        for k, st in self.streams.items():
            v = 0
            for op in st:
                if k[0] == "dma":
                    v += 16
                    op["inc"] = 16
                elif op["marked"]:
                    v += 1
                    op["inc"] = 1
                else:
                    op["inc"] = 0
                op["val"] = v
        with contextlib.ExitStack() as es:
            sems = {}
            for n, k in enumerate(self.streams):
                sems[k] = es.enter_context(nc.semaphore("s%d" % n))
            block = es.enter_context(nc.Block())

            def run(eng, ename):
                for op in self.ops[ename]:
                    for k, i in op["deps"].items():
                        eng.wait_ge(sems[k], self.streams[k][i]["val"])
                    ins = op["fn"](eng)
                    if op["inc"]:
                        ins.then_inc(sems[op["skey"]], op["inc"])
                if ename == "sync":
                    for k, i in fin.items():
                        eng.wait_ge(sems[k], self.streams[k][i]["val"])

            @block.sync
            def _(eng):
                run(eng, "sync")

            @block.scalar
            def _(eng):
                run(eng, "scalar")

            @block.vector
            def _(eng):
                run(eng, "vector")

            @block.gpsimd
            def _(eng):
                run(eng, "gpsimd")

            @block.tensor
            def _(eng):
                run(eng, "tensor")


SB_WORDS = 48400


def build_program(stop_after=None, dbg=()):
    nc = bass.Bass("TRN2", target_bir_lowering=False)

    def din(name, shape, dt=F32):
        return nc.dram_tensor(name, list(shape), dt, kind="ExternalInput").ap()

    xA = din("xA", [1024, 2048])
    xB = din("xB", [1024, 2048])
    c_in = din("c_in", [128, 16])
    pos_in2 = din("pos_in", [2, 128, 16], I32)
    cst = din("cst", [128, 3072])
    kbias2 = din("kbias", [2, 128, 1024])
    pcore2 = din("pcore", [2, 128, 80])
    w_ada_a = din("w_ada", [2, 2048, 12288])
    b_ada_a = din("b_ada", [2, 128, 12288])
    w_in_a = din("w_in", [2, 2048, 3144])
    w_out_a = din("w_out", [2, 2048, 2048])
    ssm_small_a = din("ssm_small", [2, 128, 48])
    ssm_bc_a = din("ssm_bc", [2, 128, 4 * 256])
    vecs_a = din("vecs", [2, 128, 12])
    w_glu_a = din("w_glu", [2, 512, 512])
    pool_w_a = din("pool_w", [2, 512, 128])
    lnp_a = din("lnp", [2, 128, 4 * 2048])
    w_router = din("w_router", [128, 256])
    e_gate_a = din("e_gate", [2, 16, 2048, 1024])
    e_up_a = din("e_up", [2, 16, 2048, 1024])
    e_down_a = din("e_down", [2, 16, 1024, 2048])
    xout_f = nc.dram_tensor("xout", [1024, 2048], F32, kind="ExternalOutput").ap()
    xmid = nc.dram_tensor("xmid_i", [1024, 2048], F32, kind="Internal").ap()
    S1 = nc.dram_tensor("s1_i", [1024, 2048], F32, kind="Internal").ap()
    S2 = nc.dram_tensor("s2_i", [1024, 2048], F32, kind="Internal").ap()
    KS = nc.dram_tensor("ks_i", [128, 2048], BF16, kind="Internal").ap()
    VS = nc.dram_tensor("vs_i", [128, 2048], BF16, kind="Internal").ap()
    IS = nc.dram_tensor("is_i", [128, 1024], BF16, kind="Internal").ap()
    UPS = nc.dram_tensor("ups_i", [128, 512], BF16, kind="Internal").ap()
    CS = nc.dram_tensor("cs_i", [128, 32], F32, kind="Internal").ap()
    dbg_out = {}

    es = contextlib.ExitStack()
    with es:
        SB = es.enter_context(nc.sbuf_tensor("SB", [128, SB_WORDS], F32))
        PS = [es.enter_context(nc.psum_tensor("ps%d" % i, [128, 512], F32)) for i in range(8)]
        PSK = ["ps%d" % i for i in range(8)]
        PSb = [t[:, :].bitcast(BF16) for t in PS]
        p = Prog(nc)
        top = [0]
        fin = []

        def A(n, dt=F32):
            w = n if dt != BF16 else (n + 1) // 2
            assert top[0] + w <= SB_WORDS, ("SBUF overflow", top[0], w)
            v = SB[:, top[0]:top[0] + w]
            top[0] += w
            return v if dt == F32 else v.bitcast(dt)

        def V(fn, r, w):
            return p.op("vector", fn, reads=r, writes=w)

        def S(fn, r, w):
            return p.op("scalar", fn, reads=r, writes=w)

        def mm(out, lhsT, rhs, start, stop, r, w):
            return p.op("tensor", lambda e: e.matmul(out, lhsT=lhsT, rhs=rhs, start=start, stop=stop), reads=r, writes=w)

        def tr(out, in_, ident, r, w):
            return p.op("tensor", lambda e: e.transpose(out=out, in_=in_, identity=ident), reads=r, writes=w)

        def dma(out, in_, r, w, key, eng="sync", slow=False):
            if slow:
                return p.dma(eng, lambda e: e.dma_start(out=out, in_=in_, allow_slow_non_contiguous=True), reads=r, writes=w, key=key)
            return p.dma(eng, lambda e: e.dma_start(out=out, in_=in_), reads=r, writes=w, key=key)

        def dma_w(out3, in3, w, key, nsplit=4):
            n = out3.shape[1]
            step = max(1, n // nsplit)
            tok = None
            for a in range(0, n, step):
                tok = dma(out3[:, a:a + step, :], in3[:, a:a + step, :], [], w, key, eng="gpsimd")
            return tok

        def dump(name, ap, r):
            if name in dbg_out:
                fin.append(dma(dbg_out[name], ap, r, [], "dbg_" + name))

        def act(out, in_, func, r, w, **kw):
            return S(lambda e: e.activation(out=out, in_=in_, func=func, **kw), r, w)

        def tt(out, in0, in1, op, r, w):
            return V(lambda e: e.tensor_tensor(out=out, in0=in0, in1=in1, op=op), r, w)

        def ts(out, in0, s1, s2, op0, op1, r, w):
            if op1 is None:
                return V(lambda e: e.tensor_scalar(out=out, in0=in0, scalar1=s1, scalar2=None, op0=op0), r, w)
            return V(lambda e: e.tensor_scalar(out=out, in0=in0, scalar1=s1, scalar2=s2, op0=op0, op1=op1), r, w)

        def stt(out, in0, scalar, in1, op0, op1, r, w):
            return V(lambda e: e.scalar_tensor_tensor(out=out, in0=in0, scalar=scalar, in1=in1, op0=op0, op1=op1), r, w)

        def cp(out, in_, r, w):
            return V(lambda e: e.tensor_copy(out=out, in_=in_), r, w)

        def G(fn, r, w):
            return p.op("gpsimd", fn, reads=r, writes=w)

        def gtt(out, in0, in1, op, r, w):
            return G(lambda e: e.tensor_tensor(out=out, in0=in0, in1=in1, op=op), r, w)

        def gstt(out, in0, scalar, in1, op0, op1, r, w):
            return G(lambda e: e.scalar_tensor_tensor(out=out, in0=in0, scalar=scalar, in1=in1, op0=op0, op1=op1), r, w)

        def vmax(out, in_, r, w):
            return V(lambda e: e.max(out=out, in_=in_), r, w)

        def vmr(out, rep_, vals, r, w):
            return V(lambda e: e.match_replace(out=out, in_to_replace=rep_, in_values=vals, imm_value=NEG), r, w)

        def red(out, in_, op, r, w):
            return V(lambda e: e.tensor_reduce(out=out, in_=in_, axis=AX.X, op=op), r, w)

        def recip(out, in_, r, w):
            return V(lambda e: e.reciprocal(out=out, in_=in_), r, w)

        def mset(out, val, w):
            return V(lambda e: e.memset(out, val), [], w)

        cst_sb = A(256 + 1024 + 32)
        ident_f = cst_sb[:, 0:128]
        iota_f = cst_sb[:, 128:256]
        ztri = cst_sb[:, 256:1280]
        invf = cst_sb[:, 1280:1304]
        dma(cst_sb[:, 0:256], cst[:, 0:256], [], ["cst"], "cst")
        dma(cst_sb[:, 256:1280], cst[:, 1024:2048], [], ["cst"], "cst")
        dma(cst_sb[:, 1280:1312], cst[:, 2048:2080], [], ["cst"], "cst")
        cstb = A(3 * 128, BF16)
        ident_b = cstb[:, 0:128]
        tri_b = cstb[:, 128:256]
        ones_b = cstb[:, 256:384]
        dma(ident_b, cst[:, 0:128], [], ["cstb"], "cstb", eng="gpsimd")
        dma(tri_b, cst[:, 256:384], [], ["cstb"], "cstb", eng="gpsimd")
        dma(ones_b, cst[:, 384:512], [], ["cstb"], "cstb", eng="gpsimd")
        pc_sb = A(80)
        pflag = pc_sb[:, 0:1]
        invcnt16 = pc_sb[:, 16:80].rearrange("p (g t) -> p g t", t=16)
        g1p = A(2048)
        g2p = A(2048)
        modp = A(64)
        modp3 = modp.rearrange("p (v k) -> p v k", k=16)
        gates = A(128)
        gates3 = gates.rearrange("p (t e) -> p t e", e=16)
        vec_sb = A(12)
        eps_t = A(1)
        V(lambda e: e.memset(eps_t, LN_EPS), [], ["eps"])
        perm_top = top[0]

        def run_pass(l, cfg, xpre, xown, xout, do_ada, prek, ownk, outk, noprefix=False, reuse=False):
            w_ada, b_ada, w_in, w_out = w_ada_a[l], b_ada_a[l], w_in_a[l], w_out_a[l]
            ssm_small, ssm_bc, vecs, w_glu, pool_w, lnp = ssm_small_a[l], ssm_bc_a[l], vecs_a[l], w_glu_a[l], pool_w_a[l], lnp_a[l]
            e_gate, e_up, e_down = e_gate_a[l], e_up_a[l], e_down_a[l]
            pos_in, kbias_in, pcore = pos_in2[cfg], kbias2[cfg], pcore2[cfg]
            dma(pc_sb, pcore[:, :], [], ["pc"], "pc")
            dma(vec_sb, vecs[:, :], [], ["vec"], "vec")
            if do_ada:
                phase_A(w_ada, b_ada)
            phase_rest(w_in, w_out, ssm_small, ssm_bc, w_glu, pool_w, lnp, e_gate, e_up, e_down, pos_in, kbias_in, xpre, xown, xout, prek, ownk, outk, noprefix, reuse)

        def phase_A(w_ada, b_ada):
            top[0] = perm_top
            c_sb = A(16)
            cond = A(16)
            condrep = A(16 * 128, BF16)
            condrep3 = condrep.rearrange("p (k j) -> p k j", j=128)
            condf = A(16 * 128)
            condf3 = condf.rearrange("p (k j) -> p k j", j=128)
            ada = A(12288)
            wbA = [A(16 * 512, BF16) for _ in range(2)]
            wbF = [A(16 * 512) for _ in range(2)]
            tmpA = wbF[0][:, 0:2048]
            tmpA3 = tmpA.rearrange("p (k j) -> p k j", j=128)
            dma(c_sb, c_in[:, :], [], ["c_sb"], "c_sb")
            act(cond, c_sb, AF.Silu, ["c_sb"], ["cond"])
            cp(condrep3, cond.unsqueeze(2).to_broadcast([128, 16, 128]), ["cond"], ["condrep"])
            cp(condf3, cond.unsqueeze(2).to_broadcast([128, 16, 128]), ["cond"], ["condf"])
            dma(ada, b_ada[:, :], [], ["ada"], "ada")
            w_ada_r = w_ada.rearrange("(k p) n -> p k n", p=128)
            for nb in range(24):
                j = (nb // 2) % 2
                src = w_ada_r[:, :, nb * 512:(nb + 1) * 512]
                if nb % 2 == 0:
                    wv = wbA[j].rearrange("p (k n) -> p k n", n=512)
                    wk = "wbA%d" % j
                    dma_w(wv, src, [wk], wk)
                    bank, bk, lhs, lk = PS[j], PSK[j], condrep3, "condrep"
                else:
                    wv = wbF[j].rearrange("p (k n) -> p k n", n=512)
                    wk = "wbF%d" % j
                    for q, eng in enumerate(("sync", "scalar")):
                        for a in range(q * 8, q * 8 + 8, 4):
                            dma(wv[:, a:a + 4, :], src[:, a:a + 4, :], [], [wk], wk + eng, eng=eng)
                    bank, bk, lhs, lk = PS[2 + j], PSK[2 + j], condf3, "condf"
                for kc in range(16):
                    mm(bank[:, :], lhs[:, kc, :], wv[:, kc, :], kc == 0, kc == 15, [lk, wk], [bk])
                tt(ada[:, nb * 512:(nb + 1) * 512], bank[:, :], ada[:, nb * 512:(nb + 1) * 512], ALU.add, [bk, "ada"], ["ada"])
            p.barrier()
            ts(g1p, ada[:, 2 * 2048:3 * 2048], 1.0, None, ALU.add, None, ["ada"], ["g1p"])
            ts(g2p, ada[:, 5 * 2048:6 * 2048], 1.0, None, ALU.add, None, ["ada"], ["g2p"])
            for slot, idx in enumerate((0, 1, 3, 4)):
                tt(tmpA3, ada[:, idx * 2048:(idx + 1) * 2048].rearrange("p (k j) -> p k j", j=128),
                   ident_f.unsqueeze(1).to_broadcast([128, 16, 128]), ALU.mult, ["ada", "cst"], ["tmpA"])
                V(lambda e, slot=slot: e.tensor_reduce(out=modp3[:, slot, :], in_=tmpA3, axis=AX.X, op=ALU.add), ["tmpA"], ["modp"])
            ts(modp3[:, 1, :], modp3[:, 1, :], 1.0, None, ALU.add, None, ["modp"], ["modp"])
            ts(modp3[:, 3, :], modp3[:, 3, :], 1.0, None, ALU.add, None, ["modp"], ["modp"])
            dump("ada", ada, ["ada"])
            dump("modp", modp, ["modp"])
            p.barrier()
            if stop_after == "A":
                p.emit(fin)
                return nc


        def phase_rest(w_in, w_out, ssm_small, ssm_bc, w_glu, pool_w, lnp, e_gate, e_up, e_down, pos_in, kbias_in, xpre, xown, xout, prek, ownk, outk, noprefix, reuse):
            nonlocal xts
            top[0] = perm_top
            mixT = A(16 * 1024, BF16)
            mixT3 = mixT.rearrange("p (k t) -> p k t", t=1024)
            uT3 = mixT3
            mix_top = top[0]
            usT = A(4 * 2048, BF16)
            usT3 = usT.rearrange("p (c t) -> p c t", t=2048)
            upT = A(4 * 1152, BF16)
            upT3 = upT.rearrange("p (g t) -> p g t", t=1152)
            l1_top = top[0]
            qT = A(8 * 1024, BF16)
            qT3 = qT.rearrange("p (h t) -> p h t", t=1024)
            kT = A(2 * 2048, BF16)
            kT3 = kT.rearrange("p (h t) -> p h t", t=2048)
            v_sb = A(16 * 256, BF16)
            v_sb4 = v_sb.rearrange("p (b h d) -> p b h d", h=2, d=128)
            ikT2 = A(2048, BF16)
            iqT = A(4 * 1024, BF16)
            iqT3 = iqT.rearrange("p (j t) -> p j t", t=1024)
            iw_sb = A(64)
            iw3 = iw_sb.rearrange("p (t h) -> p t h", h=8)
            cosT = A(16 * 24)
            sinT = A(16 * 24)
            cos3 = cosT.rearrange("p (t f) -> p t f", f=24)
            sin3 = sinT.rearrange("p (t f) -> p t f", f=24)
            att_top = top[0]
            wb = [A(16 * 512, BF16) for _ in range(2)]
            xts = [A(2048) for _ in range(2)]
            qt = A(512)
            kt = A(256)
            rtmp = A(4 * 64)
            pos_i = A(16, I32)
            pos_f = A(16)
            ang = A(16 * 24)
            ang3 = ang.rearrange("p (t f) -> p t f", f=24)
            sc_kf = A(16 * 24)
            sc_ki = A(16 * 24, I32)
            sc_t = A(16 * 24)

            def sin_of(out, in_, addc, kf, ki, t, rk, wk, kfk, kik, tk):
                ts(t, in_, addc, None, ALU.add, None, rk, [tk])
                ts(kf, t, 1.0 / (2 * PI), None, ALU.mult, None, [tk], [kfk])
                cp(ki, kf, [kfk], [kik])
                cp(kf, ki, [kik], [kfk])
                stt(t, kf, -2 * PI, t, ALU.mult, ALU.add, [kfk, tk], [tk])
                ts(kf, t, PI, -2 * PI, ALU.is_gt, ALU.mult, [tk], [kfk])
                tt(t, t, kf, ALU.add, [tk, kfk], [tk])
                ts(kf, t, -PI, 2 * PI, ALU.is_lt, ALU.mult, [tk], [kfk])
                tt(t, t, kf, ALU.add, [tk, kfk], [tk])
                act(out, t, AF.Sin, [tk], [wk])

            dma(pos_i, pos_in[:, :], [], ["pos_i"], "pos_i")
            cp(pos_f, pos_i, ["pos_i"], ["pos_f"])
            tt(ang3, pos_f.unsqueeze(2).to_broadcast([128, 16, 24]), invf.unsqueeze(1).to_broadcast([128, 16, 24]), ALU.mult, ["pos_f", "cst"], ["ang"])
            sin_of(sinT, ang, 0.0, sc_kf, sc_ki, sc_t, ["ang"], "sinT", "sc_kf", "sc_ki", "sc_t")
            sin_of(cosT, ang, PI / 2, sc_kf, sc_ki, sc_t, ["ang"], "cosT", "sc_kf", "sc_ki", "sc_t")
            dump("cosT", cosT, ["cosT"])

            w_in_r = w_in.rearrange("(k p) n -> p k n", p=128)
            wcnt = [0]

            def load_w(col0, ncols):
                i = wcnt[0] % 2
                wcnt[0] += 1
                wv = wb[i].rearrange("p (k n) -> p k n", n=512)
                dma_w(wv[:, :, 0:ncols], w_in_r[:, :, col0:col0 + ncols], ["wb%d" % i], "wb%d" % i)
                return wv, "wb%d" % i

            def rope(x3, nh, half, tile, foff, rk):
                cs = cos3[:, tile, foff:foff + half].unsqueeze(1).to_broadcast([128, nh, half])
                sn = sin3[:, tile, foff:foff + half].unsqueeze(1).to_broadcast([128, nh, half])
                x1 = x3[:, :, 0:half]
                x2 = x3[:, :, half:2 * half]
                t = [rtmp[:, j * 64:j * 64 + nh * half].rearrange("p (h f) -> p h f", f=half) for j in range(4)]
                tt(t[0], x1, cs, ALU.mult, [rk, "cosT"], ["rt0"])
                tt(t[1], x2, sn, ALU.mult, [rk, "sinT"], ["rt1"])
                tt(t[2], x2, cs, ALU.mult, [rk, "cosT"], ["rt2"])
                tt(t[3], x1, sn, ALU.mult, [rk, "sinT"], ["rt3"])
                tt(x1, t[0], t[1], ALU.subtract, ["rt0", "rt1"], [rk])
                tt(x2, t[2], t[3], ALU.add, ["rt2", "rt3"], [rk])

            def make_uT(xsrc, slot_sh, slot_sc, srck=()):
                for t in range(8):
                    xt = xts[t % 2]
                    xk = "xt%d" % (t % 2)
                    dma(xt, xsrc[t * 128:(t + 1) * 128, :], list(srck), [xk], xk)
                    for g in range(4):
                        b = 2 + (g % 2)
                        for j in range(4):
                            kc = g * 4 + j
                            tr(PS[b][:, j * 128:(j + 1) * 128], xt[:, kc * 128:(kc + 1) * 128], ident_f, [xk, "cst"], [PSK[b]])
                        for j in range(4):
                            kc = g * 4 + j
                            o = uT3[:, kc, t * 128:(t + 1) * 128]
                            i_ = PS[b][:, j * 128:(j + 1) * 128]
                            if j % 2 == 0:
                                act(o, i_, AF.Identity, [PSK[b], "modp"], ["uT"], scale=modp3[:, slot_sc, kc:kc + 1], bias=modp3[:, slot_sh, kc:kc + 1])
                            else:
                                ts(o, i_, modp3[:, slot_sc, kc:kc + 1], modp3[:, slot_sh, kc:kc + 1], ALU.mult, ALU.add, [PSK[b], "modp"], ["uT"])

            def proj_tok(col0, ncols, consume):
                wv, wk = load_w(col0, ncols)
                for t in range(8):
                    b = t % 2
                    for kc in range(16):
                        mm(PS[b][:, 0:ncols], uT3[:, kc, t * 128:(t + 1) * 128], wv[:, kc, 0:ncols], kc == 0, kc == 15, ["uT", wk], [PSK[b]])
                    consume(t, PS[b], PSK[b])

            def proj_T(col0, nchunks, consume, blocks):
                wv, wk = load_w(col0, nchunks * 128)
                n = 0
                for cc in range(nchunks):
                    for (t0, tn) in blocks:
                        b = n % 2
                        n += 1
                        for kc in range(16):
                            mm(PS[b][:, 0:tn], wv[:, kc, cc * 128:(cc + 1) * 128], uT3[:, kc, t0:t0 + tn], kc == 0, kc == 15, ["uT", wk], [PSK[b]])
                        consume(cc, t0, tn, PS[b], PSK[b])

            def kv_consumer(tile_base):
                def f(t, bank, bk):
                    gt = tile_base + t
                    act(kt, bank[:, 0:256], AF.Copy, [bk], ["kt"])
                    act(v_sb4[:, gt, :, :], bank[:, 256:512].rearrange("p (h d) -> p h d", d=128), AF.Copy, [bk], ["v_sb"])
                    rope(kt.rearrange("p (h d) -> p h d", d=128), 2, 16, gt, 0, "kt")
                    for h in range(2):
                        tr(PS[4][:, h * 128:(h + 1) * 128], kt[:, h * 128:(h + 1) * 128], ident_f, ["kt", "cst"], [PSK[4]])
                    cp(kT3[:, :, gt * 128:(gt + 1) * 128], PS[4][:, 0:256].rearrange("p (h t) -> p h t", t=128), [PSK[4]], ["kT"])
                return f

            def ik_consumer(tile_base, own):
                def f(t, bank, bk):
                    gt = tile_base + t
                    act(kt[:, 0:64], bank[:, 0:64], AF.Copy, [bk], ["kt"])
                    if own:
                        act(iw3[:, t, :], bank[:, 64:72], AF.Copy, [bk], ["iw"])
                    rope(kt[:, 0:64].rearrange("p (h d) -> p h d", d=64), 1, 8, gt, 16, "kt")
                    cp(kt[:, 64:128], kt[:, 0:64], ["kt"], ["kt"])
                    tr(PS[5][:, 0:128], kt[:, 0:128], ident_f, ["kt", "cst"], [PSK[5]])
                    cp(ikT2[:, gt * 128:(gt + 1) * 128], PS[5][:, 0:128], [PSK[5]], ["ikT2"])
                return f

            def us_consumer(tok_base, prefix):
                def f(cc, t0, tn, bank, bk):
                    o = usT3[:, cc, tok_base + t0:tok_base + t0 + tn]
                    if prefix:
                        ts(o, bank[:, 0:tn], pflag, None, ALU.mult, None, [bk, "pc"], ["usT"])
                    else:
                        act(o, bank[:, 0:tn], AF.Copy, [bk], ["usT"])
                return f

            def up_consumer(prefix):
                def f(cc, t0, tn, bank, bk):
                    if prefix:
                        ts(upT3[:, cc, 0:128], bank[:, 0:tn], pflag, None, ALU.mult, None, [bk, "pc"], ["upT"])
                    else:
                        act(upT3[:, cc, 128 + t0:128 + t0 + tn], bank[:, 0:tn], AF.Copy, [bk], ["upT"])
                return f

            def q_consumer(g):
                def f(t, bank, bk):
                    act(qt, bank[:, :], AF.Copy, [bk], ["qt"])
                    rope(qt.rearrange("p (h d) -> p h d", d=128), 4, 16, 8 + t, 0, "qt")
                    for h in range(4):
                        tr(PS[6][:, h * 128:(h + 1) * 128], qt[:, h * 128:(h + 1) * 128], ident_f, ["qt", "cst"], [PSK[6]])
                    cp(qT3[:, 4 * g:4 * g + 4, t * 128:(t + 1) * 128], PS[6][:, :].rearrange("p (h t) -> p h t", t=128), [PSK[6]], ["qT"])
                return f

            def iq_consumer(t, bank, bk):
                act(qt, bank[:, :], AF.Copy, [bk], ["qt"])
                rope(qt.rearrange("p (h d) -> p h d", d=64), 8, 8, 8 + t, 16, "qt")
                for j in range(4):
                    tr(PS[7][:, j * 128:(j + 1) * 128], qt[:, j * 128:(j + 1) * 128], ident_f, ["qt", "cst"], [PSK[7]])
                cp(iqT3[:, :, t * 128:(t + 1) * 128], PS[7][:, :].rearrange("p (j t) -> p j t", t=128), [PSK[7]], ["iqT"])

            if reuse:
                dma(kT3[:, :, 0:1024], KS.rearrange("p (h t) -> p h t", t=1024), ["KS"], ["kT"], "ld_kT")
                dma(v_sb4[:, 0:8, :, :], VS.rearrange("p (b h d) -> p b h d", h=2, d=128), ["VS"], ["v_sb"], "ld_v")
                dma(ikT2[:, 0:1024], IS[:, :], ["IS"], ["ikT2"], "ld_ik")
                dma(upT3[:, :, 0:128], UPS.rearrange("p (g t) -> p g t", t=128), ["UPS"], ["upT"], "ld_up")
                ts(upT3[:, :, 0:128], upT3[:, :, 0:128], pflag, None, ALU.mult, None, ["upT", "pc"], ["upT"])
            elif not noprefix:
                make_uT(xpre, 0, 1, srck=[prek])
                proj_tok(OK_, 512, kv_consumer(0))
                proj_tok(OIK, 72, ik_consumer(0, False))
                proj_T(OUS, 4, us_consumer(0, True), [(0, 512), (512, 512)])
                proj_T(OUP, 4, up_consumer(True), [(896, 128)])
            else:
                mset(upT3[:, :, 0:128], 0.0, ["upT"])
            make_uT(xown, 0, 1, srck=[ownk])
            dump("uT", None, None) if False else None
            proj_tok(OQ, 512, q_consumer(0))
            proj_tok(OQ + 512, 512, q_consumer(1))
            proj_tok(OK_, 512, kv_consumer(8))
            proj_tok(OIQ, 512, iq_consumer)
            proj_tok(OIK, 72, ik_consumer(8, True))
            proj_T(OUS, 4, us_consumer(1024, False), [(0, 512), (512, 512)])
            proj_T(OUP, 4, up_consumer(False), [(0, 512), (512, 512)])
            if noprefix:
                dma(KS.rearrange("p (h t) -> p h t", t=1024), kT3[:, :, 1024:2048], ["kT"], ["KS"], "st_k")
                dma(VS.rearrange("p (b h d) -> p b h d", h=2, d=128), v_sb4[:, 8:16, :, :], ["v_sb"], ["VS"], "st_v")
                dma(IS[:, :], ikT2[:, 1024:2048], ["ikT2"], ["IS"], "st_ik")
                dma(UPS.rearrange("p (g t) -> p g t", t=128), upT3[:, :, 1024:1152], ["upT"], ["UPS"], "st_up")
            if "qT" in dbg_out:
                for nm, ap_, k_ in (("qT", qT, "qT"), ("kT", kT, "kT"), ("v_sb", v_sb, "v_sb"), ("ikT2", ikT2, "ikT2"),
                                    ("iqT", iqT, "iqT"), ("usT", usT, "usT"), ("upT", upT, "upT")):
                    fin.append(dma(dbg_out[nm], ap_, [k_], [], "dbg_" + nm, eng="gpsimd"))
                dump("iw", iw_sb, ["iw"])
            p.barrier()
            if stop_after == "BC":
                p.emit(fin)
                return nc

            top[0] = att_top
            accs = [A(2048), A(2048)]
            work = A(2048)
            rl = [A(512) for _ in range(2)]
            m8 = A(256)
            thr = A(1)
            m01 = A(2048, BF16)
            m01Ts = [A(16 * 128, BF16).rearrange("p (k q) -> p k q", q=128) for _ in range(2)]
            osb = A(512)
            dsb = A(512)
            mb_c = A(2)
            mset(mb_c[:, 0:1], 30000.0, ["mb_c"])
            mset(mb_c[:, 1:2], -30000.0, ["mb_c"])
            PTs = [A(512, BF16) for _ in range(2)]
            rden = A(512)
            kb_sb = A(1024)
            dma(kb_sb, kbias_in[:, :], [], ["kbias"], "kbias")
            SCALE = 128 ** -0.5
            k0 = 1024 if noprefix else 0
            kb0 = k0 // 128
            def geom(i):
                nk = 1024 + (i + 1) * 128 - k0
                return nk, nk // 128, (nk + 511) // 512

            cntr = [0]

            def indexer(i):
                nk, nkb, n5 = geom(i)
                acc, acck = accs[i % 2], "acc%d" % (i % 2)
                for h in range(8):
                    pr = (h % 2) * 64
                    for b5 in range(n5):
                        c0 = b5 * 512
                        w = min(512, nk - c0)
                        b = cntr[0] % 2
                        cntr[0] += 1
                        mm(PS[b][:, 0:w], iqT3[pr:pr + 64, h // 2, i * 128:(i + 1) * 128], ikT2[pr:pr + 64, k0 + c0:k0 + c0 + w], True, True, ["iqT", "ikT2"], [PSK[b]])
                        act(rl[b][:, 0:w], PS[b][:, 0:w], AF.Relu, [PSK[b]], ["rl%d" % b])
                        if h == 0:
                            if k0 + c0 < 1024:
                                in1 = kb_sb[:, c0:c0 + w]
                            else:
                                o0 = 896 - i * 128 + (k0 + c0 - 1024)
                                in1 = ztri[:, o0:o0 + w]
                            stt(acc[:, c0:c0 + w], rl[b][:, 0:w], iw3[:, i, h:h + 1], in1, ALU.mult, ALU.add, ["rl%d" % b, "iw", "kbias", "cst"], [acck])
                        else:
                            stt(acc[:, c0:c0 + w], rl[b][:, 0:w], iw3[:, i, h:h + 1], acc[:, c0:c0 + w], ALU.mult, ALU.add, ["rl%d" % b, "iw", acck], [acck])

            def topk(i):
                nk, nkb, n5 = geom(i)
                acc, acck = accs[i % 2], "acc%d" % (i % 2)
                if nk > 256:
                    cur, ck = acc, acck
                    for r in range(32):
                        vmax(m8[:, r * 8:(r + 1) * 8], cur[:, 0:nk], [ck], ["m8"])
                        if r < 31:
                            vmr(work[:, 0:nk], m8[:, r * 8:(r + 1) * 8], cur[:, 0:nk], [ck, "m8"], ["work"])
                            cur, ck = work, "work"
                    ts(thr, m8[:, 255:256], -1e29, None, ALU.max, None, ["m8"], ["thr"])
                    ts(m01[:, 0:nk], acc[:, 0:nk], thr[:, 0:1], None, ALU.is_ge, None, [acck, "thr"], ["m01"])
                else:
                    ts(m01[:, 0:nk], acc[:, 0:nk], -1e29, None, ALU.is_ge, None, [acck], ["m01"])

            def attn(i):
                nk, nkb, n5 = geom(i)
                m01T3, m01Tk = m01Ts[i % 2], "m01T%d" % (i % 2)
                for g0 in range(0, nkb, 8):
                    gn = min(8, nkb - g0)
                    b = 2 + (g0 // 8) % 2
                    for j in range(gn):
                        kb = g0 + j
                        tr(PSb[b][:, j * 128:(j + 1) * 128], m01[:, kb * 128:(kb + 1) * 128], ident_b, ["m01", "cstb"], [PSK[b]])
                    act(m01T3[:, g0:g0 + gn, :], PSb[b][:, 0:gn * 128].rearrange("p (k q) -> p k q", q=128), AF.Identity, [PSK[b], "mb_c"], [m01Tk],
                        scale=mb_c[:, 0:1], bias=mb_c[:, 1:2])
                for kvh in range(2):
                    for kb in range(nkb):
                        gkb = kb0 + kb
                        sb_ = 4 + kb % 2
                        pk = "PT%d" % (kb % 2)
                        PT = PTs[kb % 2]
                        PT3 = PT.rearrange("p (h q) -> p h q", q=128)
                        mm(PS[sb_][:, :], kT3[:, kvh, gkb * 128:(gkb + 1) * 128], qT3[:, 4 * kvh:4 * kvh + 4, i * 128:(i + 1) * 128], True, False, ["kT", "qT"], [PSK[sb_]])
                        for hh in range(4):
                            mm(PS[sb_][:, hh * 128:(hh + 1) * 128], ident_b, m01T3[:, kb, :], False, True, ["cstb", m01Tk], [PSK[sb_]])
                        act(PT, PS[sb_][:, :], AF.Exp, [PSK[sb_]], [pk], scale=SCALE)
                        mm(PS[6][:, :], v_sb4[:, gkb, kvh, :], PT, kb == 0, kb == nkb - 1, ["v_sb", pk], [PSK[6]])
                        mm(PS[7][:, :], ones_b, PT, kb == 0, kb == nkb - 1, ["cstb", pk], [PSK[7]])
                    act(osb, PS[6][:, :], AF.Copy, [PSK[6]], ["osb"])
                    act(dsb, PS[7][:, :], AF.Ln, [PSK[7]], ["dsb"])
                    act(dsb, dsb, AF.Exp, ["dsb"], ["dsb"], scale=-1.0)
                    gtt(mixT3[:, 4 * kvh:4 * kvh + 4, i * 128:(i + 1) * 128], osb.rearrange("p (h q) -> p h q", q=128),
                        dsb.rearrange("p (h q) -> p h q", q=128), ALU.mult, ["osb", "dsb"], ["mixT"])

            indexer(0)
            for i in range(8):
                if i + 1 < 8:
                    indexer(i + 1)
                topk(i)
                attn(i)
            p.barrier()
            if stop_after == "EF":
                if "mixT" in dbg_out:
                    fin.append(dma(dbg_out["mixT"], mixT, ["mixT"], [], "dbg_mixT", eng="gpsimd"))
                p.emit(fin)
                return nc

            top[0] = l1_top
            sm = A(48)
            dma(sm, ssm_small[:, :], [], ["sm"], "sm")
            lam_re, lam_im, lstep = sm[:, 0:16], sm[:, 16:32], sm[:, 32:48]
            PTre = A(2048)
            PTim = A(2048)
            PinvRe = A(2048)
            PinvIm = A(2048)
            W_B = [A(4 * 512, BF16) for _ in range(2)]
            Wc = [A(16 * 128, BF16) for _ in range(2)]
            wglu_sb = A(4 * 512, BF16)
            sv = A(16 * 24)
            ki16 = A(16, I32)
            ssm_tmp = top[0]
            bc_sb = A(1024)
            dma(bc_sb, ssm_bc[:, :], [], ["bc_sb"], "bc_sb")
            b_re3 = bc_sb[:, 0:256].rearrange("p (s c) -> p s c", c=16)
            b_im3 = bc_sb[:, 256:512].rearrange("p (s c) -> p s c", c=16)
            c_re3 = bc_sb[:, 512:768].rearrange("p (s c) -> p s c", c=16)
            c_im3 = bc_sb[:, 768:1024].rearrange("p (s c) -> p s c", c=16)
            svn = [0]

            def SV():
                v = sv[:, svn[0] * 16:(svn[0] + 1) * 16]
                svn[0] += 1
                return v
            dt_, ar, th = SV(), SV(), SV()
            act(dt_, lstep, AF.Exp, ["sm"], ["dt"])
            tt(ar, lam_re, dt_, ALU.mult, ["sm", "dt"], ["ar"])
            tt(th, lam_im, dt_, ALU.mult, ["sm", "dt"], ["th"])
            PIre = A(2048)
            PIim = A(2048)
            big1 = A(2048)
            big2 = A(2048)
            big3 = A(2048)
            bigi = A(2048, I32)
            B3 = lambda v: v.rearrange("p (s t) -> p s t", t=128)
            iota_b = iota_f.unsqueeze(1).to_broadcast([128, 16, 128])
            tt(B3(big1), th.unsqueeze(2).to_broadcast([128, 16, 128]), iota_b, ALU.mult, ["th", "cst"], ["big1"])
            sin_of(PIim, big1, 0.0, big2, bigi, big3, ["big1"], "sinS", "big2", "bigi", "big3")
            sin_of(PIre, big1, PI / 2, big2, bigi, big3, ["big1"], "cosS", "big2", "bigi", "big3")
            tt(B3(big1), ar.unsqueeze(2).to_broadcast([128, 16, 128]), iota_b, ALU.mult, ["ar", "cst"], ["big1"])
            act(big2, big1, AF.Exp, ["big1"], ["big2"])
            act(big3, big1, AF.Exp, ["big1"], ["big3"], scale=-1.0)
            tt(PTre, big2, PIre, ALU.mult, ["big2", "cosS"], ["PTre"])
            tt(PTim, big2, PIim, ALU.mult, ["big2", "sinS"], ["PTim"])
            tt(PIre, big3, PIre, ALU.mult, ["big3", "cosS"], ["cosS"])
            stt(PIim, big3, -1.0, PIim, ALU.mult, ALU.mult, ["big3", "sinS"], ["sinS"])
            for comp, (src, sk, dst, dk) in enumerate(((PIre, "cosS", PinvRe, "PinvRe"), (PIim, "sinS", PinvIm, "PinvIm"))):
                for g in range(4):
                    b = 2 * comp + (g % 2)
                    for j in range(4):
                        sb_i = g * 4 + j
                        tr(PS[b][:, j * 128:(j + 1) * 128], src[:, sb_i * 128:(sb_i + 1) * 128], ident_f, [sk, "cst"], [PSK[b]])
                    cp(dst[:, g * 512:(g + 1) * 512], PS[b][:, :], [PSK[b]], [dk])
            L_re, L_im = SV(), SV()
            a128, k1, t1_, mg = SV(), SV(), SV(), SV()
            ts(a128, th, 128.0, None, ALU.mult, None, ["th"], ["a128"])
            sin_of(L_im, a128, 0.0, k1, ki16, t1_, ["a128"], "Lsin", "k1", "ki16", "t1_")
            sin_of(L_re, a128, PI / 2, k1, ki16, t1_, ["a128"], "Lcos", "k1", "ki16", "t1_")
            act(mg, ar, AF.Exp, ["ar"], ["mg"], scale=128.0)
            tt(L_re, L_re, mg, ALU.mult, ["Lcos", "mg"], ["Lcos"])
            tt(L_im, L_im, mg, ALU.mult, ["Lsin", "mg"], ["Lsin"])
            PTre3, PTim3 = B3(PTre), B3(PTim)
            a_, b_, den_, m_re, m_im, u1, u2 = SV(), SV(), SV(), SV(), SV(), SV(), SV()
            ts(a_, PTre3[:, :, 1], -1.0, None, ALU.add, None, ["PTre"], ["a_"])
            cp(b_, PTim3[:, :, 1], ["PTim"], ["b_"])
            tt(u1, lam_re, lam_re, ALU.mult, ["sm"], ["u1"])
            tt(u2, lam_im, lam_im, ALU.mult, ["sm"], ["u2"])
            tt(den_, u1, u2, ALU.add, ["u1", "u2"], ["den_"])
            V(lambda e: e.reciprocal(out=den_, in_=den_), ["den_"], ["den_"])
            tt(u1, a_, lam_re, ALU.mult, ["a_", "sm"], ["u1"])
            tt(u2, b_, lam_im, ALU.mult, ["b_", "sm"], ["u2"])
            tt(m_re, u1, u2, ALU.add, ["u1", "u2"], ["m_re"])
            tt(m_re, m_re, den_, ALU.mult, ["m_re", "den_"], ["m_re"])
            tt(u1, b_, lam_re, ALU.mult, ["b_", "sm"], ["u1"])
            tt(u2, a_, lam_im, ALU.mult, ["a_", "sm"], ["u2"])
            tt(m_im, u1, u2, ALU.subtract, ["u1", "u2"], ["m_im"])
            tt(m_im, m_im, den_, ALU.mult, ["m_im", "den_"], ["m_im"])
            p.barrier()
            bigf = bigi.bitcast(F32)
            Bb_re, Bb_im, bt1, bt2 = bigf[:, 0:256], bigf[:, 256:512], bigf[:, 512:768], bigf[:, 768:1024]
            S3 = lambda v: v.rearrange("p (s c) -> p s c", c=16)
            mre_b = m_re.unsqueeze(2).to_broadcast([128, 16, 16])
            mim_b = m_im.unsqueeze(2).to_broadcast([128, 16, 16])
            tt(S3(bt1), b_re3, mre_b, ALU.mult, ["bc_sb", "m_re"], ["bt1"])
            tt(S3(bt2), b_im3, mim_b, ALU.mult, ["bc_sb", "m_im"], ["bt2"])
            tt(Bb_re, bt1, bt2, ALU.subtract, ["bt1", "bt2"], ["Bb_re"])
            tt(S3(bt1), b_im3, mre_b, ALU.mult, ["bc_sb", "m_re"], ["bt1"])
            tt(S3(bt2), b_re3, mim_b, ALU.mult, ["bc_sb", "m_im"], ["bt2"])
            tt(Bb_im, bt1, bt2, ALU.add, ["bt1", "bt2"], ["Bb_im"])
            for comp, (srcB, kB, srcC, cneg) in enumerate(((Bb_re, "Bb_re", c_re3, 1.0), (Bb_im, "Bb_im", c_im3, -1.0))):
                wide = big1 if comp == 0 else big2
                wk = "big1" if comp == 0 else "big2"
                V(lambda e, wide=wide: e.memset(wide, 0.0), [], [wk])
                w4 = wide.rearrange("p (a j c) -> p a j c", j=4, c=128)
                s4 = srcB.rearrange("p (a j c) -> p a j c", j=4, c=16)
                for j in range(4):
                    for gl in range(2):
                        cp(w4[gl * 64:(gl + 1) * 64, :, j, j * 32 + gl * 16:j * 32 + gl * 16 + 16], s4[gl * 64:(gl + 1) * 64, :, j, :], [kB], [wk])
                for g in range(4):
                    b = 4 + (g % 2)
                    for j in range(4):
                        sb_i = g * 4 + j
                        tr(PS[b][:, j * 128:(j + 1) * 128], wide[:, sb_i * 128:(sb_i + 1) * 128], ident_f, [wk, "cst"], [PSK[b]])
                    cp(W_B[comp][:, g * 512:(g + 1) * 512], PS[b][:, :], [PSK[b]], ["W_B%d" % comp])
                wide2 = big3 if comp == 0 else PIre
                wk2 = "big3" if comp == 0 else "cosS"
                V(lambda e, wide2=wide2: e.memset(wide2, 0.0), [], [wk2])
                w4 = wide2.rearrange("p (a j c) -> p a j c", j=4, c=128)
                s4 = srcC.rearrange("p (a j) c -> p a j c", j=4)
                for j in range(4):
                    for gl in range(2):
                        ts(w4[gl * 64:(gl + 1) * 64, :, j, j * 32 + gl * 16:j * 32 + gl * 16 + 16], s4[gl * 64:(gl + 1) * 64, :, j, :], cneg, None, ALU.mult, None, ["bc_sb"], [wk2])
                cp(Wc[comp], wide2, [wk2], ["Wc%d" % comp])
            p.barrier()
            top[0] = ssm_tmp
            wglu3 = wglu_sb.rearrange("p (k n) -> p k n", n=512)
            dma(wglu3, w_glu.rearrange("(k p) n -> p k n", p=128), [], ["wglu"], "wglu", eng="gpsimd")
            Xre = [A(2048, BF16) for _ in range(2)]
            Xim = [A(2048, BF16) for _ in range(2)]
            sTre = A(2048, BF16)
            sTim = A(2048, BF16)
            yg = A(4 * 1024, BF16)
            yg3 = yg.rearrange("p (c t) -> p c t", t=1024)
            tq = [A(512) for _ in range(4)]
            Are, Aim = A(512), A(512)
            car_re, car_im, al_re, al_im, cu1, cu2 = SV(), SV(), SV(), SV(), SV(), SV()
            yv = A(512)
            if reuse:
                dma(car_re, CS[:, 0:16], ["CS"], ["car_re"], "ld_cre")
                dma(car_im, CS[:, 16:32], ["CS"], ["car_im"], "ld_cim")
                ts(car_re, car_re, pflag, None, ALU.mult, None, ["car_re", "pc"], ["car_re"])
                ts(car_im, car_im, pflag, None, ALU.mult, None, ["car_im", "pc"], ["car_im"])
            else:
                V(lambda e: e.memset(car_re, 0.0), [], ["car_re"])
                V(lambda e: e.memset(car_im, 0.0), [], ["car_im"])
            for j in range(8 if (noprefix or reuse) else 0, 16):
                own = j >= 8
                xr, xi = Xre[j % 2], Xim[j % 2]
                xrk, xik = "Xre%d" % (j % 2), "Xim%d" % (j % 2)
                for cc in range(4):
                    sl = slice(cc * 512, (cc + 1) * 512)
                    mm(PS[0][:, :], usT3[:, cc, j * 128:(j + 1) * 128], W_B[0][:, sl], True, True, ["usT", "W_B0"], [PSK[0]])
                    mm(PS[1][:, :], usT3[:, cc, j * 128:(j + 1) * 128], W_B[1][:, sl], True, True, ["usT", "W_B1"], [PSK[1]])
                    tt(tq[0], PS[0][:, :], PinvRe[:, sl], ALU.mult, [PSK[0], "PinvRe"], ["tq0"])
                    tt(tq[1], PS[1][:, :], PinvIm[:, sl], ALU.mult, [PSK[1], "PinvIm"], ["tq1"])
                    tt(tq[2], PS[1][:, :], PinvRe[:, sl], ALU.mult, [PSK[1], "PinvRe"], ["tq2"])
                    tt(tq[3], PS[0][:, :], PinvIm[:, sl], ALU.mult, [PSK[0], "PinvIm"], ["tq3"])
                    tt(xr[:, sl], tq[0], tq[1], ALU.subtract, ["tq0", "tq1"], [xrk])
                    tt(xi[:, sl], tq[2], tq[3], ALU.add, ["tq2", "tq3"], [xik])
                if not own:
                    for sb_i in range(16):
                        mm(PS[2][:, sb_i:sb_i + 1], xr[:, sb_i * 128:(sb_i + 1) * 128], ones_b[:, 0:1], True, True, [xrk, "cstb"], [PSK[2]])
                        mm(PS[3][:, sb_i:sb_i + 1], xi[:, sb_i * 128:(sb_i + 1) * 128], ones_b[:, 0:1], True, True, [xik, "cstb"], [PSK[3]])
                    tt(al_re, PS[2][:, 0:16], car_re, ALU.add, [PSK[2], "car_re"], ["al_re"])
                    tt(al_im, PS[3][:, 0:16], car_im, ALU.add, [PSK[3], "car_im"], ["al_im"])
                else:
                    tl = j - 8
                    for g in range(4):
                        for jj in range(4):
                            sb_i = g * 4 + jj
                            mm(PS[2][:, jj * 128:(jj + 1) * 128], xr[:, sb_i * 128:(sb_i + 1) * 128], tri_b, True, True, [xrk, "cstb"], [PSK[2]])
                            mm(PS[3][:, jj * 128:(jj + 1) * 128], xi[:, sb_i * 128:(sb_i + 1) * 128], tri_b, True, True, [xik, "cstb"], [PSK[3]])
                        gs = slice(g * 512, (g + 1) * 512)
                        A3 = lambda v: v.rearrange("p (s t) -> p s t", t=128)
                        tt(A3(Are), A3(PS[2][:, :]), car_re[:, g * 4:(g + 1) * 4].unsqueeze(2).to_broadcast([128, 4, 128]), ALU.add, [PSK[2], "car_re"], ["Are"])
                        tt(A3(Aim), A3(PS[3][:, :]), car_im[:, g * 4:(g + 1) * 4].unsqueeze(2).to_broadcast([128, 4, 128]), ALU.add, [PSK[3], "car_im"], ["Aim"])
                        cp(al_re[:, g * 4:(g + 1) * 4], A3(Are)[:, :, 127], ["Are"], ["al_re"])
                        cp(al_im[:, g * 4:(g + 1) * 4], A3(Aim)[:, :, 127], ["Aim"], ["al_im"])
                        tt(tq[0], PTre[:, gs], Are, ALU.mult, ["PTre", "Are"], ["tq0"])
                        tt(tq[1], PTim[:, gs], Aim, ALU.mult, ["PTim", "Aim"], ["tq1"])
                        tt(tq[2], PTre[:, gs], Aim, ALU.mult, ["PTre", "Aim"], ["tq2"])
                        tt(tq[3], PTim[:, gs], Are, ALU.mult, ["PTim", "Are"], ["tq3"])
                        tt(sTre[:, gs], tq[0], tq[1], ALU.subtract, ["tq0", "tq1"], ["sTre"])
                        tt(sTim[:, gs], tq[2], tq[3], ALU.add, ["tq2", "tq3"], ["sTim"])
                    for cc in range(4):
                        for jj in range(4):
                            sb_i = cc * 4 + jj
                            mm(PS[4][:, cc * 128:(cc + 1) * 128], Wc[0][:, sb_i * 128:(sb_i + 1) * 128], sTre[:, sb_i * 128:(sb_i + 1) * 128], jj == 0, False, ["Wc0", "sTre"], [PSK[4]])
                            mm(PS[4][:, cc * 128:(cc + 1) * 128], Wc[1][:, sb_i * 128:(sb_i + 1) * 128], sTim[:, sb_i * 128:(sb_i + 1) * 128], False, jj == 3, ["Wc1", "sTim"], [PSK[4]])
                    for cc in range(4):
                        stt(yv[:, cc * 128:(cc + 1) * 128], usT3[:, cc, j * 128:(j + 1) * 128], vec_sb[:, cc:cc + 1], PS[4][:, cc * 128:(cc + 1) * 128], ALU.mult, ALU.add, ["usT", "vec", PSK[4]], ["yv"])
                    act(yg3[:, :, tl * 128:(tl + 1) * 128], yv.rearrange("p (c t) -> p c t", t=128), AF.Gelu, ["yv"], ["yg"])
                tt(cu1, L_re, al_re, ALU.mult, ["Lcos", "al_re"], ["cu1"])
                tt(cu2, L_im, al_im, ALU.mult, ["Lsin", "al_im"], ["cu2"])
                tt(car_re, cu1, cu2, ALU.subtract, ["cu1", "cu2"], ["car_re"])
                tt(cu1, L_re, al_im, ALU.mult, ["Lcos", "al_im"], ["cu1"])
                tt(cu2, L_im, al_re, ALU.mult, ["Lsin", "al_re"], ["cu2"])
                tt(car_im, cu1, cu2, ALU.add, ["cu1", "cu2"], ["car_im"])
            if noprefix:
                dma(CS[:, 0:16], car_re, ["car_re"], ["CS"], "st_c")
                dma(CS[:, 16:32], car_im, ["car_im"], ["CS"], "st_c")
            sg = [A(512, BF16) for _ in range(2)]
            n = 0
            for co in range(4):
                for tb in range(2):
                    b = n % 2
                    n += 1
                    for cc in range(4):
                        mm(PS[b][:, :], wglu3[:, cc, co * 128:(co + 1) * 128], yg3[:, cc, tb * 512:(tb + 1) * 512], cc == 0, cc == 3, ["wglu", "yg"], [PSK[b]])
                    act(sg[b], PS[b][:, :], AF.Sigmoid, [PSK[b], "vec"], ["sg%d" % b], bias=vec_sb[:, 4 + co:5 + co])
                    tt(mixT3[:, 8 + co, tb * 512:(tb + 1) * 512], sg[b], yg3[:, co, tb * 512:(tb + 1) * 512], ALU.mult, ["sg%d" % b, "yg"], ["mixT"])
            p.barrier()

            top[0] = l1_top
            pw_sb = A(4 * 128, BF16)
            pw3 = pw_sb.rearrange("p (g d) -> p g d", d=128)
            dma(pw3, pool_w.rearrange("(g c) d -> c g d", c=128), [], ["pw"], "pw", eng="gpsimd")
            xf = A(1152)
            pa = A(1152)
            pb = A(1152)
            pl = A(1024, BF16)
            for g in range(4):
                win = 2 ** (g + 1)
                cp(xf, upT3[:, g, :], ["upT"], ["xf"])
                src, sk = xf, "xf"
                bufs = [(pa, "pa"), (pb, "pb")]
                sh = 1
                for s in range(g + 1):
                    dst, dk = bufs[s % 2]
                    tt(dst[:, 16:1152], src[:, 16:1152], src[:, 16 - sh:1152 - sh], ALU.add, [sk], [dk])
                    src, sk = dst, dk
                    sh *= 2
                dst, dk = bufs[(g + 1) % 2]
                stt(dst[:, 128:1152], src[:, 128:1152], 1.0 / win, xf[:, 128:1152], ALU.mult, ALU.subtract, [sk, "xf"], [dk])
                tt(dst[:, 128:144], src[:, 128:144], invcnt16[:, g, :], ALU.mult, [sk, "pc"], [dk])
                tt(dst[:, 128:144], dst[:, 128:144], xf[:, 128:144], ALU.subtract, [dk, "xf"], [dk])
                cp(pl, dst[:, 128:1152], [dk], ["pl"])
                for tb in range(2):
                    b = tb
                    mm(PS[b][:, :], pw3[:, g, :], pl[:, tb * 512:(tb + 1) * 512], True, True, ["pw", "pl"], [PSK[b]])
                    ts(mixT3[:, 12 + g, tb * 512:(tb + 1) * 512], PS[b][:, :], vec_sb[:, 8 + g:9 + g], None, ALU.mult, None, [PSK[b], "vec"], ["mixT"])
            p.barrier()
            if "mixT" in dbg_out:
                fin.append(dma(dbg_out["mixT"], mixT, ["mixT"], [], "dbg_mixT", eng="gpsimd"))
                p.barrier()
            if stop_after == "H":
                p.emit(fin)
                return nc

            top[0] = mix_top
            wbI = [A(16 * 512, BF16) for _ in range(2)]
            rbuf = A(8 * 2048)
            r3 = rbuf.rearrange("p (t c) -> p t c", c=2048)
            xch = [A(512) for _ in range(2)]
            tmpI = A(512)
            lng = A(2048)
            lnb = A(2048)
            u2Tf = A(16 * 128)
            u2Tf3 = u2Tf.rearrange("p (k t) -> p k t", t=128)
            wr_sb = A(256)
            wr_hi = A(256, BF16)
            wr_lo = A(256, BF16)
            wrh3 = wr_hi.rearrange("p (k e) -> p k e", e=16)
            wrl3 = wr_lo.rearrange("p (k e) -> p k e", e=16)
            st_ = A(32)
            dma(lng, lnp[:, 0:2048], [], ["lng"], "lng")
            dma(lnb, lnp[:, 2048:4096], [], ["lnb"], "lnb")
            dma(wr_sb, w_router[:, :], [], ["wr"], "wr")
            cp(wr_hi, wr_sb, ["wr"], ["wr_hi"])
            tt(wr_lo, wr_sb, wr_hi, ALU.subtract, ["wr", "wr_hi"], ["wr_lo"])
            w_out_r = w_out.rearrange("(k p) n -> p k n", p=128)
            n = 0
            for cg in range(4):
                wv = wbI[cg % 2].rearrange("p (k n) -> p k n", n=512)
                wk = "wbI%d" % (cg % 2)
                dma_w(wv, w_out_r[:, :, cg * 512:(cg + 1) * 512], [wk], wk)
                for t in range(8):
                    b = n % 2
                    xc = xch[n % 2]
                    xck = "xch%d" % (n % 2)
                    n += 1
                    dma(xc, xown[t * 128:(t + 1) * 128, cg * 512:(cg + 1) * 512], [ownk], [xck], xck)
                    for kc in range(16):
                        mm(PS[b][:, :], mixT3[:, kc, t * 128:(t + 1) * 128], wv[:, kc, :], kc == 0, kc == 15, ["mixT", wk], [PSK[b]])
                    tt(tmpI, PS[b][:, :], g1p[:, cg * 512:(cg + 1) * 512], ALU.mult, [PSK[b], "g1p"], ["tmpI"])
                    stt(r3[:, t, cg * 512:(cg + 1) * 512], xc, ALU_ALPHA, tmpI, ALU.mult, ALU.add, [xck, "tmpI"], ["r%d" % t])
            p.barrier()

            def layer_norm(x_ap, xk, g_ap, b_ap, gk, bk, sidx, junk, junkk):
                s_sum = st_[:, sidx * 4 + 0:sidx * 4 + 1]
                s_mean = st_[:, sidx * 4 + 1:sidx * 4 + 2]
                s_ss = st_[:, sidx * 4 + 2:sidx * 4 + 3]
                s_rstd = st_[:, sidx * 4 + 3:sidx * 4 + 4]
                sk_ = "st%d" % sidx
                V(lambda e: e.tensor_reduce(out=s_sum, in_=x_ap, axis=AX.X, op=ALU.add), [xk], [sk_])
                ts(s_mean, s_sum, 1.0 / 2048, None, ALU.mult, None, [sk_], [sk_])
                ts(x_ap, x_ap, s_mean, None, ALU.subtract, None, [xk, sk_], [xk])
                tt(junk, x_ap, x_ap, ALU.mult, [xk], [junkk])
                red(s_ss, junk, ALU.add, [junkk], [sk_])
                act(s_rstd, s_ss, AF.Sqrt, [sk_, "eps"], [sk_], scale=1.0 / 2048, bias=eps_t[:, 0:1])
                V(lambda e: e.reciprocal(out=s_rstd, in_=s_rstd), [sk_], [sk_])
                stt(x_ap, x_ap, s_rstd, g_ap, ALU.mult, ALU.mult, [xk, sk_, gk], [xk])
                tt(x_ap, x_ap, b_ap, ALU.add, [xk, bk], [xk])

            u2T3 = mixT3
            rt_ = A(16 * 8)
            for t in range(8):
                xk = "r%d" % t
                x1 = r3[:, t, :]
                layer_norm(x1, xk, lng, lnb, "lng", "lnb", t % 2, u2Tf, "u2Tf")
                dma(xmid[t * 128:(t + 1) * 128, :], x1, [xk], ["xmid"], "xmid_w")
            p.barrier()
            xts = [u2Tf, lng]
            make_uT(xmid, 2, 3, srck=["xmid"])
            for t in range(8):
                for kc in range(16):
                    hi_ = u2T3[:, kc, t * 128:(t + 1) * 128]
                    mm(PS[4][:, 0:16], hi_, wrh3[:, kc, :], kc == 0, False, ["uT", "wr_hi"], [PSK[4]])
                    mm(PS[4][:, 0:16], hi_, wrl3[:, kc, :], False, kc == 15, ["uT", "wr_lo"], [PSK[4]])
                lg = rt_[:, 0:16]
                mx = rt_[:, 16:17]
                ex = rt_[:, 32:48]
                pr6 = rt_[:, 48:72]
                gsc = rt_[:, 72:76]
                gmx = rt_[:, 76:77]
                goh = rt_[:, 80:84]
                eg = rt_[:, 84:100]
                m1 = rt_[:, 100:101]
                m2 = rt_[:, 101:102]
                eg2 = rt_[:, 104:120]
                msk = rt_[:, 120:128] if False else None
                cp(lg, PS[4][:, 0:16], [PSK[4]], ["rt"])
                if stop_after == "I3":
                    cp(gates3[:, t, :], lg, ["rt"], ["gates"])
                    continue
                V(lambda e: e.tensor_reduce(out=mx, in_=lg, axis=AX.X, op=ALU.max), ["rt"], ["rt"])
                ts(lg, lg, mx, None, ALU.subtract, None, ["rt"], ["rt"])
                act(ex, lg, AF.Exp, ["rt"], ["rt"])
                ex3 = ex.rearrange("p (g j) -> p g j", j=4)
                pr63 = pr6.rearrange("p (g k) -> p g k", k=6)
                kk = 0
                for a in range(4):
                    for bq in range(a + 1, 4):
                        tt(pr63[:, :, kk], ex3[:, :, a], ex3[:, :, bq], ALU.add, ["rt"], ["rt"])
                        kk += 1
                V(lambda e: e.tensor_reduce(out=gsc, in_=pr63, axis=AX.X, op=ALU.max), ["rt"], ["rt"])
                V(lambda e: e.tensor_reduce(out=gmx, in_=gsc, axis=AX.X, op=ALU.max), ["rt"], ["rt"])
                ts(goh, gsc, gmx, None, ALU.is_ge, None, ["rt"], ["rt"])
                tt(eg.rearrange("p (g j) -> p g j", j=4), ex3, goh.unsqueeze(2).to_broadcast([128, 4, 4]), ALU.mult, ["rt"], ["rt"])
                V(lambda e: e.tensor_reduce(out=m1, in_=eg, axis=AX.X, op=ALU.max), ["rt"], ["rt"])
                ts(eg2, eg, m1, None, ALU.is_lt, None, ["rt"], ["rt"])
                tt(eg2, eg2, eg, ALU.mult, ["rt"], ["rt"])
                V(lambda e: e.tensor_reduce(out=m2, in_=eg2, axis=AX.X, op=ALU.max), ["rt"], ["rt"])
                ts(eg2, eg, m2, None, ALU.is_ge, None, ["rt"], ["rt"])
                tt(eg2, eg2, eg, ALU.mult, ["rt"], ["rt"])
                tt(m1, m1, m2, ALU.add, ["rt"], ["rt"])
                V(lambda e: e.reciprocal(out=m1, in_=m1), ["rt"], ["rt"])
                ts(gates3[:, t, :], eg2, m1, None, ALU.mult, None, ["rt"], ["gates"])
            dump("gates", gates, ["gates"])
            p.barrier()
            if stop_after in ("I", "I1", "I2", "I3"):
                fin.append(("dma", "xmid_w") and p.res["xmid"][0])
                p.emit(fin)
                return nc

            top[0] = mix_top
            accm = A(8 * 2048)
            acc3 = accm.rearrange("p (t c) -> p t c", c=2048)
            hT = A(8 * 1024, BF16)
            hT3 = hT.rearrange("p (f t) -> p f t", t=1024)
            NB = 3
            wg = [A(16 * 256, BF16) for _ in range(2)]
            wu = [A(16 * 256, BF16) for _ in range(2)]
            wd = [A(8 * 512, BF16) for _ in range(2)]
            sgm = [A(512, BF16) for _ in range(2)]
            moe_top = top[0]
            mset(accm, 0.0, ["acc_%d" % t for t in range(8)])
            ng = 0
            nd_ = 0
            nps = 0
            for ex_i in range(16):
                eg_r = e_gate[ex_i].rearrange("(k p) f -> p k f", p=128)
                eu_r = e_up[ex_i].rearrange("(k p) f -> p k f", p=128)
                ed_r = e_down[ex_i].rearrange("(f p) c -> p f c", p=128)
                for fcp in range(4):
                    i = ng % 2
                    ng += 1
                    wgv = wg[i].rearrange("p (k f) -> p k f", f=256)
                    wuv = wu[i].rearrange("p (k f) -> p k f", f=256)
                    dma_w(wgv, eg_r[:, :, fcp * 256:(fcp + 1) * 256], ["wg%d" % i], "wg%d" % i)
                    dma_w(wuv, eu_r[:, :, fcp * 256:(fcp + 1) * 256], ["wu%d" % i], "wu%d" % i)
                    for sub in range(2):
                        fc = fcp * 2 + sub
                        fsl = slice(sub * 128, (sub + 1) * 128)
                        for th_ in range(2):
                            tsl = slice(th_ * 512, (th_ + 1) * 512)
                            bg, bu = 0 + (nps % 2), 2 + (nps % 2)
                            s_ = sgm[nps % 2]
                            sk_ = "sgm%d" % (nps % 2)
                            nps += 1
                            for kc in range(16):
                                mm(PS[bg][:, :], wgv[:, kc, fsl], u2T3[:, kc, tsl], kc == 0, kc == 15, ["wg%d" % i, "uT"], [PSK[bg]])
                            for kc in range(16):
                                mm(PS[bu][:, :], wuv[:, kc, fsl], u2T3[:, kc, tsl], kc == 0, kc == 15, ["wu%d" % i, "uT"], [PSK[bu]])
                            act(s_, PS[bg][:, :], AF.Silu, [PSK[bg]], [sk_])
                            tt(hT3[:, fc, tsl], s_, PS[bu][:, :], ALU.mult, [sk_, PSK[bu]], ["hT"])
                for cg in range(4):
                    i = nd_ % 2
                    wdv = wd[i].rearrange("p (f c) -> p f c", c=512)
                    dma_w(wdv, ed_r[:, :, cg * 512:(cg + 1) * 512], ["wd%d" % i], "wd%d" % i, nsplit=2)
                    for t in range(8):
                        b = 4 + (nd_ * 8 + t) % 4
                        for fc in range(8):
                            mm(PS[b][:, :], hT3[:, fc, t * 128:(t + 1) * 128], wdv[:, fc, :], fc == 0, fc == 7, ["hT", "wd%d" % i], [PSK[b]])
                        stt(acc3[:, t, cg * 512:(cg + 1) * 512], PS[b][:, :], gates3[:, t, ex_i:ex_i + 1], acc3[:, t, cg * 512:(cg + 1) * 512], ALU.mult, ALU.add, [PSK[b], "gates", "acc_%d" % t], ["acc_%d" % t])
                    nd_ += 1
            p.barrier()
            top[0] = mix_top + 8 * 2048
            lng2 = A(2048)
            lnb2 = A(2048)
            xm = [A(2048) for _ in range(2)]
            junk = A(2048)
            st_ = A(32)
            dma(lng2, lnp[:, 4096:6144], [], ["lng2"], "lng2")
            dma(lnb2, lnp[:, 6144:8192], [], ["lnb2"], "lnb2")
            for t in range(8):
                xk = "acc_%d" % t
                a_t = acc3[:, t, :]
                xmk = "xm%d" % (t % 2)
                dma(xm[t % 2], xmid[t * 128:(t + 1) * 128, :], ["xmid"], [xmk], xmk)
                tt(a_t, a_t, g2p, ALU.mult, [xk, "g2p"], [xk])
                stt(a_t, xm[t % 2], ALU_ALPHA, a_t, ALU.mult, ALU.add, [xmk, xk], [xk])
                layer_norm(a_t, xk, lng2, lnb2, "lng2", "lnb2", t % 2, junk, "junk")
                fin.append(dma(xout[t * 128:(t + 1) * 128, :], a_t, [xk], [outk], "xout"))
            p.barrier()

        xts = None
        run_pass(0, 0, xA, xA, S1, True, "xA", "xA", "S1", noprefix=True)
        run_pass(0, 1, xA, xB, S2, False, "xA", "xB", "S2", reuse=True)
        run_pass(1, 1, S1, S2, xout_f, True, "S1", "S2", "xoutf")
        p.emit(fin)
    return nc


ALU_ALPHA = float(ALPHA)


def _consts():
    c = np.zeros((128, 3072), np.float32)
    c[:, 0:128] = np.eye(128, dtype=np.float32)
    c[:, 128:256] = np.arange(128, dtype=np.float32)[None, :]
    c[:, 256:384] = np.triu(np.ones((128, 128), np.float32))
    c[:, 384:512] = 1.0
    zt = np.zeros((128, 1024), np.float32)
    q = np.arange(128)[:, None]
    s = np.arange(128)[None, :]
    zt[:, 896:1024] = np.where(s <= q, 0.0, NEG)
    c[:, 1024:2048] = zt
    rot, half = 32, 16
    c[:, 2048:2064] = (500000.0 ** (-np.arange(half, dtype=np.float32) * 2.0 / rot))[None, :]
    rot, half = 16, 8
    c[:, 2064:2072] = (500000.0 ** (-np.arange(half, dtype=np.float32) * 2.0 / rot))[None, :]
    return c


def _layer_inputs(inp, l):
    f = np.float32
    rep = lambda v: np.ascontiguousarray(np.broadcast_to(np.asarray(v, f)[None, :], (128, v.shape[0])))
    d = {}
    d["b_ada"] = rep(inp["b_ada"][l])

    def st_layout(a):
        return np.ascontiguousarray(a.reshape(16, 2, 64).transpose(1, 2, 0).reshape(128, 16))
    lam_re = st_layout(inp["ssm_lam_re"][l])
    lam_im = st_layout(inp["ssm_lam_im"][l])
    lstep = st_layout(np.broadcast_to(inp["ssm_log_step"][l][:, None], (32, 64)))
    d["ssm_small"] = np.concatenate([lam_re, lam_im, lstep], axis=1).astype(f)

    def b_layout(a):
        return a.reshape(16, 2, 64, 16).transpose(1, 2, 0, 3).reshape(128, 256)

    def c_layout(a):
        return a.reshape(16, 2, 16, 64).transpose(1, 3, 0, 2).reshape(128, 256)
    d["ssm_bc"] = np.ascontiguousarray(np.concatenate(
        [b_layout(inp["ssm_b_re"][l]), b_layout(inp["ssm_b_im"][l]), c_layout(inp["ssm_c_re"][l]), c_layout(inp["ssm_c_im"][l])], axis=1)).astype(f)
    pk = lambda v: np.asarray(v, f).reshape(4, 128).T
    d["vecs"] = np.ascontiguousarray(np.concatenate([pk(inp["ssm_d"][l]), pk(inp["ssm_b_glu"][l]), pk(inp["pool_scale"][l])], axis=1))
    d["pool_w"] = np.ascontiguousarray(inp["pool_w"][l].reshape(512, 128))
    d["lnp"] = np.ascontiguousarray(np.concatenate([rep(inp["ln1_g"][l]), rep(inp["ln1_b"][l]), rep(inp["ln2_g"][l]), rep(inp["ln2_b"][l])], axis=1))
    return d


def _shared_inputs(inp):
    f = np.float32
    per = [_layer_inputs(inp, l) for l in range(2)]
    d = {k: np.ascontiguousarray(np.stack([per[0][k], per[1][k]], axis=0)) for k in per[0]}
    for k, src in (("w_ada", "w_ada"), ("w_in", "w_in"), ("w_out", "w_out"), ("w_glu", "ssm_w_glu"),
                   ("e_gate", "e_gate"), ("e_up", "e_up"), ("e_down", "e_down")):
        d[k] = np.ascontiguousarray(np.asarray(inp[src], f))
    d["w_router"] = np.ascontiguousarray(np.asarray(inp["w_router"], f).reshape(16, 128, 16).transpose(1, 0, 2).reshape(128, 256))
    d["cst"] = _consts()
    return d


def _cfg(pos_b, h):
    f = np.float32
    own = pos_b[h * 1024:(h + 1) * 1024].reshape(8, 128).T
    pre = pos_b[0:1024].reshape(8, 128).T if h == 1 else np.zeros((128, 8), np.int32)
    pos_in = np.concatenate([pre, own], axis=1).astype(np.int32)
    kbias = np.full((128, 1024), 0.0 if h == 1 else NEG, f)
    pc = np.zeros((128, 80), f)
    pc[:, 0] = float(h)
    for g, win in enumerate((2, 4, 8, 16)):
        t = np.arange(16) + h * 1024
        pc[:, 16 + g * 16:32 + g * 16] = (1.0 / np.minimum(t + 1, win))[None, :]
    return pos_in, kbias, pc


def _core_inputs(inp, core):
    b, h = core // 2, core % 2
    f = np.float32
    d = {}
    xb = np.asarray(inp["x"][b], f)
    d["xA"] = np.ascontiguousarray(xb[0:1024])
    d["xB"] = np.ascontiguousarray(xb[h * 1024:(h + 1) * 1024])
    d["c_in"] = np.ascontiguousarray(np.asarray(inp["c"][b], f).reshape(16, 128).T)
    pos = np.asarray(inp["positions"][b], np.int32)
    c0 = _cfg(pos, 0)
    c1 = _cfg(pos, h)
    d["pos_in"] = np.ascontiguousarray(np.stack([c0[0], c1[0]], 0))
    d["kbias"] = np.ascontiguousarray(np.stack([c0[1], c1[1]], 0))
    d["pcore"] = np.ascontiguousarray(np.stack([c0[2], c1[2]], 0))
    return d


_NC_CACHE = {}


def kernel(**inputs):
    inp = {k: np.asarray(v) for k, v in inputs.items()}
    if "prog" not in _NC_CACHE:
        _NC_CACHE["prog"] = build_program()
    nc = _NC_CACHE["prog"]
    shared = _shared_inputs(inp)
    in_maps = []
    for core in range(8):
        d = dict(shared)
        d.update(_core_inputs(inp, core))
        in_maps.append(d)
    res = run_bass_kernel_spmd(nc, in_maps, core_ids=list(range(8)))
    out = np.empty((4, 2048, 2048), np.float32)
    for core in range(8):
        b, h = core // 2, core % 2
        out[b, h * 1024:(h + 1) * 1024] = res.results[core]["xout"]
    return out
```

```python
import math
import contextlib
import numpy as np
import concourse.bass as bass
import concourse.mybir as mybir
from concourse.bass_utils import run_bass_kernel_spmd

F32 = mybir.dt.float32
BF16 = mybir.dt.bfloat16
I32 = mybir.dt.int32
AF = mybir.ActivationFunctionType
ALU = mybir.AluOpType
AX = mybir.AxisListType

ENGINES = ("sync", "scalar", "vector", "gpsimd", "tensor")
ALPHA = (2.0 * 2) ** 0.25
LN_EPS = 1e-5
PI = math.pi
NEG = -1e30

OQ, OK_, OV, OIQ, OIK, OIW, OUS, OUP = 0, 1024, 1280, 1536, 2048, 2112, 2120, 2632


class Prog:
    def __init__(self, nc):
        self.nc = nc
        self.ops = {e: [] for e in ENGINES}
        self.res = {}
        self.streams = {}
        self.pending = {e: {} for e in ENGINES}

    def _add(self, engine, fn, reads, writes, dma_key=None):
        skey = ("dma", dma_key) if dma_key is not None else ("eng", engine)
        st = self.streams.setdefault(skey, [])
        deps = dict(self.pending[engine])
        self.pending[engine] = {}

        def need(tok):
            if tok is None:
                return
            k, i = tok
            if k == skey and engine == "tensor" and dma_key is None:
                return
            if k[0] == "dma":
                i = len(self.streams[k]) - 1
                if k == skey:
                    i = len(st) - 1
            if i >= 0 and deps.get(k, -1) < i:
                deps[k] = i

        for r in reads:
            ent = self.res.get(r)
            if ent is not None:
                need(ent[0])
        for w in writes:
            ent = self.res.get(w)
            if ent is not None:
                need(ent[0])
                for t in ent[1]:
                    need(t)
        op = dict(fn=fn, deps=deps, skey=skey, idx=len(st), marked=False)
        st.append(op)
        self.ops[engine].append(op)
        tok = (skey, op["idx"])
        for w in writes:
            self.res[w] = [tok, []]
        for r in reads:
            if r in writes:
                continue
            ent = self.res.setdefault(r, [None, []])
            ent[1].append(tok)
        return tok

    def op(self, engine, fn, reads=(), writes=()):
        return self._add(engine, fn, reads, writes)

    def dma(self, engine, fn, reads=(), writes=(), key=None):
        return self._add(engine, fn, reads, writes, dma_key=key)

    def barrier(self):
        last = {k: len(st) - 1 for k, st in self.streams.items() if st}
        for e in ENGINES:
            for k, i in last.items():
                if self.pending[e].get(k, -1) < i:
                    self.pending[e][k] = i

    def emit(self, final_waits=()):
        nc = self.nc
        for e in ENGINES:
            waited = {}
            for op in self.ops[e]:
                nd = {}
                for k, i in op["deps"].items():
                    if waited.get(k, -1) >= i:
                        continue
                    waited[k] = i
                    nd[k] = i
                    self.streams[k][i]["marked"] = True
                op["deps"] = nd
        fin = {}
        for k, i in final_waits:
            if k[0] == "dma":
                i = len(self.streams[k]) - 1
            fin[k] = max(fin.get(k, -1), i)
        for k, st in self.streams.items():
            if st:
                fin[k] = len(st) - 1
        for k, i in fin.items():
            self.streams[k][i]["marked"] = True
        for k, st in self.streams.items():
            v = 0
            for op in st:
                if k[0] == "dma":
                    v += 16
                    op["inc"] = 16
                elif op["marked"]:
                    v += 1
                    op["inc"] = 1
                else:
                    op["inc"] = 0
                op["val"] = v
        with contextlib.ExitStack() as es:
            sems = {}
            for n, k in enumerate(self.streams):
                sems[k] = es.enter_context(nc.semaphore("s%d" % n))
            block = es.enter_context(nc.Block())

            def run(eng, ename):
                for op in self.ops[ename]:
                    for k, i in op["deps"].items():
                        eng.wait_ge(sems[k], self.streams[k][i]["val"])
                    ins = op["fn"](eng)
                    if op["inc"]:
                        ins.then_inc(sems[op["skey"]], op["inc"])
                if ename == "sync":
                    for k, i in fin.items():
                        eng.wait_ge(sems[k], self.streams[k][i]["val"])

            @block.sync
            def _(eng):
                run(eng, "sync")

            @block.scalar
            def _(eng):
                run(eng, "scalar")

            @block.vector
            def _(eng):
                run(eng, "vector")

            @block.gpsimd
            def _(eng):
                run(eng, "gpsimd")

            @block.tensor
            def _(eng):
                run(eng, "tensor")


SB_WORDS = 48400


def build_program(stop_after=None, dbg=()):
    nc = bass.Bass("TRN2", target_bir_lowering=False)

    def din(name, shape, dt=F32):
        return nc.dram_tensor(name, list(shape), dt, kind="ExternalInput").ap()

    xA = din("xA", [1024, 2048])
    xB = din("xB", [1024, 2048])
    c_in = din("c_in", [128, 16])
    pos_in2 = din("pos_in", [2, 128, 16], I32)
    cst = din("cst", [128, 3072])
    kbias2 = din("kbias", [2, 128, 1024])
    pcore2 = din("pcore", [2, 128, 80])
    w_ada_a = din("w_ada", [2, 2048, 12288])
    b_ada_a = din("b_ada", [2, 128, 12288])
    w_in_a = din("w_in", [2, 2048, 3144])
    w_out_a = din("w_out", [2, 2048, 2048])
    ssm_small_a = din("ssm_small", [2, 128, 48])
    ssm_bc_a = din("ssm_bc", [2, 128, 4 * 256])
    vecs_a = din("vecs", [2, 128, 12])
    w_glu_a = din("w_glu", [2, 512, 512])
    pool_w_a = din("pool_w", [2, 512, 128])
    lnp_a = din("lnp", [2, 128, 4 * 2048])
    w_router = din("w_router", [128, 256])
    e_gate_a = din("e_gate", [2, 16, 2048, 1024])
    e_up_a = din("e_up", [2, 16, 2048, 1024])
    e_down_a = din("e_down", [2, 16, 1024, 2048])
    xout_f = nc.dram_tensor("xout", [1024, 2048], F32, kind="ExternalOutput").ap()
    xmid = nc.dram_tensor("xmid_i", [1024, 2048], F32, kind="Internal").ap()
    S1 = nc.dram_tensor("s1_i", [1024, 2048], F32, kind="Internal").ap()
    S2 = nc.dram_tensor("s2_i", [1024, 2048], F32, kind="Internal").ap()
    KS = nc.dram_tensor("ks_i", [128, 2048], BF16, kind="Internal").ap()
    VS = nc.dram_tensor("vs_i", [128, 2048], BF16, kind="Internal").ap()
    IS = nc.dram_tensor("is_i", [128, 1024], BF16, kind="Internal").ap()
    UPS = nc.dram_tensor("ups_i", [128, 512], BF16, kind="Internal").ap()
    CS = nc.dram_tensor("cs_i", [128, 32], F32, kind="Internal").ap()
    dbg_out = {}

    es = contextlib.ExitStack()
    with es:
        SB = es.enter_context(nc.sbuf_tensor("SB", [128, SB_WORDS], F32))
        PS = [es.enter_context(nc.psum_tensor("ps%d" % i, [128, 512], F32)) for i in range(8)]
        PSK = ["ps%d" % i for i in range(8)]
        PSb = [t[:, :].bitcast(BF16) for t in PS]
        p = Prog(nc)
        top = [0]
        fin = []

        def A(n, dt=F32):
            w = n if dt != BF16 else (n + 1) // 2
            assert top[0] + w <= SB_WORDS, ("SBUF overflow", top[0], w)
            v = SB[:, top[0]:top[0] + w]
            top[0] += w
            return v if dt == F32 else v.bitcast(dt)

        def V(fn, r, w):
            return p.op("vector", fn, reads=r, writes=w)

        def S(fn, r, w):
            return p.op("scalar", fn, reads=r, writes=w)

        def mm(out, lhsT, rhs, start, stop, r, w):
            return p.op("tensor", lambda e: e.matmul(out, lhsT=lhsT, rhs=rhs, start=start, stop=stop), reads=r, writes=w)

        def tr(out, in_, ident, r, w):
            return p.op("tensor", lambda e: e.transpose(out=out, in_=in_, identity=ident), reads=r, writes=w)

        def dma(out, in_, r, w, key, eng="sync", slow=False):
            if slow:
                return p.dma(eng, lambda e: e.dma_start(out=out, in_=in_, allow_slow_non_contiguous=True), reads=r, writes=w, key=key)
            return p.dma(eng, lambda e: e.dma_start(out=out, in_=in_), reads=r, writes=w, key=key)

        def dma_w(out3, in3, w, key, nsplit=4):
            n = out3.shape[1]
            step = max(1, n // nsplit)
            tok = None
            for a in range(0, n, step):
                tok = dma(out3[:, a:a + step, :], in3[:, a:a + step, :], [], w, key, eng="gpsimd")
            return tok

        def dump(name, ap, r):
            if name in dbg_out:
                fin.append(dma(dbg_out[name], ap, r, [], "dbg_" + name))

        def act(out, in_, func, r, w, **kw):
            return S(lambda e: e.activation(out=out, in_=in_, func=func, **kw), r, w)

        def tt(out, in0, in1, op, r, w):
            return V(lambda e: e.tensor_tensor(out=out, in0=in0, in1=in1, op=op), r, w)

        def ts(out, in0, s1, s2, op0, op1, r, w):
            if op1 is None:
                return V(lambda e: e.tensor_scalar(out=out, in0=in0, scalar1=s1, scalar2=None, op0=op0), r, w)
            return V(lambda e: e.tensor_scalar(out=out, in0=in0, scalar1=s1, scalar2=s2, op0=op0, op1=op1), r, w)

        def stt(out, in0, scalar, in1, op0, op1, r, w):
            return V(lambda e: e.scalar_tensor_tensor(out=out, in0=in0, scalar=scalar, in1=in1, op0=op0, op1=op1), r, w)

        def cp(out, in_, r, w):
            return V(lambda e: e.tensor_copy(out=out, in_=in_), r, w)

        def G(fn, r, w):
            return p.op("gpsimd", fn, reads=r, writes=w)

        def gtt(out, in0, in1, op, r, w):
            return G(lambda e: e.tensor_tensor(out=out, in0=in0, in1=in1, op=op), r, w)

        def gstt(out, in0, scalar, in1, op0, op1, r, w):
            return G(lambda e: e.scalar_tensor_tensor(out=out, in0=in0, scalar=scalar, in1=in1, op0=op0, op1=op1), r, w)

        def vmax(out, in_, r, w):
            return V(lambda e: e.max(out=out, in_=in_), r, w)

        def vmr(out, rep_, vals, r, w):
            return V(lambda e: e.match_replace(out=out, in_to_replace=rep_, in_values=vals, imm_value=NEG), r, w)

        def red(out, in_, op, r, w):
            return V(lambda e: e.tensor_reduce(out=out, in_=in_, axis=AX.X, op=op), r, w)

        def recip(out, in_, r, w):
            return V(lambda e: e.reciprocal(out=out, in_=in_), r, w)

        def mset(out, val, w):
            return V(lambda e: e.memset(out, val), [], w)

        cst_sb = A(256 + 1024 + 32)
        ident_f = cst_sb[:, 0:128]
        iota_f = cst_sb[:, 128:256]
        ztri = cst_sb[:, 256:1280]
        invf = cst_sb[:, 1280:1304]
        dma(cst_sb[:, 0:256], cst[:, 0:256], [], ["cst"], "cst")
        dma(cst_sb[:, 256:1280], cst[:, 1024:2048], [], ["cst"], "cst")
        dma(cst_sb[:, 1280:1312], cst[:, 2048:2080], [], ["cst"], "cst")
        cstb = A(3 * 128, BF16)
        ident_b = cstb[:, 0:128]
        tri_b = cstb[:, 128:256]
        ones_b = cstb[:, 256:384]
        dma(ident_b, cst[:, 0:128], [], ["cstb"], "cstb", eng="gpsimd")
        dma(tri_b, cst[:, 256:384], [], ["cstb"], "cstb", eng="gpsimd")
        dma(ones_b, cst[:, 384:512], [], ["cstb"], "cstb", eng="gpsimd")
        pc_sb = A(80)
        pflag = pc_sb[:, 0:1]
        invcnt16 = pc_sb[:, 16:80].rearrange("p (g t) -> p g t", t=16)
        g1p = A(2048)
        g2p = A(2048)
        modp = A(64)
        modp3 = modp.rearrange("p (v k) -> p v k", k=16)
        gates = A(128)
        gates3 = gates.rearrange("p (t e) -> p t e", e=16)
        vec_sb = A(12)
        eps_t = A(1)
        V(lambda e: e.memset(eps_t, LN_EPS), [], ["eps"])
        perm_top = top[0]

        def run_pass(l, cfg, xpre, xown, xout, do_ada, prek, ownk, outk, noprefix=False, reuse=False):
            w_ada, b_ada, w_in, w_out = w_ada_a[l], b_ada_a[l], w_in_a[l], w_out_a[l]
            ssm_small, ssm_bc, vecs, w_glu, pool_w, lnp = ssm_small_a[l], ssm_bc_a[l], vecs_a[l], w_glu_a[l], pool_w_a[l], lnp_a[l]
            e_gate, e_up, e_down = e_gate_a[l], e_up_a[l], e_down_a[l]
            pos_in, kbias_in, pcore = pos_in2[cfg], kbias2[cfg], pcore2[cfg]
            dma(pc_sb, pcore[:, :], [], ["pc"], "pc")
            dma(vec_sb, vecs[:, :], [], ["vec"], "vec")
            if do_ada:
                phase_A(w_ada, b_ada)
            phase_rest(w_in, w_out, ssm_small, ssm_bc, w_glu, pool_w, lnp, e_gate, e_up, e_down, pos_in, kbias_in, xpre, xown, xout, prek, ownk, outk, noprefix, reuse)

        def phase_A(w_ada, b_ada):
            top[0] = perm_top
            c_sb = A(16)
            cond = A(16)
            condrep = A(16 * 128, BF16)
            condrep3 = condrep.rearrange("p (k j) -> p k j", j=128)
            condf = A(16 * 128)
            condf3 = condf.rearrange("p (k j) -> p k j", j=128)
            ada = A(12288)
            wbA = [A(16 * 512, BF16) for _ in range(2)]
            wbF = [A(16 * 512) for _ in range(2)]
            tmpA = wbF[0][:, 0:2048]
            tmpA3 = tmpA.rearrange("p (k j) -> p k j", j=128)
            dma(c_sb, c_in[:, :], [], ["c_sb"], "c_sb")
            act(cond, c_sb, AF.Silu, ["c_sb"], ["cond"])
            cp(condrep3, cond.unsqueeze(2).to_broadcast([128, 16, 128]), ["cond"], ["condrep"])
            cp(condf3, cond.unsqueeze(2).to_broadcast([128, 16, 128]), ["cond"], ["condf"])
            dma(ada, b_ada[:, :], [], ["ada"], "ada")
            w_ada_r = w_ada.rearrange("(k p) n -> p k n", p=128)
            for nb in range(24):
                j = (nb // 2) % 2
                src = w_ada_r[:, :, nb * 512:(nb + 1) * 512]
                if nb % 2 == 0:
                    wv = wbA[j].rearrange("p (k n) -> p k n", n=512)
                    wk = "wbA%d" % j
                    dma_w(wv, src, [wk], wk)
                    bank, bk, lhs, lk = PS[j], PSK[j], condrep3, "condrep"
                else:
                    wv = wbF[j].rearrange("p (k n) -> p k n", n=512)
                    wk = "wbF%d" % j
                    for q, eng in enumerate(("sync", "scalar")):
                        for a in range(q * 8, q * 8 + 8, 4):
                            dma(wv[:, a:a + 4, :], src[:, a:a + 4, :], [], [wk], wk + eng, eng=eng)
                    bank, bk, lhs, lk = PS[2 + j], PSK[2 + j], condf3, "condf"
                for kc in range(16):
                    mm(bank[:, :], lhs[:, kc, :], wv[:, kc, :], kc == 0, kc == 15, [lk, wk], [bk])
                tt(ada[:, nb * 512:(nb + 1) * 512], bank[:, :], ada[:, nb * 512:(nb + 1) * 512], ALU.add, [bk, "ada"], ["ada"])
            p.barrier()
            ts(g1p, ada[:, 2 * 2048:3 * 2048], 1.0, None, ALU.add, None, ["ada"], ["g1p"])
            ts(g2p, ada[:, 5 * 2048:6 * 2048], 1.0, None, ALU.add, None, ["ada"], ["g2p"])
            for slot, idx in enumerate((0, 1, 3, 4)):
                tt(tmpA3, ada[:, idx * 2048:(idx + 1) * 2048].rearrange("p (k j) -> p k j", j=128),
                   ident_f.unsqueeze(1).to_broadcast([128, 16, 128]), ALU.mult, ["ada", "cst"], ["tmpA"])
                V(lambda e, slot=slot: e.tensor_reduce(out=modp3[:, slot, :], in_=tmpA3, axis=AX.X, op=ALU.add), ["tmpA"], ["modp"])
            ts(modp3[:, 1, :], modp3[:, 1, :], 1.0, None, ALU.add, None, ["modp"], ["modp"])
            ts(modp3[:, 3, :], modp3[:, 3, :], 1.0, None, ALU.add, None, ["modp"], ["modp"])
            dump("ada", ada, ["ada"])
            dump("modp", modp, ["modp"])
            p.barrier()
            if stop_after == "A":
                p.emit(fin)
                return nc


        def phase_rest(w_in, w_out, ssm_small, ssm_bc, w_glu, pool_w, lnp, e_gate, e_up, e_down, pos_in, kbias_in, xpre, xown, xout, prek, ownk, outk, noprefix, reuse):
            nonlocal xts
            top[0] = perm_top
            mixT = A(16 * 1024, BF16)
            mixT3 = mixT.rearrange("p (k t) -> p k t", t=1024)
            uT3 = mixT3
            mix_top = top[0]
            usT = A(4 * 2048, BF16)
            usT3 = usT.rearrange("p (c t) -> p c t", t=2048)
            upT = A(4 * 1152, BF16)
            upT3 = upT.rearrange("p (g t) -> p g t", t=1152)
            l1_top = top[0]
            qT = A(8 * 1024, BF16)
            qT3 = qT.rearrange("p (h t) -> p h t", t=1024)
            kT = A(2 * 2048, BF16)
            kT3 = kT.rearrange("p (h t) -> p h t", t=2048)
            v_sb = A(16 * 256, BF16)
            v_sb4 = v_sb.rearrange("p (b h d) -> p b h d", h=2, d=128)
            ikT2 = A(2048, BF16)
            iqT = A(4 * 1024, BF16)
            iqT3 = iqT.rearrange("p (j t) -> p j t", t=1024)
            iw_sb = A(64)
            iw3 = iw_sb.rearrange("p (t h) -> p t h", h=8)
            cosT = A(16 * 24)
            sinT = A(16 * 24)
            cos3 = cosT.rearrange("p (t f) -> p t f", f=24)
            sin3 = sinT.rearrange("p (t f) -> p t f", f=24)
            att_top = top[0]
            wb = [A(16 * 512, BF16) for _ in range(2)]
            xts = [A(2048) for _ in range(2)]
            qt = A(512)
            kt = A(256)
            rtmp = A(4 * 64)
            pos_i = A(16, I32)
            pos_f = A(16)
            ang = A(16 * 24)
            ang3 = ang.rearrange("p (t f) -> p t f", f=24)
            sc_kf = A(16 * 24)
            sc_ki = A(16 * 24, I32)
            sc_t = A(16 * 24)

            def sin_of(out, in_, addc, kf, ki, t, rk, wk, kfk, kik, tk):
                ts(t, in_, addc, None, ALU.add, None, rk, [tk])
                ts(kf, t, 1.0 / (2 * PI), None, ALU.mult, None, [tk], [kfk])
                cp(ki, kf, [kfk], [kik])
                cp(kf, ki, [kik], [kfk])
                stt(t, kf, -2 * PI, t, ALU.mult, ALU.add, [kfk, tk], [tk])
                ts(kf, t, PI, -2 * PI, ALU.is_gt, ALU.mult, [tk], [kfk])
                tt(t, t, kf, ALU.add, [tk, kfk], [tk])
                ts(kf, t, -PI, 2 * PI, ALU.is_lt, ALU.mult, [tk], [kfk])
                tt(t, t, kf, ALU.add, [tk, kfk], [tk])
                act(out, t, AF.Sin, [tk], [wk])

            dma(pos_i, pos_in[:, :], [], ["pos_i"], "pos_i")
            cp(pos_f, pos_i, ["pos_i"], ["pos_f"])
            tt(ang3, pos_f.unsqueeze(2).to_broadcast([128, 16, 24]), invf.unsqueeze(1).to_broadcast([128, 16, 24]), ALU.mult, ["pos_f", "cst"], ["ang"])
            sin_of(sinT, ang, 0.0, sc_kf, sc_ki, sc_t, ["ang"], "sinT", "sc_kf", "sc_ki", "sc_t")
            sin_of(cosT, ang, PI / 2, sc_kf, sc_ki, sc_t, ["ang"], "cosT", "sc_kf", "sc_ki", "sc_t")
            dump("cosT", cosT, ["cosT"])

            w_in_r = w_in.rearrange("(k p) n -> p k n", p=128)
            wcnt = [0]

            def load_w(col0, ncols):
                i = wcnt[0] % 2
                wcnt[0] += 1
                wv = wb[i].rearrange("p (k n) -> p k n", n=512)
                dma_w(wv[:, :, 0:ncols], w_in_r[:, :, col0:col0 + ncols], ["wb%d" % i], "wb%d" % i)
                return wv, "wb%d" % i

            def rope(x3, nh, half, tile, foff, rk):
                cs = cos3[:, tile, foff:foff + half].unsqueeze(1).to_broadcast([128, nh, half])
                sn = sin3[:, tile, foff:foff + half].unsqueeze(1).to_broadcast([128, nh, half])
                x1 = x3[:, :, 0:half]
                x2 = x3[:, :, half:2 * half]
                t = [rtmp[:, j * 64:j * 64 + nh * half].rearrange("p (h f) -> p h f", f=half) for j in range(4)]
                tt(t[0], x1, cs, ALU.mult, [rk, "cosT"], ["rt0"])
                tt(t[1], x2, sn, ALU.mult, [rk, "sinT"], ["rt1"])
                tt(t[2], x2, cs, ALU.mult, [rk, "cosT"], ["rt2"])
                tt(t[3], x1, sn, ALU.mult, [rk, "sinT"], ["rt3"])
                tt(x1, t[0], t[1], ALU.subtract, ["rt0", "rt1"], [rk])
                tt(x2, t[2], t[3], ALU.add, ["rt2", "rt3"], [rk])

            def make_uT(xsrc, slot_sh, slot_sc, srck=()):
                for t in range(8):
                    xt = xts[t % 2]
                    xk = "xt%d" % (t % 2)
                    dma(xt, xsrc[t * 128:(t + 1) * 128, :], list(srck), [xk], xk)
                    for g in range(4):
                        b = 2 + (g % 2)
                        for j in range(4):
                            kc = g * 4 + j
                            tr(PS[b][:, j * 128:(j + 1) * 128], xt[:, kc * 128:(kc + 1) * 128], ident_f, [xk, "cst"], [PSK[b]])
                        for j in range(4):
                            kc = g * 4 + j
                            o = uT3[:, kc, t * 128:(t + 1) * 128]
                            i_ = PS[b][:, j * 128:(j + 1) * 128]
                            if j % 2 == 0:
                                act(o, i_, AF.Identity, [PSK[b], "modp"], ["uT"], scale=modp3[:, slot_sc, kc:kc + 1], bias=modp3[:, slot_sh, kc:kc + 1])
                            else:
                                ts(o, i_, modp3[:, slot_sc, kc:kc + 1], modp3[:, slot_sh, kc:kc + 1], ALU.mult, ALU.add, [PSK[b], "modp"], ["uT"])

            def proj_tok(col0, ncols, consume):
                wv, wk = load_w(col0, ncols)
                for t in range(8):
                    b = t % 2
                    for kc in range(16):
                        mm(PS[b][:, 0:ncols], uT3[:, kc, t * 128:(t + 1) * 128], wv[:, kc, 0:ncols], kc == 0, kc == 15, ["uT", wk], [PSK[b]])
                    consume(t, PS[b], PSK[b])

            def proj_T(col0, nchunks, consume, blocks):
                wv, wk = load_w(col0, nchunks * 128)
                n = 0
                for cc in range(nchunks):
                    for (t0, tn) in blocks:
                        b = n % 2
                        n += 1
                        for kc in range(16):
                            mm(PS[b][:, 0:tn], wv[:, kc, cc * 128:(cc + 1) * 128], uT3[:, kc, t0:t0 + tn], kc == 0, kc == 15, ["uT", wk], [PSK[b]])
                        consume(cc, t0, tn, PS[b], PSK[b])

            def kv_consumer(tile_base):
                def f(t, bank, bk):
                    gt = tile_base + t
                    act(kt, bank[:, 0:256], AF.Copy, [bk], ["kt"])
                    act(v_sb4[:, gt, :, :], bank[:, 256:512].rearrange("p (h d) -> p h d", d=128), AF.Copy, [bk], ["v_sb"])
                    rope(kt.rearrange("p (h d) -> p h d", d=128), 2, 16, gt, 0, "kt")
                    for h in range(2):
                        tr(PS[4][:, h * 128:(h + 1) * 128], kt[:, h * 128:(h + 1) * 128], ident_f, ["kt", "cst"], [PSK[4]])
                    cp(kT3[:, :, gt * 128:(gt + 1) * 128], PS[4][:, 0:256].rearrange("p (h t) -> p h t", t=128), [PSK[4]], ["kT"])
                return f

            def ik_consumer(tile_base, own):
                def f(t, bank, bk):
                    gt = tile_base + t
                    act(kt[:, 0:64], bank[:, 0:64], AF.Copy, [bk], ["kt"])
                    if own:
                        act(iw3[:, t, :], bank[:, 64:72], AF.Copy, [bk], ["iw"])
                    rope(kt[:, 0:64].rearrange("p (h d) -> p h d", d=64), 1, 8, gt, 16, "kt")
                    cp(kt[:, 64:128], kt[:, 0:64], ["kt"], ["kt"])
                    tr(PS[5][:, 0:128], kt[:, 0:128], ident_f, ["kt", "cst"], [PSK[5]])
                    cp(ikT2[:, gt * 128:(gt + 1) * 128], PS[5][:, 0:128], [PSK[5]], ["ikT2"])
                return f

            def us_consumer(tok_base, prefix):
                def f(cc, t0, tn, bank, bk):
                    o = usT3[:, cc, tok_base + t0:tok_base + t0 + tn]
                    if prefix:
                        ts(o, bank[:, 0:tn], pflag, None, ALU.mult, None, [bk, "pc"], ["usT"])
                    else:
                        act(o, bank[:, 0:tn], AF.Copy, [bk], ["usT"])
                return f

            def up_consumer(prefix):
                def f(cc, t0, tn, bank, bk):
                    if prefix:
                        ts(upT3[:, cc, 0:128], bank[:, 0:tn], pflag, None, ALU.mult, None, [bk, "pc"], ["upT"])
                    else:
                        act(upT3[:, cc, 128 + t0:128 + t0 + tn], bank[:, 0:tn], AF.Copy, [bk], ["upT"])
                return f

            def q_consumer(g):
                def f(t, bank, bk):
                    act(qt, bank[:, :], AF.Copy, [bk], ["qt"])
                    rope(qt.rearrange("p (h d) -> p h d", d=128), 4, 16, 8 + t, 0, "qt")
                    for h in range(4):
                        tr(PS[6][:, h * 128:(h + 1) * 128], qt[:, h * 128:(h + 1) * 128], ident_f, ["qt", "cst"], [PSK[6]])
                    cp(qT3[:, 4 * g:4 * g + 4, t * 128:(t + 1) * 128], PS[6][:, :].rearrange("p (h t) -> p h t", t=128), [PSK[6]], ["qT"])
                return f

            def iq_consumer(t, bank, bk):
                act(qt, bank[:, :], AF.Copy, [bk], ["qt"])
                rope(qt.rearrange("p (h d) -> p h d", d=64), 8, 8, 8 + t, 16, "qt")
                for j in range(4):
                    tr(PS[7][:, j * 128:(j + 1) * 128], qt[:, j * 128:(j + 1) * 128], ident_f, ["qt", "cst"], [PSK[7]])
                cp(iqT3[:, :, t * 128:(t + 1) * 128], PS[7][:, :].rearrange("p (j t) -> p j t", t=128), [PSK[7]], ["iqT"])

            if reuse:
                dma(kT3[:, :, 0:1024], KS.rearrange("p (h t) -> p h t", t=1024), ["KS"], ["kT"], "ld_kT")
                dma(v_sb4[:, 0:8, :, :], VS.rearrange("p (b h d) -> p b h d", h=2, d=128), ["VS"], ["v_sb"], "ld_v")
                dma(ikT2[:, 0:1024], IS[:, :], ["IS"], ["ikT2"], "ld_ik")
                dma(upT3[:, :, 0:128], UPS.rearrange("p (g t) -> p g t", t=128), ["UPS"], ["upT"], "ld_up")
                ts(upT3[:, :, 0:128], upT3[:, :, 0:128], pflag, None, ALU.mult, None, ["upT", "pc"], ["upT"])
            elif not noprefix:
                make_uT(xpre, 0, 1, srck=[prek])
                proj_tok(OK_, 512, kv_consumer(0))
                proj_tok(OIK, 72, ik_consumer(0, False))
                proj_T(OUS, 4, us_consumer(0, True), [(0, 512), (512, 512)])
                proj_T(OUP, 4, up_consumer(True), [(896, 128)])
            else:
                mset(upT3[:, :, 0:128], 0.0, ["upT"])
            make_uT(xown, 0, 1, srck=[ownk])
            dump("uT", None, None) if False else None
            proj_tok(OQ, 512, q_consumer(0))
            proj_tok(OQ + 512, 512, q_consumer(1))
            proj_tok(OK_, 512, kv_consumer(8))
            proj_tok(OIQ, 512, iq_consumer)
            proj_tok(OIK, 72, ik_consumer(8, True))
            proj_T(OUS, 4, us_consumer(1024, False), [(0, 512), (512, 512)])
            proj_T(OUP, 4, up_consumer(False), [(0, 512), (512, 512)])
            if noprefix:
                dma(KS.rearrange("p (h t) -> p h t", t=1024), kT3[:, :, 1024:2048], ["kT"], ["KS"], "st_k")
                dma(VS.rearrange("p (b h d) -> p b h d", h=2, d=128), v_sb4[:, 8:16, :, :], ["v_sb"], ["VS"], "st_v")
                dma(IS[:, :], ikT2[:, 1024:2048], ["ikT2"], ["IS"], "st_ik")
                dma(UPS.rearrange("p (g t) -> p g t", t=128), upT3[:, :, 1024:1152], ["upT"], ["UPS"], "st_up")
            if "qT" in dbg_out:
                for nm, ap_, k_ in (("qT", qT, "qT"), ("kT", kT, "kT"), ("v_sb", v_sb, "v_sb"), ("ikT2", ikT2, "ikT2"),
                                    ("iqT", iqT, "iqT"), ("usT", usT, "usT"), ("upT", upT, "upT")):
                    fin.append(dma(dbg_out[nm], ap_, [k_], [], "dbg_" + nm, eng="gpsimd"))
                dump("iw", iw_sb, ["iw"])
            p.barrier()
            if stop_after == "BC":
                p.emit(fin)
                return nc

            top[0] = att_top
            accs = [A(2048), A(2048)]
            work = A(2048)
            rl = [A(512) for _ in range(2)]
            m8 = A(256)
            thr = A(1)
            m01 = A(2048, BF16)
            m01Ts = [A(16 * 128, BF16).rearrange("p (k q) -> p k q", q=128) for _ in range(2)]
            osb = A(512)
            dsb = A(512)
            mb_c = A(2)
            mset(mb_c[:, 0:1], 30000.0, ["mb_c"])
            mset(mb_c[:, 1:2], -30000.0, ["mb_c"])
            PTs = [A(512, BF16) for _ in range(2)]
            rden = A(512)
            kb_sb = A(1024)
            dma(kb_sb, kbias_in[:, :], [], ["kbias"], "kbias")
            SCALE = 128 ** -0.5
            k0 = 1024 if noprefix else 0
            kb0 = k0 // 128
            def geom(i):
                nk = 1024 + (i + 1) * 128 - k0
                return nk, nk // 128, (nk + 511) // 512

            cntr = [0]

            def indexer(i):
                nk, nkb, n5 = geom(i)
                acc, acck = accs[i % 2], "acc%d" % (i % 2)
                for h in range(8):
                    pr = (h % 2) * 64
                    for b5 in range(n5):
                        c0 = b5 * 512
                        w = min(512, nk - c0)
                        b = cntr[0] % 2
                        cntr[0] += 1
                        mm(PS[b][:, 0:w], iqT3[pr:pr + 64, h // 2, i * 128:(i + 1) * 128], ikT2[pr:pr + 64, k0 + c0:k0 + c0 + w], True, True, ["iqT", "ikT2"], [PSK[b]])
                        act(rl[b][:, 0:w], PS[b][:, 0:w], AF.Relu, [PSK[b]], ["rl%d" % b])
                        if h == 0:
                            if k0 + c0 < 1024:
                                in1 = kb_sb[:, c0:c0 + w]
                            else:
                                o0 = 896 - i * 128 + (k0 + c0 - 1024)
                                in1 = ztri[:, o0:o0 + w]
                            stt(acc[:, c0:c0 + w], rl[b][:, 0:w], iw3[:, i, h:h + 1], in1, ALU.mult, ALU.add, ["rl%d" % b, "iw", "kbias", "cst"], [acck])
                        else:
                            stt(acc[:, c0:c0 + w], rl[b][:, 0:w], iw3[:, i, h:h + 1], acc[:, c0:c0 + w], ALU.mult, ALU.add, ["rl%d" % b, "iw", acck], [acck])

            def topk(i):
                nk, nkb, n5 = geom(i)
                acc, acck = accs[i % 2], "acc%d" % (i % 2)
                if nk > 256:
                    cur, ck = acc, acck
                    for r in range(32):
                        vmax(m8[:, r * 8:(r + 1) * 8], cur[:, 0:nk], [ck], ["m8"])
                        if r < 31:
                            vmr(work[:, 0:nk], m8[:, r * 8:(r + 1) * 8], cur[:, 0:nk], [ck, "m8"], ["work"])
                            cur, ck = work, "work"
                    ts(thr, m8[:, 255:256], -1e29, None, ALU.max, None, ["m8"], ["thr"])
                    ts(m01[:, 0:nk], acc[:, 0:nk], thr[:, 0:1], None, ALU.is_ge, None, [acck, "thr"], ["m01"])
                else:
                    ts(m01[:, 0:nk], acc[:, 0:nk], -1e29, None, ALU.is_ge, None, [acck], ["m01"])

            def attn(i):
                nk, nkb, n5 = geom(i)
                m01T3, m01Tk = m01Ts[i % 2], "m01T%d" % (i % 2)
                for g0 in range(0, nkb, 8):
                    gn = min(8, nkb - g0)
                    b = 2 + (g0 // 8) % 2
                    for j in range(gn):
                        kb = g0 + j
                        tr(PSb[b][:, j * 128:(j + 1) * 128], m01[:, kb * 128:(kb + 1) * 128], ident_b, ["m01", "cstb"], [PSK[b]])
                    act(m01T3[:, g0:g0 + gn, :], PSb[b][:, 0:gn * 128].rearrange("p (k q) -> p k q", q=128), AF.Identity, [PSK[b], "mb_c"], [m01Tk],
                        scale=mb_c[:, 0:1], bias=mb_c[:, 1:2])
                for kvh in range(2):
                    for kb in range(nkb):
                        gkb = kb0 + kb
                        sb_ = 4 + kb % 2
                        pk = "PT%d" % (kb % 2)
                        PT = PTs[kb % 2]
                        PT3 = PT.rearrange("p (h q) -> p h q", q=128)
                        mm(PS[sb_][:, :], kT3[:, kvh, gkb * 128:(gkb + 1) * 128], qT3[:, 4 * kvh:4 * kvh + 4, i * 128:(i + 1) * 128], True, False, ["kT", "qT"], [PSK[sb_]])
                        for hh in range(4):
                            mm(PS[sb_][:, hh * 128:(hh + 1) * 128], ident_b, m01T3[:, kb, :], False, True, ["cstb", m01Tk], [PSK[sb_]])
                        act(PT, PS[sb_][:, :], AF.Exp, [PSK[sb_]], [pk], scale=SCALE)
                        mm(PS[6][:, :], v_sb4[:, gkb, kvh, :], PT, kb == 0, kb == nkb - 1, ["v_sb", pk], [PSK[6]])
                        mm(PS[7][:, :], ones_b, PT, kb == 0, kb == nkb - 1, ["cstb", pk], [PSK[7]])
                    act(osb, PS[6][:, :], AF.Copy, [PSK[6]], ["osb"])
                    act(dsb, PS[7][:, :], AF.Ln, [PSK[7]], ["dsb"])
                    act(dsb, dsb, AF.Exp, ["dsb"], ["dsb"], scale=-1.0)
                    gtt(mixT3[:, 4 * kvh:4 * kvh + 4, i * 128:(i + 1) * 128], osb.rearrange("p (h q) -> p h q", q=128),
                        dsb.rearrange("p (h q) -> p h q", q=128), ALU.mult, ["osb", "dsb"], ["mixT"])

            indexer(0)
            for i in range(8):
                if i + 1 < 8:
                    indexer(i + 1)
                topk(i)
                attn(i)
            p.barrier()
            if stop_after == "EF":
                if "mixT" in dbg_out:
                    fin.append(dma(dbg_out["mixT"], mixT, ["mixT"], [], "dbg_mixT", eng="gpsimd"))
                p.emit(fin)
                return nc

            top[0] = l1_top
            sm = A(48)
            dma(sm, ssm_small[:, :], [], ["sm"], "sm")
            lam_re, lam_im, lstep = sm[:, 0:16], sm[:, 16:32], sm[:, 32:48]
            PTre = A(2048)
            PTim = A(2048)
            PinvRe = A(2048)
            PinvIm = A(2048)
            W_B = [A(4 * 512, BF16) for _ in range(2)]
            Wc = [A(16 * 128, BF16) for _ in range(2)]
            wglu_sb = A(4 * 512, BF16)
            sv = A(16 * 24)
            ki16 = A(16, I32)
            ssm_tmp = top[0]
            bc_sb = A(1024)
            dma(bc_sb, ssm_bc[:, :], [], ["bc_sb"], "bc_sb")
            b_re3 = bc_sb[:, 0:256].rearrange("p (s c) -> p s c", c=16)
            b_im3 = bc_sb[:, 256:512].rearrange("p (s c) -> p s c", c=16)
            c_re3 = bc_sb[:, 512:768].rearrange("p (s c) -> p s c", c=16)
            c_im3 = bc_sb[:, 768:1024].rearrange("p (s c) -> p s c", c=16)
            svn = [0]

            def SV():
                v = sv[:, svn[0] * 16:(svn[0] + 1) * 16]
                svn[0] += 1
                return v
            dt_, ar, th = SV(), SV(), SV()
            act(dt_, lstep, AF.Exp, ["sm"], ["dt"])
            tt(ar, lam_re, dt_, ALU.mult, ["sm", "dt"], ["ar"])
            tt(th, lam_im, dt_, ALU.mult, ["sm", "dt"], ["th"])
            PIre = A(2048)
            PIim = A(2048)
            big1 = A(2048)
            big2 = A(2048)
            big3 = A(2048)
            bigi = A(2048, I32)
            B3 = lambda v: v.rearrange("p (s t) -> p s t", t=128)
            iota_b = iota_f.unsqueeze(1).to_broadcast([128, 16, 128])
            tt(B3(big1), th.unsqueeze(2).to_broadcast([128, 16, 128]), iota_b, ALU.mult, ["th", "cst"], ["big1"])
            sin_of(PIim, big1, 0.0, big2, bigi, big3, ["big1"], "sinS", "big2", "bigi", "big3")
            sin_of(PIre, big1, PI / 2, big2, bigi, big3, ["big1"], "cosS", "big2", "bigi", "big3")
            tt(B3(big1), ar.unsqueeze(2).to_broadcast([128, 16, 128]), iota_b, ALU.mult, ["ar", "cst"], ["big1"])
            act(big2, big1, AF.Exp, ["big1"], ["big2"])
            act(big3, big1, AF.Exp, ["big1"], ["big3"], scale=-1.0)
            tt(PTre, big2, PIre, ALU.mult, ["big2", "cosS"], ["PTre"])
            tt(PTim, big2, PIim, ALU.mult, ["big2", "sinS"], ["PTim"])
            tt(PIre, big3, PIre, ALU.mult, ["big3", "cosS"], ["cosS"])
            stt(PIim, big3, -1.0, PIim, ALU.mult, ALU.mult, ["big3", "sinS"], ["sinS"])
            for comp, (src, sk, dst, dk) in enumerate(((PIre, "cosS", PinvRe, "PinvRe"), (PIim, "sinS", PinvIm, "PinvIm"))):
                for g in range(4):
                    b = 2 * comp + (g % 2)
                    for j in range(4):
                        sb_i = g * 4 + j
                        tr(PS[b][:, j * 128:(j + 1) * 128], src[:, sb_i * 128:(sb_i + 1) * 128], ident_f, [sk, "cst"], [PSK[b]])
                    cp(dst[:, g * 512:(g + 1) * 512], PS[b][:, :], [PSK[b]], [dk])
            L_re, L_im = SV(), SV()
            a128, k1, t1_, mg = SV(), SV(), SV(), SV()
            ts(a128, th, 128.0, None, ALU.mult, None, ["th"], ["a128"])
            sin_of(L_im, a128, 0.0, k1, ki16, t1_, ["a128"], "Lsin", "k1", "ki16", "t1_")
            sin_of(L_re, a128, PI / 2, k1, ki16, t1_, ["a128"], "Lcos", "k1", "ki16", "t1_")
            act(mg, ar, AF.Exp, ["ar"], ["mg"], scale=128.0)
            tt(L_re, L_re, mg, ALU.mult, ["Lcos", "mg"], ["Lcos"])
            tt(L_im, L_im, mg, ALU.mult, ["Lsin", "mg"], ["Lsin"])
            PTre3, PTim3 = B3(PTre), B3(PTim)
            a_, b_, den_, m_re, m_im, u1, u2 = SV(), SV(), SV(), SV(), SV(), SV(), SV()
            ts(a_, PTre3[:, :, 1], -1.0, None, ALU.add, None, ["PTre"], ["a_"])
            cp(b_, PTim3[:, :, 1], ["PTim"], ["b_"])
            tt(u1, lam_re, lam_re, ALU.mult, ["sm"], ["u1"])
            tt(u2, lam_im, lam_im, ALU.mult, ["sm"], ["u2"])
            tt(den_, u1, u2, ALU.add, ["u1", "u2"], ["den_"])
            V(lambda e: e.reciprocal(out=den_, in_=den_), ["den_"], ["den_"])
            tt(u1, a_, lam_re, ALU.mult, ["a_", "sm"], ["u1"])
            tt(u2, b_, lam_im, ALU.mult, ["b_", "sm"], ["u2"])
            tt(m_re, u1, u2, ALU.add, ["u1", "u2"], ["m_re"])
            tt(m_re, m_re, den_, ALU.mult, ["m_re", "den_"], ["m_re"])
            tt(u1, b_, lam_re, ALU.mult, ["b_", "sm"], ["u1"])
            tt(u2, a_, lam_im, ALU.mult, ["a_", "sm"], ["u2"])
            tt(m_im, u1, u2, ALU.subtract, ["u1", "u2"], ["m_im"])
            tt(m_im, m_im, den_, ALU.mult, ["m_im", "den_"], ["m_im"])
            p.barrier()
            bigf = bigi.bitcast(F32)
            Bb_re, Bb_im, bt1, bt2 = bigf[:, 0:256], bigf[:, 256:512], bigf[:, 512:768], bigf[:, 768:1024]
            S3 = lambda v: v.rearrange("p (s c) -> p s c", c=16)
            mre_b = m_re.unsqueeze(2).to_broadcast([128, 16, 16])
            mim_b = m_im.unsqueeze(2).to_broadcast([128, 16, 16])
            tt(S3(bt1), b_re3, mre_b, ALU.mult, ["bc_sb", "m_re"], ["bt1"])
            tt(S3(bt2), b_im3, mim_b, ALU.mult, ["bc_sb", "m_im"], ["bt2"])
            tt(Bb_re, bt1, bt2, ALU.subtract, ["bt1", "bt2"], ["Bb_re"])
            tt(S3(bt1), b_im3, mre_b, ALU.mult, ["bc_sb", "m_re"], ["bt1"])
            tt(S3(bt2), b_re3, mim_b, ALU.mult, ["bc_sb", "m_im"], ["bt2"])
            tt(Bb_im, bt1, bt2, ALU.add, ["bt1", "bt2"], ["Bb_im"])
            for comp, (srcB, kB, srcC, cneg) in enumerate(((Bb_re, "Bb_re", c_re3, 1.0), (Bb_im, "Bb_im", c_im3, -1.0))):
                wide = big1 if comp == 0 else big2
                wk = "big1" if comp == 0 else "big2"
                V(lambda e, wide=wide: e.memset(wide, 0.0), [], [wk])
                w4 = wide.rearrange("p (a j c) -> p a j c", j=4, c=128)
                s4 = srcB.rearrange("p (a j c) -> p a j c", j=4, c=16)
                for j in range(4):
                    for gl in range(2):
                        cp(w4[gl * 64:(gl + 1) * 64, :, j, j * 32 + gl * 16:j * 32 + gl * 16 + 16], s4[gl * 64:(gl + 1) * 64, :, j, :], [kB], [wk])
                for g in range(4):
                    b = 4 + (g % 2)
                    for j in range(4):
                        sb_i = g * 4 + j
                        tr(PS[b][:, j * 128:(j + 1) * 128], wide[:, sb_i * 128:(sb_i + 1) * 128], ident_f, [wk, "cst"], [PSK[b]])
                    cp(W_B[comp][:, g * 512:(g + 1) * 512], PS[b][:, :], [PSK[b]], ["W_B%d" % comp])
                wide2 = big3 if comp == 0 else PIre
                wk2 = "big3" if comp == 0 else "cosS"
                V(lambda e, wide2=wide2: e.memset(wide2, 0.0), [], [wk2])
                w4 = wide2.rearrange("p (a j c) -> p a j c", j=4, c=128)
                s4 = srcC.rearrange("p (a j) c -> p a j c", j=4)
                for j in range(4):
                    for gl in range(2):
                        ts(w4[gl * 64:(gl + 1) * 64, :, j, j * 32 + gl * 16:j * 32 + gl * 16 + 16], s4[gl * 64:(gl + 1) * 64, :, j, :], cneg, None, ALU.mult, None, ["bc_sb"], [wk2])
                cp(Wc[comp], wide2, [wk2], ["Wc%d" % comp])
            p.barrier()
            top[0] = ssm_tmp
            wglu3 = wglu_sb.rearrange("p (k n) -> p k n", n=512)
            dma(wglu3, w_glu.rearrange("(k p) n -> p k n", p=128), [], ["wglu"], "wglu", eng="gpsimd")
            Xre = [A(2048, BF16) for _ in range(2)]
            Xim = [A(2048, BF16) for _ in range(2)]
            sTre = A(2048, BF16)
            sTim = A(2048, BF16)
            yg = A(4 * 1024, BF16)
            yg3 = yg.rearrange("p (c t) -> p c t", t=1024)
            tq = [A(512) for _ in range(4)]
            Are, Aim = A(512), A(512)
            car_re, car_im, al_re, al_im, cu1, cu2 = SV(), SV(), SV(), SV(), SV(), SV()
            yv = A(512)
            if reuse:
                dma(car_re, CS[:, 0:16], ["CS"], ["car_re"], "ld_cre")
                dma(car_im, CS[:, 16:32], ["CS"], ["car_im"], "ld_cim")
                ts(car_re, car_re, pflag, None, ALU.mult, None, ["car_re", "pc"], ["car_re"])
                ts(car_im, car_im, pflag, None, ALU.mult, None, ["car_im", "pc"], ["car_im"])
            else:
                V(lambda e: e.memset(car_re, 0.0), [], ["car_re"])
                V(lambda e: e.memset(car_im, 0.0), [], ["car_im"])
            for j in range(8 if (noprefix or reuse) else 0, 16):
                own = j >= 8
                xr, xi = Xre[j % 2], Xim[j % 2]
                xrk, xik = "Xre%d" % (j % 2), "Xim%d" % (j % 2)
                for cc in range(4):
                    sl = slice(cc * 512, (cc + 1) * 512)
                    mm(PS[0][:, :], usT3[:, cc, j * 128:(j + 1) * 128], W_B[0][:, sl], True, True, ["usT", "W_B0"], [PSK[0]])
                    mm(PS[1][:, :], usT3[:, cc, j * 128:(j + 1) * 128], W_B[1][:, sl], True, True, ["usT", "W_B1"], [PSK[1]])
                    tt(tq[0], PS[0][:, :], PinvRe[:, sl], ALU.mult, [PSK[0], "PinvRe"], ["tq0"])
                    tt(tq[1], PS[1][:, :], PinvIm[:, sl], ALU.mult, [PSK[1], "PinvIm"], ["tq1"])
                    tt(tq[2], PS[1][:, :], PinvRe[:, sl], ALU.mult, [PSK[1], "PinvRe"], ["tq2"])
                    tt(tq[3], PS[0][:, :], PinvIm[:, sl], ALU.mult, [PSK[0], "PinvIm"], ["tq3"])
                    tt(xr[:, sl], tq[0], tq[1], ALU.subtract, ["tq0", "tq1"], [xrk])
                    tt(xi[:, sl], tq[2], tq[3], ALU.add, ["tq2", "tq3"], [xik])
                if not own:
                    for sb_i in range(16):
                        mm(PS[2][:, sb_i:sb_i + 1], xr[:, sb_i * 128:(sb_i + 1) * 128], ones_b[:, 0:1], True, True, [xrk, "cstb"], [PSK[2]])
                        mm(PS[3][:, sb_i:sb_i + 1], xi[:, sb_i * 128:(sb_i + 1) * 128], ones_b[:, 0:1], True, True, [xik, "cstb"], [PSK[3]])
                    tt(al_re, PS[2][:, 0:16], car_re, ALU.add, [PSK[2], "car_re"], ["al_re"])
                    tt(al_im, PS[3][:, 0:16], car_im, ALU.add, [PSK[3], "car_im"], ["al_im"])
                else:
                    tl = j - 8
                    for g in range(4):
                        for jj in range(4):
                            sb_i = g * 4 + jj
                            mm(PS[2][:, jj * 128:(jj + 1) * 128], xr[:, sb_i * 128:(sb_i + 1) * 128], tri_b, True, True, [xrk, "cstb"], [PSK[2]])
                            mm(PS[3][:, jj * 128:(jj + 1) * 128], xi[:, sb_i * 128:(sb_i + 1) * 128], tri_b, True, True, [xik, "cstb"], [PSK[3]])
                        gs = slice(g * 512, (g + 1) * 512)
                        A3 = lambda v: v.rearrange("p (s t) -> p s t", t=128)
                        tt(A3(Are), A3(PS[2][:, :]), car_re[:, g * 4:(g + 1) * 4].unsqueeze(2).to_broadcast([128, 4, 128]), ALU.add, [PSK[2], "car_re"], ["Are"])
                        tt(A3(Aim), A3(PS[3][:, :]), car_im[:, g * 4:(g + 1) * 4].unsqueeze(2).to_broadcast([128, 4, 128]), ALU.add, [PSK[3], "car_im"], ["Aim"])
                        cp(al_re[:, g * 4:(g + 1) * 4], A3(Are)[:, :, 127], ["Are"], ["al_re"])
                        cp(al_im[:, g * 4:(g + 1) * 4], A3(Aim)[:, :, 127], ["Aim"], ["al_im"])
                        tt(tq[0], PTre[:, gs], Are, ALU.mult, ["PTre", "Are"], ["tq0"])
                        tt(tq[1], PTim[:, gs], Aim, ALU.mult, ["PTim", "Aim"], ["tq1"])
                        tt(tq[2], PTre[:, gs], Aim, ALU.mult, ["PTre", "Aim"], ["tq2"])
                        tt(tq[3], PTim[:, gs], Are, ALU.mult, ["PTim", "Are"], ["tq3"])
                        tt(sTre[:, gs], tq[0], tq[1], ALU.subtract, ["tq0", "tq1"], ["sTre"])
                        tt(sTim[:, gs], tq[2], tq[3], ALU.add, ["tq2", "tq3"], ["sTim"])
                    for cc in range(4):
                        for jj in range(4):
                            sb_i = cc * 4 + jj
                            mm(PS[4][:, cc * 128:(cc + 1) * 128], Wc[0][:, sb_i * 128:(sb_i + 1) * 128], sTre[:, sb_i * 128:(sb_i + 1) * 128], jj == 0, False, ["Wc0", "sTre"], [PSK[4]])
                            mm(PS[4][:, cc * 128:(cc + 1) * 128], Wc[1][:, sb_i * 128:(sb_i + 1) * 128], sTim[:, sb_i * 128:(sb_i + 1) * 128], False, jj == 3, ["Wc1", "sTim"], [PSK[4]])
                    for cc in range(4):
                        stt(yv[:, cc * 128:(cc + 1) * 128], usT3[:, cc, j * 128:(j + 1) * 128], vec_sb[:, cc:cc + 1], PS[4][:, cc * 128:(cc + 1) * 128], ALU.mult, ALU.add, ["usT", "vec", PSK[4]], ["yv"])
                    act(yg3[:, :, tl * 128:(tl + 1) * 128], yv.rearrange("p (c t) -> p c t", t=128), AF.Gelu, ["yv"], ["yg"])
                tt(cu1, L_re, al_re, ALU.mult, ["Lcos", "al_re"], ["cu1"])
                tt(cu2, L_im, al_im, ALU.mult, ["Lsin", "al_im"], ["cu2"])
                tt(car_re, cu1, cu2, ALU.subtract, ["cu1", "cu2"], ["car_re"])
                tt(cu1, L_re, al_im, ALU.mult, ["Lcos", "al_im"], ["cu1"])
                tt(cu2, L_im, al_re, ALU.mult, ["Lsin", "al_re"], ["cu2"])
                tt(car_im, cu1, cu2, ALU.add, ["cu1", "cu2"], ["car_im"])
            if noprefix:
                dma(CS[:, 0:16], car_re, ["car_re"], ["CS"], "st_c")
                dma(CS[:, 16:32], car_im, ["car_im"], ["CS"], "st_c")
            sg = [A(512, BF16) for _ in range(2)]
            n = 0
            for co in range(4):
                for tb in range(2):
                    b = n % 2
                    n += 1
                    for cc in range(4):
                        mm(PS[b][:, :], wglu3[:, cc, co * 128:(co + 1) * 128], yg3[:, cc, tb * 512:(tb + 1) * 512], cc == 0, cc == 3, ["wglu", "yg"], [PSK[b]])
                    act(sg[b], PS[b][:, :], AF.Sigmoid, [PSK[b], "vec"], ["sg%d" % b], bias=vec_sb[:, 4 + co:5 + co])
                    tt(mixT3[:, 8 + co, tb * 512:(tb + 1) * 512], sg[b], yg3[:, co, tb * 512:(tb + 1) * 512], ALU.mult, ["sg%d" % b, "yg"], ["mixT"])
            p.barrier()

            top[0] = l1_top
            pw_sb = A(4 * 128, BF16)
            pw3 = pw_sb.rearrange("p (g d) -> p g d", d=128)
            dma(pw3, pool_w.rearrange("(g c) d -> c g d", c=128), [], ["pw"], "pw", eng="gpsimd")
            xf = A(1152)
            pa = A(1152)
            pb = A(1152)
            pl = A(1024, BF16)
            for g in range(4):
                win = 2 ** (g + 1)
                cp(xf, upT3[:, g, :], ["upT"], ["xf"])
                src, sk = xf, "xf"
                bufs = [(pa, "pa"), (pb, "pb")]
                sh = 1
                for s in range(g + 1):
                    dst, dk = bufs[s % 2]
                    tt(dst[:, 16:1152], src[:, 16:1152], src[:, 16 - sh:1152 - sh], ALU.add, [sk], [dk])
                    src, sk = dst, dk
                    sh *= 2
                dst, dk = bufs[(g + 1) % 2]
                stt(dst[:, 128:1152], src[:, 128:1152], 1.0 / win, xf[:, 128:1152], ALU.mult, ALU.subtract, [sk, "xf"], [dk])
                tt(dst[:, 128:144], src[:, 128:144], invcnt16[:, g, :], ALU.mult, [sk, "pc"], [dk])
                tt(dst[:, 128:144], dst[:, 128:144], xf[:, 128:144], ALU.subtract, [dk, "xf"], [dk])
                cp(pl, dst[:, 128:1152], [dk], ["pl"])
                for tb in range(2):
                    b = tb
                    mm(PS[b][:, :], pw3[:, g, :], pl[:, tb * 512:(tb + 1) * 512], True, True, ["pw", "pl"], [PSK[b]])
                    ts(mixT3[:, 12 + g, tb * 512:(tb + 1) * 512], PS[b][:, :], vec_sb[:, 8 + g:9 + g], None, ALU.mult, None, [PSK[b], "vec"], ["mixT"])
            p.barrier()
            if "mixT" in dbg_out:
                fin.append(dma(dbg_out["mixT"], mixT, ["mixT"], [], "dbg_mixT", eng="gpsimd"))
                p.barrier()
            if stop_after == "H":
                p.emit(fin)
                return nc

            top[0] = mix_top
            wbI = [A(16 * 512, BF16) for _ in range(2)]
            rbuf = A(8 * 2048)
            r3 = rbuf.rearrange("p (t c) -> p t c", c=2048)
            xch = [A(512) for _ in range(2)]
            tmpI = A(512)
            lng = A(2048)
            lnb = A(2048)
            u2Tf = A(16 * 128)
            u2Tf3 = u2Tf.rearrange("p (k t) -> p k t", t=128)
            wr_sb = A(256)
            wr_hi = A(256, BF16)
            wr_lo = A(256, BF16)
            wrh3 = wr_hi.rearrange("p (k e) -> p k e", e=16)
            wrl3 = wr_lo.rearrange("p (k e) -> p k e", e=16)
            st_ = A(64)
            dma(lng, lnp[:, 0:2048], [], ["lng"], "lng")
            dma(lnb, lnp[:, 2048:4096], [], ["lnb"], "lnb")
            dma(wr_sb, w_router[:, :], [], ["wr"], "wr")
            cp(wr_hi, wr_sb, ["wr"], ["wr_hi"])
            tt(wr_lo, wr_sb, wr_hi, ALU.subtract, ["wr", "wr_hi"], ["wr_lo"])
            w_out_r = w_out.rearrange("(k p) n -> p k n", p=128)
            n = 0
            for cg in range(4):
                wv = wbI[cg % 2].rearrange("p (k n) -> p k n", n=512)
                wk = "wbI%d" % (cg % 2)
                dma_w(wv, w_out_r[:, :, cg * 512:(cg + 1) * 512], [wk], wk)
                for t in range(8):
                    b = n % 2
                    xc = xch[n % 2]
                    xck = "xch%d" % (n % 2)
                    n += 1
                    dma(xc, xown[t * 128:(t + 1) * 128, cg * 512:(cg + 1) * 512], [ownk], [xck], xck)
                    for kc in range(16):
                        mm(PS[b][:, :], mixT3[:, kc, t * 128:(t + 1) * 128], wv[:, kc, :], kc == 0, kc == 15, ["mixT", wk], [PSK[b]])
                    tt(tmpI, PS[b][:, :], g1p[:, cg * 512:(cg + 1) * 512], ALU.mult, [PSK[b], "g1p"], ["tmpI"])
                    stt(r3[:, t, cg * 512:(cg + 1) * 512], xc, ALU_ALPHA, tmpI, ALU.mult, ALU.add, [xck, "tmpI"], ["r%d" % t])
            p.barrier()

            def layer_norm(x_ap, xk, g_ap, b_ap, gk, bk, sidx, junk, junkk):
                base = sidx * 32
                stats = st_[:, base:base + 24]
                mv = st_[:, base + 24:base + 26]
                rstd = st_[:, base + 26:base + 27]
                sk_ = "st%d" % sidx
                for c in range(4):
                    V(lambda e, c=c: e.bn_stats(out=stats[:, c * 6:(c + 1) * 6], in_=x_ap[:, c * 512:(c + 1) * 512]), [xk], [sk_])
                V(lambda e: e.bn_aggr(out=mv, in_=stats), [sk_], [sk_])
                act(rstd, mv[:, 1:2], AF.Sqrt, [sk_, "eps"], [sk_], scale=1.0, bias=eps_t[:, 0:1])
                recip(rstd, rstd, [sk_], [sk_])
                stt(x_ap, x_ap, mv[:, 0:1], g_ap, ALU.subtract, ALU.mult, [xk, sk_, gk], [xk])
                stt(x_ap, x_ap, rstd, b_ap, ALU.mult, ALU.add, [xk, sk_, bk], [xk])

            u2T3 = mixT3
            rt_ = A(16 * 8)
            for t in range(8):
                xk = "r%d" % t
                x1 = r3[:, t, :]
                layer_norm(x1, xk, lng, lnb, "lng", "lnb", t % 2, u2Tf, "u2Tf")
                dma(xmid[t * 128:(t + 1) * 128, :], x1, [xk], ["xmid"], "xmid_w")
            p.barrier()
            xts = [u2Tf, lng]
            make_uT(xmid, 2, 3, srck=["xmid"])
            for t in range(8):
                for kc in range(16):
                    hi_ = u2T3[:, kc, t * 128:(t + 1) * 128]
                    mm(PS[4][:, 0:16], hi_, wrh3[:, kc, :], kc == 0, False, ["uT", "wr_hi"], [PSK[4]])
                    mm(PS[4][:, 0:16], hi_, wrl3[:, kc, :], False, kc == 15, ["uT", "wr_lo"], [PSK[4]])
                lg = rt_[:, 0:16]
                mx = rt_[:, 16:17]
                ex = rt_[:, 32:48]
                pr6 = rt_[:, 48:72]
                gsc = rt_[:, 72:76]
                gmx = rt_[:, 76:77]
                goh = rt_[:, 80:84]
                eg = rt_[:, 84:100]
                m1 = rt_[:, 100:101]
                m2 = rt_[:, 101:102]
                eg2 = rt_[:, 104:120]
                msk = rt_[:, 120:128] if False else None
                cp(lg, PS[4][:, 0:16], [PSK[4]], ["rt"])
                if stop_after == "I3":
                    cp(gates3[:, t, :], lg, ["rt"], ["gates"])
                    continue
                V(lambda e: e.tensor_reduce(out=mx, in_=lg, axis=AX.X, op=ALU.max), ["rt"], ["rt"])
                ts(lg, lg, mx, None, ALU.subtract, None, ["rt"], ["rt"])
                act(ex, lg, AF.Exp, ["rt"], ["rt"])
                ex3 = ex.rearrange("p (g j) -> p g j", j=4)
                pr63 = pr6.rearrange("p (g k) -> p g k", k=6)
                kk = 0
                for a in range(4):
                    for bq in range(a + 1, 4):
                        tt(pr63[:, :, kk], ex3[:, :, a], ex3[:, :, bq], ALU.add, ["rt"], ["rt"])
                        kk += 1
                V(lambda e: e.tensor_reduce(out=gsc, in_=pr63, axis=AX.X, op=ALU.max), ["rt"], ["rt"])
                V(lambda e: e.tensor_reduce(out=gmx, in_=gsc, axis=AX.X, op=ALU.max), ["rt"], ["rt"])
                ts(goh, gsc, gmx, None, ALU.is_ge, None, ["rt"], ["rt"])
                tt(eg.rearrange("p (g j) -> p g j", j=4), ex3, goh.unsqueeze(2).to_broadcast([128, 4, 4]), ALU.mult, ["rt"], ["rt"])
                V(lambda e: e.tensor_reduce(out=m1, in_=eg, axis=AX.X, op=ALU.max), ["rt"], ["rt"])
                ts(eg2, eg, m1, None, ALU.is_lt, None, ["rt"], ["rt"])
                tt(eg2, eg2, eg, ALU.mult, ["rt"], ["rt"])
                V(lambda e: e.tensor_reduce(out=m2, in_=eg2, axis=AX.X, op=ALU.max), ["rt"], ["rt"])
                ts(eg2, eg, m2, None, ALU.is_ge, None, ["rt"], ["rt"])
                tt(eg2, eg2, eg, ALU.mult, ["rt"], ["rt"])
                tt(m1, m1, m2, ALU.add, ["rt"], ["rt"])
                V(lambda e: e.reciprocal(out=m1, in_=m1), ["rt"], ["rt"])
                ts(gates3[:, t, :], eg2, m1, None, ALU.mult, None, ["rt"], ["gates"])
            dump("gates", gates, ["gates"])
            p.barrier()
            if stop_after in ("I", "I1", "I2", "I3"):
                fin.append(("dma", "xmid_w") and p.res["xmid"][0])
                p.emit(fin)
                return nc

            top[0] = mix_top
            accm = A(8 * 2048)
            acc3 = accm.rearrange("p (t c) -> p t c", c=2048)
            hT = A(8 * 1024, BF16)
            hT3 = hT.rearrange("p (f t) -> p f t", t=1024)
            NB = 3
            wg = [A(16 * 256, BF16) for _ in range(2)]
            wu = [A(16 * 256, BF16) for _ in range(2)]
            wd = [A(8 * 512, BF16) for _ in range(2)]
            sgm = [A(512, BF16) for _ in range(2)]
            moe_top = top[0]
            mset(accm, 0.0, ["acc_%d" % t for t in range(8)])
            ng = 0
            nd_ = 0
            nps = 0
            for ex_i in range(16):
                eg_r = e_gate[ex_i].rearrange("(k p) f -> p k f", p=128)
                eu_r = e_up[ex_i].rearrange("(k p) f -> p k f", p=128)
                ed_r = e_down[ex_i].rearrange("(f p) c -> p f c", p=128)
                for fcp in range(4):
                    i = ng % 2
                    ng += 1
                    wgv = wg[i].rearrange("p (k f) -> p k f", f=256)
                    wuv = wu[i].rearrange("p (k f) -> p k f", f=256)
                    dma_w(wgv, eg_r[:, :, fcp * 256:(fcp + 1) * 256], ["wg%d" % i], "wg%d" % i)
                    dma_w(wuv, eu_r[:, :, fcp * 256:(fcp + 1) * 256], ["wu%d" % i], "wu%d" % i)
                    for sub in range(2):
                        fc = fcp * 2 + sub
                        fsl = slice(sub * 128, (sub + 1) * 128)
                        for th_ in range(2):
                            tsl = slice(th_ * 512, (th_ + 1) * 512)
                            bg, bu = 0 + (nps % 2), 2 + (nps % 2)
                            s_ = sgm[nps % 2]
                            sk_ = "sgm%d" % (nps % 2)
                            nps += 1
                            for kc in range(16):
                                mm(PS[bg][:, :], wgv[:, kc, fsl], u2T3[:, kc, tsl], kc == 0, kc == 15, ["wg%d" % i, "uT"], [PSK[bg]])
                            for kc in range(16):
                                mm(PS[bu][:, :], wuv[:, kc, fsl], u2T3[:, kc, tsl], kc == 0, kc == 15, ["wu%d" % i, "uT"], [PSK[bu]])
                            act(s_, PS[bg][:, :], AF.Silu, [PSK[bg]], [sk_])
                            tt(hT3[:, fc, tsl], s_, PS[bu][:, :], ALU.mult, [sk_, PSK[bu]], ["hT"])
                for cg in range(4):
                    i = nd_ % 2
                    wdv = wd[i].rearrange("p (f c) -> p f c", c=512)
                    dma_w(wdv, ed_r[:, :, cg * 512:(cg + 1) * 512], ["wd%d" % i], "wd%d" % i, nsplit=2)
                    for t in range(8):
                        b = 4 + (nd_ * 8 + t) % 4
                        for fc in range(8):
                            mm(PS[b][:, :], hT3[:, fc, t * 128:(t + 1) * 128], wdv[:, fc, :], fc == 0, fc == 7, ["hT", "wd%d" % i], [PSK[b]])
                        stt(acc3[:, t, cg * 512:(cg + 1) * 512], PS[b][:, :], gates3[:, t, ex_i:ex_i + 1], acc3[:, t, cg * 512:(cg + 1) * 512], ALU.mult, ALU.add, [PSK[b], "gates", "acc_%d" % t], ["acc_%d" % t])
                    nd_ += 1
            p.barrier()
            top[0] = mix_top + 8 * 2048
            lng2 = A(2048)
            lnb2 = A(2048)
            xm = [A(2048) for _ in range(2)]
            junk = A(2048)
            st_ = A(64)
            dma(lng2, lnp[:, 4096:6144], [], ["lng2"], "lng2")
            dma(lnb2, lnp[:, 6144:8192], [], ["lnb2"], "lnb2")
            for t in range(8):
                xk = "acc_%d" % t
                a_t = acc3[:, t, :]
                xmk = "xm%d" % (t % 2)
                dma(xm[t % 2], xmid[t * 128:(t + 1) * 128, :], ["xmid"], [xmk], xmk)
                tt(a_t, a_t, g2p, ALU.mult, [xk, "g2p"], [xk])
                stt(a_t, xm[t % 2], ALU_ALPHA, a_t, ALU.mult, ALU.add, [xmk, xk], [xk])
                layer_norm(a_t, xk, lng2, lnb2, "lng2", "lnb2", t % 2, junk, "junk")
                fin.append(dma(xout[t * 128:(t + 1) * 128, :], a_t, [xk], [outk], "xout"))
            p.barrier()

        xts = None
        run_pass(0, 0, xA, xA, S1, True, "xA", "xA", "S1", noprefix=True)
        run_pass(0, 1, xA, xB, S2, False, "xA", "xB", "S2", reuse=True)
        run_pass(1, 1, S1, S2, xout_f, True, "S1", "S2", "xoutf")
        p.emit(fin)
    return nc


ALU_ALPHA = float(ALPHA)


def _consts():
    c = np.zeros((128, 3072), np.float32)
    c[:, 0:128] = np.eye(128, dtype=np.float32)
    c[:, 128:256] = np.arange(128, dtype=np.float32)[None, :]
    c[:, 256:384] = np.triu(np.ones((128, 128), np.float32))
    c[:, 384:512] = 1.0
    zt = np.zeros((128, 1024), np.float32)
    q = np.arange(128)[:, None]
    s = np.arange(128)[None, :]
    zt[:, 896:1024] = np.where(s <= q, 0.0, NEG)
    c[:, 1024:2048] = zt
    rot, half = 32, 16
    c[:, 2048:2064] = (500000.0 ** (-np.arange(half, dtype=np.float32) * 2.0 / rot))[None, :]
    rot, half = 16, 8
    c[:, 2064:2072] = (500000.0 ** (-np.arange(half, dtype=np.float32) * 2.0 / rot))[None, :]
    return c


def _layer_inputs(inp, l):
    f = np.float32
    rep = lambda v: np.ascontiguousarray(np.broadcast_to(np.asarray(v, f)[None, :], (128, v.shape[0])))
    d = {}
    d["b_ada"] = rep(inp["b_ada"][l])

    def st_layout(a):
        return np.ascontiguousarray(a.reshape(16, 2, 64).transpose(1, 2, 0).reshape(128, 16))
    lam_re = st_layout(inp["ssm_lam_re"][l])
    lam_im = st_layout(inp["ssm_lam_im"][l])
    lstep = st_layout(np.broadcast_to(inp["ssm_log_step"][l][:, None], (32, 64)))
    d["ssm_small"] = np.concatenate([lam_re, lam_im, lstep], axis=1).astype(f)

    def b_layout(a):
        return a.reshape(16, 2, 64, 16).transpose(1, 2, 0, 3).reshape(128, 256)

    def c_layout(a):
        return a.reshape(16, 2, 16, 64).transpose(1, 3, 0, 2).reshape(128, 256)
    d["ssm_bc"] = np.ascontiguousarray(np.concatenate(
        [b_layout(inp["ssm_b_re"][l]), b_layout(inp["ssm_b_im"][l]), c_layout(inp["ssm_c_re"][l]), c_layout(inp["ssm_c_im"][l])], axis=1)).astype(f)
    pk = lambda v: np.asarray(v, f).reshape(4, 128).T
    d["vecs"] = np.ascontiguousarray(np.concatenate([pk(inp["ssm_d"][l]), pk(inp["ssm_b_glu"][l]), pk(inp["pool_scale"][l])], axis=1))
    d["pool_w"] = np.ascontiguousarray(inp["pool_w"][l].reshape(512, 128))
    d["lnp"] = np.ascontiguousarray(np.concatenate([rep(inp["ln1_g"][l]), rep(inp["ln1_b"][l]), rep(inp["ln2_g"][l]), rep(inp["ln2_b"][l])], axis=1))
    return d


def _shared_inputs(inp):
    f = np.float32
    per = [_layer_inputs(inp, l) for l in range(2)]
    d = {k: np.ascontiguousarray(np.stack([per[0][k], per[1][k]], axis=0)) for k in per[0]}
    for k, src in (("w_ada", "w_ada"), ("w_in", "w_in"), ("w_out", "w_out"), ("w_glu", "ssm_w_glu"),
                   ("e_gate", "e_gate"), ("e_up", "e_up"), ("e_down", "e_down")):
        d[k] = np.ascontiguousarray(np.asarray(inp[src], f))
    d["w_router"] = np.ascontiguousarray(np.asarray(inp["w_router"], f).reshape(16, 128, 16).transpose(1, 0, 2).reshape(128, 256))
    d["cst"] = _consts()
    return d


def _cfg(pos_b, h):
    f = np.float32
    own = pos_b[h * 1024:(h + 1) * 1024].reshape(8, 128).T
    pre = pos_b[0:1024].reshape(8, 128).T if h == 1 else np.zeros((128, 8), np.int32)
    pos_in = np.concatenate([pre, own], axis=1).astype(np.int32)
    kbias = np.full((128, 1024), 0.0 if h == 1 else NEG, f)
    pc = np.zeros((128, 80), f)
    pc[:, 0] = float(h)
    for g, win in enumerate((2, 4, 8, 16)):
        t = np.arange(16) + h * 1024
        pc[:, 16 + g * 16:32 + g * 16] = (1.0 / np.minimum(t + 1, win))[None, :]
    return pos_in, kbias, pc


def _core_inputs(inp, core):
    b, h = core // 2, core % 2
    f = np.float32
    d = {}
    xb = np.asarray(inp["x"][b], f)
    d["xA"] = np.ascontiguousarray(xb[0:1024])
    d["xB"] = np.ascontiguousarray(xb[h * 1024:(h + 1) * 1024])
    d["c_in"] = np.ascontiguousarray(np.asarray(inp["c"][b], f).reshape(16, 128).T)
    pos = np.asarray(inp["positions"][b], np.int32)
    c0 = _cfg(pos, 0)
    c1 = _cfg(pos, h)
    d["pos_in"] = np.ascontiguousarray(np.stack([c0[0], c1[0]], 0))
    d["kbias"] = np.ascontiguousarray(np.stack([c0[1], c1[1]], 0))
    d["pcore"] = np.ascontiguousarray(np.stack([c0[2], c1[2]], 0))
    return d


_NC_CACHE = {}


def kernel(**inputs):
    inp = {k: np.asarray(v) for k, v in inputs.items()}
    if "prog" not in _NC_CACHE:
        _NC_CACHE["prog"] = build_program()
    nc = _NC_CACHE["prog"]
    shared = _shared_inputs(inp)
    in_maps = []
    for core in range(8):
        d = dict(shared)
        d.update(_core_inputs(inp, core))
        in_maps.append(d)
    res = run_bass_kernel_spmd(nc, in_maps, core_ids=list(range(8)))
    out = np.empty((4, 2048, 2048), np.float32)
    for core in range(8):
        b, h = core // 2, core % 2
        out[b, h * 1024:(h + 1) * 1024] = res.results[core]["xout"]
    return out
```
